# Optimizing a Trainium2 kernel written in Bass

```python
import jax, jax.numpy as jnp
from jax import lax
import numpy as np

D_MODEL = 1024
BATCH = 16
SEQ = 2048
DEPTH = 2

MEM_LEN = 256
N_MIXERS = 4
GROUP_WIDTH = D_MODEL // N_MIXERS
N_HEADS = 4
HEAD_DIM = GROUP_WIDTH // N_HEADS
GLA_KEY_DIM = HEAD_DIM // 2
GLA_LOWRANK = 16
GLA_GATE_NORM = 16.0
GDN_CONV = 4
CHUNK = 64
ROPE_BASE = 10000.0
XATTN_HEADS = 4
XATTN_HEAD_DIM = D_MODEL // XATTN_HEADS
D_FF = 2816
FFN_CONV = 3
EPS = 1e-6

IN_SPLITS = (
    GROUP_WIDTH, GROUP_WIDTH, GROUP_WIDTH, GROUP_WIDTH,
    GROUP_WIDTH, GROUP_WIDTH, GROUP_WIDTH, N_HEADS, N_HEADS, GROUP_WIDTH,
    N_HEADS * GLA_KEY_DIM, N_HEADS * GLA_KEY_DIM, GROUP_WIDTH, GLA_LOWRANK, GROUP_WIDTH,
    GROUP_WIDTH, GROUP_WIDTH, GROUP_WIDTH, GROUP_WIDTH,
)
IN_WIDTH = sum(IN_SPLITS)

kernel_name = 'hybrid_parallel_heads_decoder'


def _rmsnorm(x, w):
    xf = x.astype(jnp.float32)
    y = xf * lax.rsqrt(jnp.mean(xf * xf, axis=-1, keepdims=True) + EPS)
    return (y * w.astype(jnp.float32)).astype(x.dtype)


def _norm_f32(x):
    return x * lax.rsqrt(jnp.mean(x * x, axis=-1, keepdims=True) + EPS)


def _l2norm(x):
    return x * lax.rsqrt(jnp.sum(x * x, axis=-1, keepdims=True) + EPS)


def _rope(x, positions):
    d = x.shape[-1]
    freqs = ROPE_BASE ** (-jnp.arange(0, d, 2, dtype=jnp.float32) / d)
    ang = positions.astype(jnp.float32)[..., None] * freqs
    cos, sin = jnp.cos(ang)[:, :, None, :], jnp.sin(ang)[:, :, None, :]
    x1, x2 = jnp.split(x, 2, axis=-1)
    return jnp.concatenate([x1 * cos - x2 * sin, x1 * sin + x2 * cos], axis=-1)


def _causal_dwconv(x, w):
    k, c = w.shape
    return lax.conv_general_dilated(
        x, w[:, None, :].astype(x.dtype), window_strides=(1,), padding=[(k - 1, 0)],
        dimension_numbers=('NWC', 'WIO', 'NWC'), feature_group_count=c)


def _to_chunks(t):
    b, tt, h = t.shape[:3]
    t = t.reshape((b, tt // CHUNK, CHUNK, h) + t.shape[3:])
    return jnp.moveaxis(t, 3, 1)


def _from_chunks(t):
    b, h, n, c, d = t.shape
    return jnp.moveaxis(t, 1, 3).reshape(b, n * c, h, d)


def _retention_chunked(q, k, v, log_gamma):
    c = q.shape[3]
    idx = jnp.arange(c, dtype=jnp.float32)
    lg = log_gamma[:, None]
    rel = idx[:, None] - idx[None, :]
    decay = jnp.where(rel >= 0, jnp.exp(lg[:, :, None] * jnp.maximum(rel, 0.0)), 0.0)
    scores = jnp.einsum('bhncd,bhnsd->bhncs', q, k) * decay[None, :, None]
    o_intra = jnp.einsum('bhncs,bhnsv->bhncv', scores, v)
    q_decay = jnp.exp(lg * (idx + 1.0))
    k_decay = jnp.exp(lg * (c - 1.0 - idx))
    chunk_decay = jnp.exp(log_gamma * c)
    kv = jnp.einsum('bhncd,bhncv->bhndv', k * k_decay[None, :, None, :, None], v)

    def step(state, inp):
        kv_n, q_n = inp
        o = jnp.einsum('bhcd,bhdv->bhcv', q_n, state) * q_decay[None, :, :, None]
        state = state * chunk_decay[None, :, None, None] + kv_n
        return state, o

    b, h, _, _, dk = q.shape
    s0 = jnp.zeros((b, h, dk, v.shape[-1]), jnp.float32)
    _, o_inter = lax.scan(step, s0, (jnp.moveaxis(kv, 2, 0), jnp.moveaxis(q, 2, 0)))
    return o_intra + jnp.moveaxis(o_inter, 0, 2)


def _gated_delta_chunked(q, k, v, beta, g):
    c = q.shape[3]
    gc = jnp.cumsum(g, axis=-1)
    causal = jnp.tril(jnp.ones((c, c), dtype=bool))
    strict = jnp.tril(jnp.ones((c, c), dtype=bool), k=-1)
    ratio = jnp.exp(jnp.where(causal, gc[..., :, None] - gc[..., None, :], -jnp.inf))
    kk = jnp.where(strict, jnp.einsum('bhncd,bhnsd->bhncs', k, k) * ratio, 0.0)
    a_mat = jnp.eye(c, dtype=jnp.float32) + beta[..., None] * kk
    gamma = jnp.exp(gc)
    u_v = lax.linalg.triangular_solve(a_mat, beta[..., None] * v, left_side=True, lower=True, unit_diagonal=True)
    w_k = lax.linalg.triangular_solve(a_mat, (beta * gamma)[..., None] * k, left_side=True, lower=True, unit_diagonal=True)
    qk = jnp.einsum('bhncd,bhnsd->bhncs', q, k) * ratio
    q_g = q * gamma[..., None]
    k_tail = k * jnp.exp(gc[..., -1:] - gc)[..., None]
    chunk_decay = jnp.exp(gc[..., -1])

    def step(state, inp):
        uv_n, w_n, qk_n, qg_n, kt_n, cd_n = inp
        u = uv_n - jnp.einsum('bhcd,bhdv->bhcv', w_n, state)
        o = jnp.einsum('bhcd,bhdv->bhcv', qg_n, state) + jnp.einsum('bhcs,bhsv->bhcv', qk_n, u)
        state = cd_n[..., None, None] * state + jnp.einsum('bhcd,bhcv->bhdv', kt_n, u)
        return state, o

    b, h, _, _, dk = q.shape
    s0 = jnp.zeros((b, h, dk, v.shape[-1]), jnp.float32)
    xs = tuple(jnp.moveaxis(t, 2, 0) for t in (u_v, w_k, qk, q_g, k_tail, chunk_decay))
    _, o = lax.scan(step, s0, xs)
    return jnp.moveaxis(o, 0, 2)


def _gated_linear_chunked(q, k, v, log_f):
    c = q.shape[3]
    gc = jnp.cumsum(log_f, axis=3)
    causal = jnp.tril(jnp.ones((c, c), dtype=bool))[:, :, None]

    def step(state, inp):
        q_n, k_n, v_n, g_n = inp
        diff = g_n[:, :, :, None, :] - g_n[:, :, None, :, :]
        decay = jnp.exp(jnp.where(causal, diff, -jnp.inf))
        scores = jnp.einsum('bhcd,bhsd,bhcsd->bhcs', q_n, k_n, decay)
        o = jnp.einsum('bhcs,bhsv->bhcv', scores, v_n) + jnp.einsum('bhcd,bhdv->bhcv', q_n * jnp.exp(g_n), state)
        k_tail = k_n * jnp.exp(g_n[:, :, -1:] - g_n)
        state = jnp.exp(g_n[:, :, -1])[..., None] * state + jnp.einsum('bhcd,bhcv->bhdv', k_tail, v_n)
        return state, o

    b, h, _, _, dk = q.shape
    s0 = jnp.zeros((b, h, dk, v.shape[-1]), jnp.float32)
    xs = tuple(jnp.moveaxis(t, 2, 0) for t in (q, k, v, gc))
    _, o = lax.scan(step, s0, xs)
    return jnp.moveaxis(o, 0, 2)


def _hybrid_mixer(h, positions, w_in, gdn_conv_w, gdn_a_log, gdn_dt_bias, gdn_norm_w,
                  gla_gk_up, gla_gk_bias, gla_norm_w, hgrn_lb, hgrn_norm_w, w_out):
    b, t, _ = h.shape
    f32 = jnp.float32
    z = jnp.matmul(h, w_in).astype(f32)
    split_points = [int(p) for p in np.cumsum(IN_SPLITS)[:-1]]
    (rq, rk, rv, rg, bq, bk, bv, ba, bb, bg,
     cq, ck, cv, c_lr, cg, dq, df, di, dg) = jnp.split(z, split_points, axis=-1)

    def heads(a):
        return a.reshape(b, t, N_HEADS, -1)

    q_a = _rope(heads(rq), positions)
    k_a = _rope(heads(rk), positions) * HEAD_DIM ** -0.5
    log_gamma = jnp.log(1.0 - jnp.exp2(-5.0 - jnp.arange(N_HEADS, dtype=f32)))
    o_a = _from_chunks(_retention_chunked(_to_chunks(q_a), _to_chunks(k_a), _to_chunks(heads(rv)), log_gamma))
    o_a = _norm_f32(o_a) * jax.nn.silu(heads(rg))

    qkv = jax.nn.silu(_causal_dwconv(jnp.concatenate([bq, bk, bv], axis=-1), gdn_conv_w.astype(f32)))
    q_b, k_b, v_b = jnp.split(qkv, 3, axis=-1)
    q_b = _l2norm(heads(q_b)) * HEAD_DIM ** -0.5
    k_b = _l2norm(heads(k_b))
    beta = jax.nn.sigmoid(bb)
    g_b = -jnp.exp(gdn_a_log.astype(f32)) * jax.nn.softplus(ba + gdn_dt_bias.astype(f32))
    o_b = _from_chunks(_gated_delta_chunked(_to_chunks(q_b), _to_chunks(k_b), _to_chunks(heads(v_b)),
                                            _to_chunks(beta), _to_chunks(g_b)))
    o_b = _norm_f32(o_b) * gdn_norm_w.astype(f32) * jax.nn.silu(heads(bg))

    q_c = heads(cq) * GLA_KEY_DIM ** -0.5
    log_f_c = jax.nn.log_sigmoid(jnp.matmul(c_lr, gla_gk_up.astype(f32)) + gla_gk_bias.astype(f32)) / GLA_GATE_NORM
    o_c = _from_chunks(_gated_linear_chunked(_to_chunks(q_c), _to_chunks(heads(ck)), _to_chunks(heads(cv)),
                                             _to_chunks(heads(log_f_c))))
    o_c = _norm_f32(o_c) * gla_norm_w.astype(f32) * jax.nn.silu(heads(cg))

    lb = hgrn_lb.reshape(N_HEADS, HEAD_DIM)
    f_pre = heads(df)
    log_f_d = jnp.logaddexp(jnp.log(lb), jnp.log1p(-lb) + jax.nn.log_sigmoid(f_pre))
    k_d = (1.0 - lb) * jax.nn.sigmoid(-f_pre)
    o_d = _from_chunks(_gated_linear_chunked(_to_chunks(heads(dq)), _to_chunks(k_d), _to_chunks(heads(di)),
                                             _to_chunks(log_f_d)))
    o_d = _norm_f32(o_d) * hgrn_norm_w.astype(f32) * jax.nn.silu(heads(dg))

    o = jnp.concatenate([o_a.reshape(b, t, GROUP_WIDTH), o_b.reshape(b, t, GROUP_WIDTH),
                         o_c.reshape(b, t, GROUP_WIDTH), o_d.reshape(b, t, GROUP_WIDTH)], axis=-1)
    return jnp.matmul(o.astype(h.dtype), w_out)


def _memory_cross_attn(h, mem_n, wq, wk, wv, wo):
    b, t, _ = h.shape
    m = mem_n.shape[1]
    q = jnp.matmul(h, wq).reshape(b, t, XATTN_HEADS, XATTN_HEAD_DIM)
    k = jnp.matmul(mem_n, wk).reshape(b, m, XATTN_HEADS, XATTN_HEAD_DIM)
    v = jnp.matmul(mem_n, wv).reshape(b, m, XATTN_HEADS, XATTN_HEAD_DIM)
    s = jnp.einsum('bthd,bmhd->bhtm', q, k).astype(jnp.float32) * XATTN_HEAD_DIM ** -0.5
    p = jax.nn.softmax(s, axis=-1).astype(v.dtype)
    o = jnp.einsum('bhtm,bmhd->bthd', p, v).reshape(b, t, D_MODEL)
    return jnp.matmul(o, wo)


def _conv_ffn(h, w_up, conv_w, w_down):
    u = _causal_dwconv(jnp.matmul(h, w_up), conv_w)
    gate, val = jnp.split(u, 2, axis=-1)
    return jnp.matmul(jax.nn.silu(gate) * val, w_down)


def setup_inputs(seed: int = 0) -> dict:
    key = jax.random.key(seed)
    ks = jax.random.split(key, 26)
    nrm = jax.random.normal

    def gain(k, shape):
        return 1.0 + 0.02 * nrm(k, shape, jnp.float32)

    dt = jnp.exp(jax.random.uniform(ks[6], (DEPTH, N_HEADS), jnp.float32, np.log(1e-3), np.log(1e-1)))
    pos_offset = jax.random.randint(ks[2], (BATCH, 1), 0, 4096, dtype=jnp.int32)
    return {
        'x': nrm(ks[0], (BATCH, SEQ, D_MODEL), jnp.float32),
        'mem': nrm(ks[1], (BATCH, MEM_LEN, D_MODEL), jnp.float32),
        'positions': pos_offset + jnp.arange(SEQ, dtype=jnp.int32)[None, :],
        'mix_norm_w': gain(ks[3], (DEPTH, D_MODEL)),
        'w_in': nrm(ks[4], (DEPTH, D_MODEL, IN_WIDTH), jnp.float32) * D_MODEL ** -0.5,
        'gdn_conv_w': nrm(ks[5], (DEPTH, GDN_CONV, 3 * GROUP_WIDTH), jnp.float32) * GDN_CONV ** -0.5,
        'gdn_a_log': jnp.log(jax.random.uniform(ks[7], (DEPTH, N_HEADS), jnp.float32, 1.0, 16.0)),
        'gdn_dt_bias': dt + jnp.log(-jnp.expm1(-dt)),
        'gdn_norm_w': gain(ks[8], (DEPTH, HEAD_DIM)),
        'gla_gk_up': nrm(ks[9], (DEPTH, GLA_LOWRANK, N_HEADS * GLA_KEY_DIM), jnp.float32) * GLA_LOWRANK ** -0.5,
        'gla_gk_bias': 0.02 * nrm(ks[10], (DEPTH, N_HEADS * GLA_KEY_DIM), jnp.float32),
        'gla_norm_w': gain(ks[11], (DEPTH, HEAD_DIM)),
        'hgrn_lb_logits': 0.1 * nrm(ks[12], (DEPTH, GROUP_WIDTH), jnp.float32),
        'hgrn_norm_w': gain(ks[13], (DEPTH, HEAD_DIM)),
        'w_out': nrm(ks[14], (DEPTH, D_MODEL, D_MODEL), jnp.float32) * D_MODEL ** -0.5,
        'xattn_norm_w': gain(ks[15], (DEPTH, D_MODEL)),
        'mem_norm_w': gain(ks[16], (D_MODEL,)),
        'xattn_wq': nrm(ks[17], (DEPTH, D_MODEL, D_MODEL), jnp.float32) * D_MODEL ** -0.5,
        'xattn_wk': nrm(ks[18], (DEPTH, D_MODEL, D_MODEL), jnp.float32) * D_MODEL ** -0.5,
        'xattn_wv': nrm(ks[19], (DEPTH, D_MODEL, D_MODEL), jnp.float32) * D_MODEL ** -0.5,
        'xattn_wo': nrm(ks[20], (DEPTH, D_MODEL, D_MODEL), jnp.float32) * D_MODEL ** -0.5,
        'ffn_norm_w': gain(ks[21], (DEPTH, D_MODEL)),
        'ffn_up': nrm(ks[22], (DEPTH, D_MODEL, 2 * D_FF), jnp.float32) * D_MODEL ** -0.5,
        'ffn_conv_w': nrm(ks[23], (DEPTH, FFN_CONV, 2 * D_FF), jnp.float32) * FFN_CONV ** -0.5,
        'ffn_down': nrm(ks[24], (DEPTH, D_FF, D_MODEL), jnp.float32) * D_FF ** -0.5,
        'final_norm_w': gain(ks[25], (D_MODEL,)),
    }


def reference(x, mem, positions, mix_norm_w, w_in, gdn_conv_w, gdn_a_log, gdn_dt_bias, gdn_norm_w,
              gla_gk_up, gla_gk_bias, gla_norm_w, hgrn_lb_logits, hgrn_norm_w, w_out,
              xattn_norm_w, mem_norm_w, xattn_wq, xattn_wk, xattn_wv, xattn_wo,
              ffn_norm_w, ffn_up, ffn_conv_w, ffn_down, final_norm_w):
    lb_all = jnp.cumsum(jax.nn.softmax(hgrn_lb_logits.astype(jnp.float32), axis=0), axis=0)
    lb_all = lb_all - lb_all[0]
    mem_n = _rmsnorm(mem, mem_norm_w)
    h = x
    for l in range(DEPTH):
        h = h + _hybrid_mixer(_rmsnorm(h, mix_norm_w[l]), positions, w_in[l], gdn_conv_w[l], gdn_a_log[l],
                              gdn_dt_bias[l], gdn_norm_w[l], gla_gk_up[l], gla_gk_bias[l], gla_norm_w[l],
                              lb_all[l], hgrn_norm_w[l], w_out[l])
        h = h + _memory_cross_attn(_rmsnorm(h, xattn_norm_w[l]), mem_n, xattn_wq[l], xattn_wk[l],
                                   xattn_wv[l], xattn_wo[l])
        h = h + _conv_ffn(_rmsnorm(h, ffn_norm_w[l]), ffn_up[l], ffn_conv_w[l], ffn_down[l])
    return _rmsnorm(h, final_norm_w)
```

```python
import contextlib
import math
import types
import numpy as np
import concourse.bass as bass
import concourse.mybir as mybir
from concourse.bass_utils import run_bass_kernel_spmd

F32 = mybir.dt.float32
BF16 = mybir.dt.bfloat16
I32 = mybir.dt.int32
AF = mybir.ActivationFunctionType
ALU = mybir.AluOpType

SEM_LIMIT = 30000
T = 2048
D = 1024
DFF = 2816
EPS = 1e-6
NEG = -30000.0


class Prog:
    ENGS = ("tensor", "vector", "scalar", "gpsimd", "sync")

    def __init__(self, nc):
        self.nc = nc
        self.ops = []
        self.last_w = {}
        self.readers = {}

    @staticmethod
    def _freeze(fn):
        if fn.__closure__ is None:
            return fn
        cells = []
        for c in fn.__closure__:
            try:
                cells.append(types.CellType(c.cell_contents))
            except ValueError:
                cells.append(c)
        return types.FunctionType(fn.__code__, fn.__globals__, fn.__name__, fn.__defaults__, tuple(cells))

    def op(self, eng, fn, reads=(), writes=(), dma=False):
        fn = self._freeze(fn)
        i = len(self.ops)
        deps = set()
        for k in reads:
            w = self.last_w.get(k)
            if w is not None:
                deps.add(w)
        for k in writes:
            w = self.last_w.get(k)
            if w is not None:
                deps.add(w)
            for r in self.readers.get(k, ()):
                deps.add(r)
        deps.discard(i)
        for k in reads:
            self.readers.setdefault(k, []).append(i)
        for k in writes:
            self.last_w[k] = i
            self.readers[k] = []
        self.ops.append(dict(eng=eng, fn=fn, deps=deps, dma=dma))
        return i

    def emit(self, final_wait_ops=()):
        nc = self.nc
        ops = self.ops
        needed = set()
        for o in ops:
            for d in o["deps"]:
                if ops[d]["eng"] == "tensor" and o["eng"] == "tensor" and not ops[d]["dma"] and not o["dma"]:
                    continue
                needed.add(d)
        for d in final_wait_ops:
            needed.add(d)
        DR = 8
        counters = {}
        sig = {}
        for i, o in enumerate(ops):
            if o["dma"]:
                n = counters.get(("dma", o["eng"]), 0)
                counters[("dma", o["eng"])] = n + 1
                sig[i] = (("d", o["eng"], n % DR), n // DR + 1, 16)
                continue
            if i not in needed:
                continue
            c = counters.get(o["eng"], 0)
            counters[o["eng"]] = c + 1
            sig[i] = (("c", o["eng"], c // SEM_LIMIT), c % SEM_LIMIT + 1, 1)
        with contextlib.ExitStack() as es:
            sems = {}
            for i in sorted(sig):
                key = sig[i][0]
                if key not in sems:
                    sems[key] = es.enter_context(nc.semaphore("s_%s_%s_%d" % key))
            block = es.enter_context(nc.Block())

            def make_body(engname):
                def body(e):
                    waited = {}

                    def wait(key, v, mult):
                        if waited.get(key, 0) >= v:
                            return
                        e.wait_ge(sems[key], v * mult)
                        waited[key] = v
                        if key[0] == "c":
                            for jj in range(key[2]):
                                waited[(key[0], key[1], jj)] = SEM_LIMIT

                    for i, o in enumerate(ops):
                        if o["eng"] != engname:
                            continue
                        for d in sorted(o["deps"]):
                            if d not in sig:
                                continue
                            if ops[d]["eng"] == "tensor" and engname == "tensor" and not ops[d]["dma"] and not o["dma"]:
                                continue
                            key, v, mult = sig[d]
                            wait(key, v, mult)
                        if o["dma"]:
                            key, v, mult = sig[i]
                            if v > 1:
                                wait(key, v - 1, mult)
                        ins = o["fn"](e)
                        if i in sig:
                            key, v, mult = sig[i]
                            ins.then_inc(sems[key], mult)
                    if engname == "sync":
                        for d in final_wait_ops:
                            key, v, mult = sig[d]
                            wait(key, v, mult)
                return body

            for engname in self.ENGS:
                if any(o["eng"] == engname for o in ops) or engname == "sync":
                    getattr(block, engname)(make_body(engname))


def make_consts():
    c = {}
    c["ident"] = np.eye(128, dtype=np.float32)
    j = np.arange(64)
    c["tri"] = (j[:, None] <= j[None, :]).astype(np.float32)
    c["masku"] = (j[:, None] > j[None, :]).astype(np.float32)
    c["negs"] = np.where(j[None, :] >= j[:, None], NEG, 0.0).astype(np.float32)
    c["negi"] = np.where(j[:, None] > j[None, :], NEG, 0.0).astype(np.float32)
    c["caus32"] = ((j[:, None] % 32) <= j[None, :32]).astype(np.float32)
    gam = 1.0 - 2.0 ** (-5.0 - np.arange(4))
    lg = np.log(gam)
    maskr = np.zeros((64, 4, 64), np.float64)
    gq = np.zeros((64, 4, 64), np.float64)
    gk = np.zeros((64, 4), np.float64)
    for h in range(4):
        rel = j[None, :] - j[:, None]
        maskr[:, h, :] = np.where(rel >= 0, np.exp(lg[h] * np.maximum(rel, 0)), 0.0) / 8.0
        gq[:, h, :] = np.exp(lg[h] * (j[None, :] + 1.0))
        gk[:, h] = np.exp(lg[h] * (63.0 - j)) / 8.0
    c["maskr"] = maskr.astype(np.float32)
    c["gq"] = gq.astype(np.float32)
    c["gk"] = gk.astype(np.float32)
    c["retdec"] = [float(np.exp(lg[h] * 64.0)) for h in range(4)]
    p = np.arange(64)
    freq = 10000.0 ** (-(np.arange(0, 64, 2, dtype=np.float32)) / 64.0)
    c["freq"] = freq.astype(np.float32)[p % 32].reshape(64, 1).astype(np.float32)
    c["sgn"] = np.where(p < 32, -1.0, 1.0).reshape(64, 1).astype(np.float32)
    perm = np.zeros((64, 64), np.float32)
    perm[(p + 32) % 64, p] = 1.0
    c["perm"] = perm
    t = np.arange(T)
    c["rst"] = np.stack([(t % 64 != 0), (t % 32 != 0)]).astype(np.float32)
    return c


CONST_SHAPES = dict(ident=[128, 128], tri=[64, 64], masku=[64, 64], negs=[64, 64], negi=[64, 64],
                    caus32=[64, 32], maskr=[64, 4, 64], gq=[64, 4, 64], gk=[64, 4], freq=[64, 1],
                    sgn=[64, 1], perm=[64, 64], rst=[2, T])

WEIGHTS = dict(
    mix_norm_w=[2, D], w_in=[2, D, 3864], gdn_conv_w=[2, 4, 768], gdn_a_log=[2, 4], gdn_dt_bias=[2, 4],
    gdn_norm_w=[2, 64], gla_gk_up=[2, 16, 128], gla_gk_bias=[2, 128], gla_norm_w=[2, 64],
    hgrn_lb_logits=[2, 256], hgrn_norm_w=[2, 64], w_out=[2, D, D], xattn_norm_w=[2, D], mem_norm_w=[D],
    xattn_wq=[2, D, D], xattn_wk=[2, D, D], xattn_wv=[2, D, D], xattn_wo=[2, D, D], ffn_norm_w=[2, D],
    ffn_up=[2, D, 2 * DFF], ffn_conv_w=[2, 3, 2 * DFF], ffn_down=[2, DFF, D], final_norm_w=[D])

RQ, RK, RV, RG = 0, 256, 512, 768
BQ, BK, BV, BA, BB, BG = 1024, 1280, 1536, 1792, 1796, 1800
CQ, CK, CV, CLR, CG = 2056, 2184, 2312, 2568, 2584
DQ, DF_, DI, DG = 2840, 3096, 3352, 3608


def build(NSEQ=2, NLAYER=2, stages=("mix", "xat", "ffn"), mixers=(0, 1, 2, 3), dbg=False, seqs=None, layers=None):
    nc = bass.Bass("TRN2", target_bir_lowering=False)
    dr = {}
    dr["x"] = nc.dram_tensor("x", [2, T, D], F32, kind="ExternalInput").ap()
    dr["mem"] = nc.dram_tensor("mem", [2, 256, D], F32, kind="ExternalInput").ap()
    dr["positions"] = nc.dram_tensor("positions", [2, T], I32, kind="ExternalInput").ap()
    for k, shp in WEIGHTS.items():
        dr[k] = nc.dram_tensor(k, shp, F32, kind="ExternalInput").ap()
    for k, shp in CONST_SHAPES.items():
        dr["c_" + k] = nc.dram_tensor("c_" + k, shp, F32, kind="ExternalInput").ap()
    y_d = nc.dram_tensor("y", [2, T, D], F32, kind="ExternalOutput").ap()
    if dbg:
        dbg_f = nc.dram_tensor("dbg_f", [64, 8, T], F32, kind="ExternalOutput").ap()
        dbg_b = nc.dram_tensor("dbg_b", [64, 8, T], BF16, kind="ExternalOutput").ap()
    C = make_consts()

    with contextlib.ExitStack() as es:
        def sb(name, shape, dt):
            return es.enter_context(nc.sbuf_tensor(name, shape, dt))

        P = Prog(nc)
        es.enter_context(nc.allow_non_contiguous_dma(reason='small strided parameter loads'))

        def V(fn, r=(), w=()):
            return P.op("vector", fn, r, w)

        def A(fn, r=(), w=()):
            return P.op("scalar", fn, r, w)

        def TE(fn, r=(), w=()):
            return P.op("tensor", fn, r, w)

        def GP(fn, r=(), w=()):
            return P.op("gpsimd", fn, r, w)

        def DS(fn, r=(), w=()):
            return P.op("sync", fn, r, w, dma=True)

        def DG_(fn, r=(), w=()):
            return P.op("gpsimd", fn, r, w, dma=True)

        tap_ops = []

        def tap(idx, ap, keys, n=T):
            if not dbg:
                return
            dst = (dbg_f if ap.dtype == F32 else dbg_b)[0:ap.shape[0], idx, 0:n]
            tap_ops.append(DS(lambda e: e.dma_start(out=dst, in_=ap), r=keys))

        banks = [es.enter_context(nc.psum_tensor(f"pb{i}", [128, 512], F32)) for i in range(8)]
        bank_ctr = [0]

        def nb():
            i = bank_ctr[0] % 8
            bank_ctr[0] += 1
            return banks[i], ("pb", i)

        hT = sb("hT", [128, 8, T], F32)
        xnT = sb("xnT", [128, 8, T], BF16)
        NSLOT = 4
        W = sb("W", [128, NSLOT, 2048], BF16)
        slot_ctr = [0]

        def wslot():
            i = slot_ctr[0] % NSLOT
            slot_ctr[0] += 1
            return i, ("W", i)

        REG = {"F0": (0, 4096), "F1": (4096, 4096), "F2": (8192, 4096)}
        for i in range(5):
            REG["B%d" % i] = (12288 + i * 2048, 2048)
        REG["TM0"] = (22528, 2048)
        REG["TM1"] = (24576, 2048)
        for h in range(4):
            REG[("OM", h)] = (26624 + h * 2048, 2048)
        SCRN = 34816
        SCR = sb("SCR", [128, SCRN], BF16)
        ROPE = sb("ROPE", [128, 4096], BF16)

        def rk(off, n):
            return [k for k, (o, m) in REG.items() if o < off + n and off < o + m]

        def reg_bf(key, parts=64):
            o, m = REG[key]
            return SCR[0:parts, o:o + m]

        def reg_f32(key, parts=64):
            o, m = REG[key]
            return SCR[0:parts, o:o + m].bitcast(F32)

        F = [reg_f32("F%d" % i) for i in range(3)]
        Bf = [reg_bf("B%d" % i) for i in range(5)]
        TM = [reg_bf("TM0"), reg_bf("TM1")]
        OMv = [reg_bf(("OM", h)) for h in range(4)]
        cosT = ROPE[0:64, 0:2048]
        sinS = ROPE[0:64, 2048:4096]
        sq = SCR[:, 26624:26624 + 4096].rearrange("p (k t) -> p k t", t=512)
        SQK = [("OM", 0), ("OM", 1)]
        rsb = sb("rsb", [128, 512], F32)
        ident = sb("ident", [128, 128], F32)
        identb = sb("identb", [128, 128], BF16)
        onesb = sb("onesb", [128, 128], BF16)
        ones64f = sb("ones64f", [64, 64], F32)
        onecol = sb("onecol", [64, 1], F32)
        cst = {}
        for k in ("tri", "masku", "negs", "negi", "caus32", "maskr", "gq", "gk", "freq", "sgn", "perm"):
            cst[k] = sb("k_" + k, CONST_SHAPES[k], F32)
        permb = sb("permb", [64, 64], BF16)
        ident64 = ident[0:64, 0:64]
        normw = sb("normw", [128, 7, 8], F32)
        memnw = sb("memnw", [128, 8], F32)
        hw = sb("hw", [64, 16], F32)
        gsc = sb("gsc", [64, 24], F32)
        glab = sb("glab", [32, 8], F32)
        glup = sb("glup", [16, 2, 128], F32)
        gcw = sb("gcw", [64, 2, 4, 12], F32)
        S32 = sb("S32", [64, 64], F32)
        ab_all = sb("ab_all", [64, 256], F32)
        Sbz = sb("Sbz", [64, 64], BF16)
        Sbf = sb("Sbf", [64, 64], BF16)
        PTb = [sb(f"PTb{i}", [64, 64], BF16) for i in range(2)]
        small = sb("small", [64, 8, 32], F32)
        decs_t = sb("decs_t", [64, 64], F32)
        memT = sb("memT", [128, 8, 256], BF16)
        wsmall = sb("wsmall", [128, 8, 16], BF16)

        for k in cst:
            DS(lambda e, k=k: e.dma_start(out=cst[k][:], in_=dr["c_" + k]), w=[("c", k)])
        DS(lambda e: e.dma_start(out=ident[:], in_=dr["c_ident"]), w=["ident"])
        V(lambda e: e.tensor_copy(out=identb[:], in_=ident[:]), r=["ident"], w=["identb"])
        V(lambda e: e.memset(onesb[:], 1.0), w=["onesb"])
        V(lambda e: e.memset(ones64f[:], 1.0), w=["ones64f"])
        V(lambda e: e.memset(onecol[:], 1.0), w=["onecol"])
        V(lambda e: e.memset(Sbz[:], 0.0), w=["Sbf"])
        V(lambda e: e.tensor_copy(out=permb[:], in_=cst["perm"][:]), r=[("c", "perm")], w=["permb"])
        for a, nm in enumerate(["mix_norm_w", "xattn_norm_w", "ffn_norm_w"]):
            for l in range(2):
                DS(lambda e, a=a, l=l, nm=nm: e.dma_start(out=normw[:, 2 * a + l, :], in_=dr[nm][l].rearrange("(k p) -> p k", p=128)), w=["normw"])
        DS(lambda e: e.dma_start(out=normw[:, 6, :], in_=dr["final_norm_w"].rearrange("(k p) -> p k", p=128)), w=["normw"])
        DS(lambda e: e.dma_start(out=memnw[:], in_=dr["mem_norm_w"].rearrange("(k p) -> p k", p=128)), w=["memnw"])
        for a, nm in enumerate(["gdn_norm_w", "gla_norm_w", "hgrn_norm_w"]):
            DS(lambda e, a=a, nm=nm: e.dma_start(out=hw[:, 2 * a:2 * a + 2], in_=dr[nm].rearrange("l p -> p l")), w=["hw"])
        for l in range(2):
            DS(lambda e, l=l: e.dma_start(out=gsc[:, 16 + 4 * l:20 + 4 * l], in_=dr["hgrn_lb_logits"][l].rearrange("(h p) -> p h", p=64)), w=["gsc"])
        A(lambda e: e.activation(out=gsc[:, 16:24], in_=gsc[:, 16:24], func=AF.Exp), r=["gsc"], w=["gsc"])
        V(lambda e: e.tensor_tensor(out=hw[:, 10:14], in0=gsc[:, 16:20], in1=gsc[:, 20:24], op=ALU.add), r=["gsc"], w=["hw"])
        V(lambda e: e.reciprocal(out=hw[:, 10:14], in_=hw[:, 10:14]), r=["hw"], w=["hw"])
        V(lambda e: e.tensor_tensor(out=hw[:, 6:10], in0=gsc[:, 20:24], in1=hw[:, 10:14], op=ALU.mult), r=["hw", "gsc"], w=["hw"])
        V(lambda e: e.tensor_tensor(out=hw[:, 10:14], in0=gsc[:, 16:20], in1=hw[:, 10:14], op=ALU.mult), r=["hw", "gsc"], w=["hw"])
        V(lambda e: e.memset(hw[:, 14:15], 0.0), w=["hw"])
        V(lambda e: e.memset(hw[:, 15:16], 1.0), w=["hw"])
        DS(lambda e: e.dma_start(out=gsc[:, 0:8], in_=dr["gdn_a_log"].rearrange("l h -> (l h)").unsqueeze(0).broadcast_to([64, 8])), w=["gsc"])
        DS(lambda e: e.dma_start(out=gsc[:, 8:16], in_=dr["gdn_dt_bias"].rearrange("l h -> (l h)").unsqueeze(0).broadcast_to([64, 8])), w=["gsc"])
        A(lambda e: e.activation(out=gsc[:, 0:8], in_=gsc[:, 0:8], func=AF.Exp), r=["gsc"], w=["gsc"])
        V(lambda e: e.tensor_scalar(out=gsc[:, 0:8], in0=gsc[:, 0:8], scalar1=-1.0, scalar2=None, op0=ALU.mult), r=["gsc"], w=["gsc"])
        for l in range(2):
            DS(lambda e, l=l: e.dma_start(out=glab[:, 4 * l:4 * l + 4], in_=dr["gla_gk_bias"][l].rearrange("(h p) -> p h", p=32)), w=["glab"])
            DS(lambda e, l=l: e.dma_start(out=glup[:, l, :], in_=dr["gla_gk_up"][l]), w=["glup"])
            for k4 in range(4):
                DS(lambda e, l=l, k4=k4: e.dma_start(out=gcw[:, l, k4, :], in_=dr["gdn_conv_w"][l, k4].rearrange("(j p) -> p j", p=64)), w=["gcw"])
        V(lambda e: e.tensor_scalar(out=glab[:], in0=glab[:], scalar1=-1.0, scalar2=None, op0=ALU.mult), r=["glab"], w=["glab"])

        def w_cols(name, l, c0, n):
            src = dr[name][l][:, c0:c0 + n].rearrange("(k p) m -> p k m", p=128)
            i, key = wslot()
            dst = W[:, i, 0:8 * n].rearrange("p (k m) -> p k m", m=n)
            DG_(lambda e, dst=dst, src=src: e.dma_start(out=dst, in_=src), w=[key])
            return dst, key

        def w_small(l, c0, n):
            src = dr["w_in"][l][:, c0:c0 + n].rearrange("(k p) m -> p k m", p=128)
            dst = wsmall[:, :, 0:n]
            DG_(lambda e: e.dma_start(out=dst, in_=src), w=["wsmall"])
            return dst, "wsmall"

        def norm(widx):
            for tt in range(4):
                ts = slice(tt * 512, (tt + 1) * 512)
                A(lambda e, ts=ts: e.activation(out=sq, in_=hT[:, :, ts], func=AF.Square), r=["hT"], w=SQK)
                pb, pk = nb()
                for k in range(8):
                    TE(lambda e, k=k, pb=pb: e.matmul(pb[:], lhsT=onesb[:], rhs=sq[:, k, :], start=(k == 0), stop=(k == 7)), r=SQK + ["onesb"], w=[pk])
                A(lambda e, pb=pb: e.activation(out=rsb[:], in_=pb[:], func=AF.Ln, scale=1.0 / D, bias=EPS), r=[pk], w=["rsb"])
                A(lambda e: e.activation(out=rsb[:], in_=rsb[:], func=AF.Exp, scale=-0.5), r=["rsb"], w=["rsb"])
                for k in range(8):
                    V(lambda e, k=k, ts=ts: e.scalar_tensor_tensor(out=xnT[:, k, ts], in0=hT[:, k, ts], scalar=normw[:, widx, k:k + 1], in1=rsb[:], op0=ALU.mult, op1=ALU.mult),
                      r=["hT", "rsb", "normw"], w=[("xnT", tt)])

        def proj64(wv, wkey, c0, M, tt):
            pb, pk = nb()
            for k in range(8):
                TE(lambda e, k=k, pb=pb: e.matmul(pb[0:M, :], lhsT=wv[:, k, c0:c0 + M], rhs=xnT[:, k, tt * 512:(tt + 1) * 512], start=(k == 0), stop=(k == 7)),
                   r=[wkey, ("xnT", tt)], w=[pk])
            return pb, pk

        def to_tm(src, skey, dst, dkey, Cc, Dd, scale_ap=None):
            nch = T // Cc
            per = 1024 // Dd
            dv = dst[0:Cc, 0:nch * Dd].rearrange("p (n d) -> p n d", d=Dd)
            for g in range(nch // per):
                pb, pk = nb()
                pbb = pb[:].bitcast(BF16)
                for j in range(per):
                    n = g * per + j
                    TE(lambda e, n=n, j=j, pbb=pbb: e.transpose(out=pbb[0:Cc, j * Dd:(j + 1) * Dd], in_=src[0:Dd, n * Cc:(n + 1) * Cc], identity=identb[0:Dd, 0:Dd]),
                       r=[skey, "identb"], w=[pk])
                o = dv[:, g * per:(g + 1) * per, :]
                i_ = pbb[0:Cc, :].rearrange("p (n d) -> p n d", d=Dd)
                A(lambda e, o=o, i_=i_: e.activation(out=o, in_=i_, func=AF.Copy), r=[pk], w=[dkey])
                if scale_ap is not None:
                    V(lambda e, o=o: e.tensor_scalar(out=o, in0=o, scalar1=scale_ap, scalar2=None, op0=ALU.mult), r=[dkey], w=[dkey])
            return dv

        def headnorm_gate(osrc, okey, gate, gkey, nw_ap, h, tmpk="TM0"):
            tmp = reg_bf(tmpk)
            A(lambda e: e.activation(out=tmp, in_=osrc, func=AF.Square), r=[okey], w=[tmpk])
            for tt in range(4):
                ts = slice(tt * 512, (tt + 1) * 512)
                pb, pk = nb()
                TE(lambda e, pb=pb, ts=ts: e.matmul(pb[0:64, :], lhsT=onesb[0:64, 0:64], rhs=tmp[:, ts], start=True, stop=True), r=[tmpk, "onesb"], w=[pk])
                A(lambda e, pb=pb, ts=ts: e.activation(out=rsb[0:64, :], in_=pb[0:64, :], func=AF.Ln, scale=1.0 / 64, bias=EPS), r=[pk], w=["rsb"])
                A(lambda e: e.activation(out=rsb[0:64, :], in_=rsb[0:64, :], func=AF.Exp, scale=-0.5), r=["rsb"], w=["rsb"])
                V(lambda e, ts=ts: e.scalar_tensor_tensor(out=osrc[:, ts], in0=osrc[:, ts], scalar=nw_ap, in1=rsb[0:64, :], op0=ALU.mult, op1=ALU.mult), r=[okey, "rsb", "hw"], w=[okey])
            V(lambda e: e.tensor_tensor(out=OMv[h], in0=osrc, in1=gate, op=ALU.mult), r=[okey, gkey], w=[("OM", h)])

        def out_proj(l, row0):
            src = dr["w_out"][l][row0:row0 + 256, :].rearrange("(h p) m -> p h m", p=64)
            i, key = wslot()
            i2, key2 = wslot()
            dsts = [W[0:64, i, :].rearrange("p (h m) -> p h m", m=1024), W[0:64, i2, :].rearrange("p (h m) -> p h m", m=1024)]
            DG_(lambda e: e.dma_start(out=dsts[0], in_=src[:, 0:2, :]), w=[key])
            DG_(lambda e: e.dma_start(out=dsts[1], in_=src[:, 2:4, :]), w=[key2])
            for mt in range(8):
                for tt in range(4):
                    ts = slice(tt * 512, (tt + 1) * 512)
                    pb, pk = nb()
                    for h in range(4):
                        TE(lambda e, h=h, pb=pb, mt=mt, ts=ts: e.matmul(pb[:], lhsT=dsts[h // 2][:, h % 2, mt * 128:(mt + 1) * 128], rhs=OMv[h][:, ts], start=(h == 0), stop=(h == 3)),
                           r=[key, key2, ("OM", h)], w=[pk])
                    V(lambda e, pb=pb, mt=mt, ts=ts: e.tensor_tensor(out=hT[:, mt, ts], in0=hT[:, mt, ts], in1=pb[:], op=ALU.add), r=[pk, "hT"], w=["hT"])

        def state_init():
            V(lambda e: e.memset(S32[:], 0.0), w=["S32"])
            V(lambda e: e.memset(Sbf[:], 0.0), w=["Sbf"])

        def state_update(Dk, p3, k3, dec):
            V(lambda e: e.scalar_tensor_tensor(out=S32[0:Dk, :], in0=S32[0:Dk, :], scalar=dec, in1=p3[0:Dk, 0:64], op0=ALU.mult, op1=ALU.add), r=["S32", k3, "dec"], w=["S32"])
            A(lambda e: e.activation(out=Sbf[0:Dk, :], in_=S32[0:Dk, :], func=AF.Copy), r=["S32"], w=["Sbf"])

        SBK = [("Sb", v) for v in range(64)]

        def lin_loop(Cc, Dk, ks, kskey, qs, qskey, qi, qikey, vtm, ktm, mask_ap, mkey, dec_ap, odst, okey):
            nblk = T // Cc
            npass = nblk // 32
            PTall = reg_bf("B2")
            SKV = F[0].rearrange("p (n v) -> p n v", v=64)
            Sball = reg_bf("F1")

            def blk(n):
                if Cc == 64:
                    return slice(0, 64), n, slice(n * 64, (n + 1) * 64)
                return slice((n % 2) * 32, (n % 2) * 32 + 32), n // 2, slice(n * 32, (n + 1) * 32)

            def phase_kv(ps_i):
                if Cc == 64:
                    for g in range(4):
                        p3, k3 = nb()
                        for j in range(8):
                            n = ps_i * 32 + g * 8 + j
                            pp, c64, cs = blk(n)
                            TE(lambda e, p3=p3, j=j, pp=pp, c64=c64: e.matmul(p3[0:Dk, j * 64:(j + 1) * 64], lhsT=ktm[pp, c64, :], rhs=vtm[pp, c64, :], start=True, stop=True), r=["TM1", "TM0"], w=[k3])
                        A(lambda e, p3=p3, g=g: e.activation(out=SKV[0:Dk, g * 8:(g + 1) * 8, :], in_=p3[0:Dk, :].rearrange("p (n v) -> p n v", v=64), func=AF.Copy), r=[k3], w=["F0"])
                    return
                SKVp = F[0].rearrange("p (m two v) -> p two m v", two=2, v=64)
                for g in range(2):
                    pbs = [nb(), nb()]
                    for j in range(16):
                        n = ps_i * 32 + g * 16 + j
                        pp, c64, cs = blk(n)
                        p3, k3 = pbs[n % 2]
                        jj = j // 2
                        TE(lambda e, p3=p3, jj=jj, pp=pp, c64=c64: e.matmul(p3[0:Dk, jj * 64:(jj + 1) * 64], lhsT=ktm[pp, c64, :], rhs=vtm[pp, c64, :], start=True, stop=True), r=["TM1", "TM0"], w=[k3])
                    for par in range(2):
                        p3, k3 = pbs[par]
                        A(lambda e, p3=p3, g=g, par=par: e.activation(out=SKVp[0:Dk, par, g * 8:(g + 1) * 8, :], in_=p3[0:Dk, :].rearrange("p (n v) -> p n v", v=64), func=AF.Copy), r=[k3], w=["F0"])

            def phase_scores():
                if Cc == 64:
                    PTv = PTall.rearrange("p (n c) -> p n c", c=64)
                    for g in range(4):
                        p1, k1 = nb()
                        for j in range(8):
                            n = g * 8 + j
                            pp, c64, cs = blk(n)
                            TE(lambda e, p1=p1, j=j, cs=cs: e.matmul(p1[0:64, j * 64:(j + 1) * 64], lhsT=ks[0:Dk, cs], rhs=qs[0:Dk, cs], start=True, stop=True), r=[kskey, qskey], w=[k1])
                        V(lambda e, p1=p1, g=g: e.tensor_tensor(out=PTv[:, g * 8:(g + 1) * 8, :], in0=p1[0:64, :].rearrange("p (n c) -> p n c", c=64), in1=mask_ap.unsqueeze(1).to_broadcast([64, 8, 64]), op=ALU.mult), r=[k1, mkey], w=["B2"])
                    return PTv
                PTv = PTall[:, 0:1024].rearrange("p (n c) -> p n c", c=32)
                for g in range(2):
                    pbs = [nb(), nb()]
                    for j in range(32):
                        n = g * 32 + j
                        pp, c64, cs = blk(n)
                        cl = c64 - g * 16
                        p1, k1 = pbs[n % 2]
                        TE(lambda e, p1=p1, pp=pp, cl=cl, cs=cs: e.matmul(p1[pp, cl * 32:(cl + 1) * 32], lhsT=ks[0:Dk, cs], rhs=qs[0:Dk, cs], start=True, stop=True), r=[kskey, qskey], w=[k1])
                    for par in range(2):
                        p1, k1 = pbs[par]
                        pp = slice(par * 32, par * 32 + 32)
                        V(lambda e, p1=p1, g=g, pp=pp: e.tensor_tensor(out=PTv[pp, g * 16:(g + 1) * 16, :], in0=p1[pp, :].rearrange("p (n c) -> p n c", c=32), in1=mask_ap[pp, :].unsqueeze(1).to_broadcast([32, 16, 32]), op=ALU.mult), r=[k1, mkey], w=["B2"])
                return PTv

            def phase_scan(ps_i):
                Sb = Sball[:, ps_i * 2048:(ps_i + 1) * 2048].rearrange("p (n v) -> p n v", v=64)
                for v in range(64):
                    init = 0.0 if ps_i == 0 else S32[0:Dk, v:v + 1]
                    V(lambda e, v=v, init=init, Sb=Sb: e.tensor_tensor_scan(out=Sb[0:Dk, :, v], data0=dec_ap[:, ps_i * 32:(ps_i + 1) * 32], data1=SKV[0:Dk, :, v], initial=init, op0=ALU.mult, op1=ALU.add),
                      r=["F0", "dec", "S32"], w=([("Sb", ps_i, v), "F1"] if v == 0 else [("Sb", ps_i, v)]))
                return Sb

            def phase_out(ps_i, PTv, Sb, carry):
                SBKp = [("Sb", ps_i, v) for v in range(64)]
                if Cc == 64:
                    for g in range(4):
                        p2, k2 = nb()
                        for j in range(8):
                            nl = g * 8 + j
                            n = ps_i * 32 + nl
                            pp, c64, cs = blk(n)
                            first = (nl == 0 and carry is None)
                            TE(lambda e, p2=p2, j=j, pp=pp, c64=c64, first=first: e.matmul(p2[0:64, j * 64:(j + 1) * 64], lhsT=vtm[pp, c64, :], rhs=PTv[pp, c64, :], start=True, stop=first), r=["TM0", "B2"], w=[k2])
                            if not first:
                                sprev = carry if nl == 0 else Sb[0:Dk, nl - 1, :]
                                rk_ = ["Sbf"] if nl == 0 else SBKp + ["F1"]
                                TE(lambda e, p2=p2, j=j, cs=cs, sprev=sprev: e.matmul(p2[0:64, j * 64:(j + 1) * 64], lhsT=sprev, rhs=qi[0:Dk, cs], start=False, stop=True), r=rk_ + [qikey], w=[k2])
                        t0 = (ps_i * 32 + g * 8) * 64
                        A(lambda e, p2=p2, t0=t0: e.activation(out=odst[:, t0:t0 + 512], in_=p2[0:64, :], func=AF.Copy), r=[k2], w=[okey])
                    return
                for g in range(2):
                    pbs = [nb(), nb()]
                    pI, kI = nb()
                    for j in range(16):
                        nl = g * 16 + j
                        n = ps_i * 32 + nl
                        pp, c64, cs = blk(n)
                        p2, k2 = pbs[j % 2]
                        jj = j // 2
                        TE(lambda e, p2=p2, jj=jj, pp=pp, c64=c64: e.matmul(p2[0:64, jj * 32:(jj + 1) * 32], lhsT=vtm[pp, c64, :], rhs=PTv[pp, c64, :], start=True, stop=True), r=["TM0", "B2"], w=[k2])
                        sprev = carry if nl == 0 else Sb[0:Dk, nl - 1, :]
                        rk_ = ["Sbf"] if nl == 0 else SBKp + ["F1"]
                        TE(lambda e, pI=pI, j=j, cs=cs, sprev=sprev: e.matmul(pI[0:64, j * 32:(j + 1) * 32], lhsT=sprev, rhs=qi[0:Dk, cs], start=True, stop=True), r=rk_ + [qikey], w=[kI])
                    t0 = (ps_i * 32 + g * 16) * 32
                    A(lambda e, pI=pI, t0=t0: e.activation(out=odst[:, t0:t0 + 512], in_=pI[0:64, :], func=AF.Copy), r=[kI], w=[okey])
                    ov = odst[:, t0:t0 + 512].rearrange("p (m two c) -> p two m c", two=2, c=32)
                    for par in range(2):
                        p2, k2 = pbs[par]
                        V(lambda e, p2=p2, ov=ov, par=par: e.tensor_tensor(out=ov[:, par, :, :], in0=ov[:, par, :, :], in1=p2[0:64, 0:256].rearrange("p (m c) -> p m c", c=32), op=ALU.add), r=[k2, okey], w=[okey])

            import os
            cut = int(os.environ.get("LL_CUT", "99"))
            phase_kv(0)
            if cut == 1:
                return
            PTv = phase_scores()
            if cut == 2:
                return
            Sb0 = phase_scan(0)
            if cut == 3:
                return
            if npass == 1:
                phase_out(0, PTv, Sb0, None)
                return
            SBK0 = [("Sb", 0, v) for v in range(64)]
            V(lambda e: e.tensor_copy(out=S32[0:Dk, :], in_=Sb0[0:Dk, 31, :]), r=SBK0 + ["F1"], w=["S32"])
            V(lambda e: e.tensor_copy(out=Sbf[0:Dk, :], in_=Sb0[0:Dk, 31, :]), r=SBK0 + ["F1"], w=["Sbf"])
            phase_out(0, PTv, Sb0, Sbz[0:Dk, :])
            if cut == 4:
                return
            phase_kv(1)
            Sb1 = phase_scan(1)
            phase_out(1, PTv, Sb1, Sbf[0:Dk, :])

        def rope_tables(s):
            pos_i = F[2].bitcast(I32)
            DS(lambda e: e.dma_start(out=pos_i, in_=dr["positions"][s:s + 1, :].broadcast_to([64, T])), w=["F2"])
            V(lambda e: e.tensor_copy(out=F[0], in_=pos_i), r=["F2"], w=["F0"])
            V(lambda e: e.tensor_scalar(out=F[0], in0=F[0], scalar1=cst["freq"][:, 0:1], scalar2=None, op0=ALU.mult), r=["F0", ("c", "freq")], w=["F0"])
            MAGIC = 12582912.0
            C1 = 6.28125
            C2 = 2 * math.pi - 6.28125
            for which, dstt in ((0, sinS), (1, cosT)):
                shift = 0.0 if which == 0 else math.pi / 2
                V(lambda e, shift=shift: e.tensor_scalar(out=F[1], in0=F[0], scalar1=shift, scalar2=None, op0=ALU.add), r=["F0"], w=["F1"])
                V(lambda e: e.tensor_scalar(out=F[2], in0=F[1], scalar1=1.0 / (2 * math.pi), scalar2=MAGIC, op0=ALU.mult, op1=ALU.add), r=["F1"], w=["F2"])
                V(lambda e: e.tensor_scalar(out=F[2], in0=F[2], scalar1=MAGIC, scalar2=None, op0=ALU.subtract), r=["F2"], w=["F2"])
                V(lambda e: e.scalar_tensor_tensor(out=F[1], in0=F[2], scalar=-C1, in1=F[1], op0=ALU.mult, op1=ALU.add), r=["F1", "F2"], w=["F1"])
                V(lambda e: e.scalar_tensor_tensor(out=F[1], in0=F[2], scalar=-C2, in1=F[1], op0=ALU.mult, op1=ALU.add), r=["F1", "F2"], w=["F1"])
                V(lambda e: e.tensor_scalar(out=F[1], in0=F[1], scalar1=-math.pi, scalar2=math.pi, op0=ALU.max, op1=ALU.min), r=["F1"], w=["F1"])
                if which == 0:
                    A(lambda e: e.activation(out=F[2], in_=F[1], func=AF.Sin), r=["F1"], w=["F2"])
                    V(lambda e: e.tensor_scalar(out=sinS, in0=F[2], scalar1=cst["sgn"][:, 0:1], scalar2=None, op0=ALU.mult), r=["F2", ("c", "sgn")], w=["ROPE"])
                else:
                    A(lambda e: e.activation(out=cosT, in_=F[1], func=AF.Sin), r=["F1"], w=["ROPE"])

        def retention(s, l):
            cut = 9
            rope_tables(s)
            if cut == 0:
                return
            wq, kq = w_cols("w_in", l, RQ, 256)
            wk, kk = w_cols("w_in", l, RK, 256)
            wv, kv = w_cols("w_in", l, RV, 256)
            wg, kg = w_cols("w_in", l, RG, 256)
            tmpb = reg_bf("TM1")
            for h in range(4):
                QB, KB, VB, GB, QI = Bf
                for tt in range(4):
                    ts = slice(tt * 512, (tt + 1) * 512)
                    for (w_, wk_, dstb, dk_) in ((wq, kq, QB, "B0"), (wk, kk, KB, "B1")):
                        pb, pk = proj64(w_, wk_, h * 64, 64, tt)
                        A(lambda e, pb=pb: e.activation(out=tmpb[:, 0:512], in_=pb[0:64, :], func=AF.Copy), r=[pk], w=["TM1"])
                        p2, k2 = nb()
                        TE(lambda e, p2=p2: e.matmul(p2[0:64, :], lhsT=permb[:], rhs=tmpb[:, 0:512], start=True, stop=True), r=["TM1", "permb"], w=[k2])
                        A(lambda e, pb=pb, ts=ts: e.activation(out=F[0][:, ts], in_=pb[0:64, :], func=AF.Copy), r=[pk], w=["F0"])
                        A(lambda e, p2=p2, ts=ts: e.activation(out=F[1][:, ts], in_=p2[0:64, :], func=AF.Copy), r=[k2], w=["F1"])
                        V(lambda e, ts=ts: e.tensor_tensor(out=F[0][:, ts], in0=F[0][:, ts], in1=cosT[:, ts], op=ALU.mult), r=["F0", "ROPE"], w=["F0"])
                        V(lambda e, ts=ts: e.tensor_tensor(out=F[1][:, ts], in0=F[1][:, ts], in1=sinS[:, ts], op=ALU.mult), r=["F1", "ROPE"], w=["F1"])
                        V(lambda e, ts=ts, dstb=dstb: e.tensor_tensor(out=dstb[:, ts], in0=F[0][:, ts], in1=F[1][:, ts], op=ALU.add), r=["F0", "F1"], w=[dk_])
                    pb, pk = proj64(wv, kv, h * 64, 64, tt)
                    A(lambda e, pb=pb, ts=ts: e.activation(out=VB[:, ts], in_=pb[0:64, :], func=AF.Copy), r=[pk], w=["B2"])
                    pb, pk = proj64(wg, kg, h * 64, 64, tt)
                    A(lambda e, pb=pb, ts=ts: e.activation(out=GB[:, ts], in_=pb[0:64, :], func=AF.Silu), r=[pk], w=["B3"])
                if cut == 1:
                    continue
                V(lambda e, h=h: e.tensor_tensor(out=QI.rearrange("p (n c) -> p n c", c=64), in0=QB.rearrange("p (n c) -> p n c", c=64),
                                                 in1=cst["gq"][:, h, :].unsqueeze(1).to_broadcast([64, 32, 64]), op=ALU.mult), r=["B0", ("c", "gq")], w=["B4"])
                vtm = to_tm(VB, "B2", TM[0], "TM0", 64, 64)
                ktm = to_tm(KB, "B1", TM[1], "TM1", 64, 64, scale_ap=cst["gk"][:, h:h + 1])
                if cut == 2:
                    continue
                dec = C["retdec"][h]
                V(lambda e: e.memset(decs_t[:, 0:32], dec), w=["dec"])
                lin_loop(64, 64, KB, "B1", QB, "B0", QI, "B4", vtm, ktm, cst["maskr"][:, h, :], ("c", "maskr"), decs_t[0:64, 0:32], F[2], "F2")
                headnorm_gate(F[2], "F2", GB, "B3", hw[:, 15:16], h)
            out_proj(l, 0)

        def diag_gated(s, l, mixer):
            Dk = 32 if mixer == 2 else 64
            if mixer == 2:
                wq, kq = w_cols("w_in", l, CQ, 128)
                wk, kk = w_cols("w_in", l, CK, 128)
                wv, kv = w_cols("w_in", l, CV, 256)
                wg, kg = w_cols("w_in", l, CG, 256)
                wl, kl = w_small(l, CLR, 16)
            else:
                wq, kq = w_cols("w_in", l, DQ, 256)
                wf, kf = w_cols("w_in", l, DF_, 256)
                wv, kv = w_cols("w_in", l, DI, 256)
                wg, kg = w_cols("w_in", l, DG, 256)
            gsl = -1.0 / 16 if mixer == 2 else -1.0
            qsc = Dk ** -0.5 if mixer == 2 else 1.0
            for h in range(4):
                QS, KS, KT, VB, GB = Bf
                L_, E_, K_ = F
                for tt in range(4):
                    ts = slice(tt * 512, (tt + 1) * 512)
                    if mixer == 2:
                        pb, pk = proj64(wl, kl, 0, 16, tt)
                        A(lambda e, pb=pb: e.activation(out=rsb[0:16, :], in_=pb[0:16, :], func=AF.Copy), r=[pk], w=["rsb"])
                        pb, pk = nb()
                        TE(lambda e, pb=pb, h=h: e.matmul(pb[0:32, :], lhsT=glup[:, l, h * 32:(h + 1) * 32], rhs=rsb[0:16, :], start=True, stop=True), r=["rsb", "glup"], w=[pk])
                        A(lambda e, pb=pb, ts=ts, h=h: e.activation(out=L_[0:32, ts], in_=pb[0:32, :], func=AF.Exp, scale=-1.0, bias=glab[:, l * 4 + h:l * 4 + h + 1]), r=[pk, "glab"], w=["F0"])
                        A(lambda e, ts=ts: e.activation(out=L_[0:32, ts], in_=L_[0:32, ts], func=AF.Ln, bias=1.0), r=["F0"], w=["F0"])
                        pb, pk = proj64(wk, kk, h * 32, 32, tt)
                        A(lambda e, pb=pb, ts=ts: e.activation(out=K_[0:32, ts], in_=pb[0:32, :], func=AF.Copy), r=[pk], w=["F2"])
                    else:
                        lbc = hw[:, 6 + h:7 + h] if l == 1 else hw[:, 14:15]
                        omc = hw[:, 10 + h:11 + h] if l == 1 else hw[:, 15:16]
                        pb, pk = proj64(wf, kf, h * 64, 64, tt)
                        A(lambda e, pb=pb, ts=ts: e.activation(out=E_[:, ts], in_=pb[0:64, :], func=AF.Sigmoid), r=[pk], w=["F1"])
                        V(lambda e, ts=ts, lbc=lbc, omc=omc: e.tensor_scalar(out=L_[:, ts], in0=E_[:, ts], scalar1=omc, scalar2=lbc, op0=ALU.mult, op1=ALU.add), r=["F1", "hw"], w=["F0"])
                        A(lambda e, ts=ts: e.activation(out=L_[:, ts], in_=L_[:, ts], func=AF.Ln), r=["F0"], w=["F0"])
                        V(lambda e, ts=ts: e.tensor_scalar(out=L_[:, ts], in0=L_[:, ts], scalar1=-1.0, scalar2=None, op0=ALU.mult), r=["F0"], w=["F0"])
                        V(lambda e, ts=ts: e.tensor_scalar(out=K_[:, ts], in0=E_[:, ts], scalar1=-1.0, scalar2=1.0, op0=ALU.mult, op1=ALU.add), r=["F1"], w=["F2"])
                        V(lambda e, ts=ts, omc=omc: e.tensor_scalar(out=K_[:, ts], in0=K_[:, ts], scalar1=omc, scalar2=None, op0=ALU.mult), r=["F2", "hw"], w=["F2"])
                    pb, pk = proj64(wv, kv, h * 64, 64, tt)
                    A(lambda e, pb=pb, ts=ts: e.activation(out=VB[:, ts], in_=pb[0:64, :], func=AF.Copy), r=[pk], w=["B3"])
                    pb, pk = proj64(wg, kg, h * 64, 64, tt)
                    A(lambda e, pb=pb, ts=ts: e.activation(out=GB[:, ts], in_=pb[0:64, :], func=AF.Silu), r=[pk], w=["B4"])
                V(lambda e: e.tensor_tensor_scan(out=E_[0:Dk, :], data0=onecol[0:Dk, 0:1].to_broadcast([Dk, T]), data1=L_[0:Dk, :], initial=0.0, op0=ALU.mult, op1=ALU.add), r=["F0", "onecol"], w=["F1"])
                Ev = E_[0:Dk, :].rearrange("p (m c) -> p m c", c=32)
                Lv = L_[0:Dk, :].rearrange("p (m c) -> p m c", c=32)
                V(lambda e: e.memset(decs_t[0:Dk, 0:1], 0.0), w=["dec"])
                V(lambda e: e.tensor_copy(out=decs_t[0:Dk, 1:64], in_=Ev[:, 0:63, 31]), r=["F1"], w=["dec"])
                V(lambda e: e.tensor_tensor(out=Ev, in0=Ev, in1=decs_t[0:Dk, 0:64].unsqueeze(2).to_broadcast([Dk, 64, 32]), op=ALU.subtract), r=["F1", "dec"], w=["F1"])
                A(lambda e: e.activation(out=L_[0:Dk, :], in_=E_[0:Dk, :], func=AF.Exp, scale=-gsl), r=["F1"], w=["F0"])
                V(lambda e: e.tensor_tensor(out=KS[0:Dk, :], in0=K_[0:Dk, :], in1=L_[0:Dk, :], op=ALU.mult), r=["F0", "F2"], w=["B1"])
                V(lambda e: e.tensor_tensor(out=Lv, in0=Ev, in1=Ev[:, :, 31:32].to_broadcast([Dk, 64, 32]), op=ALU.subtract), r=["F1", "B1"], w=["F0"])
                A(lambda e: e.activation(out=L_[0:Dk, :], in_=L_[0:Dk, :], func=AF.Exp, scale=-gsl), r=["F0"], w=["F0"])
                V(lambda e: e.tensor_tensor(out=KT[0:Dk, :], in0=K_[0:Dk, :], in1=L_[0:Dk, :], op=ALU.mult), r=["F0", "F2"], w=["B2"])
                A(lambda e: e.activation(out=decs_t[0:Dk, 0:64], in_=Ev[:, :, 31], func=AF.Exp, scale=gsl), r=["F1"], w=["dec"])
                A(lambda e: e.activation(out=L_[0:Dk, :], in_=E_[0:Dk, :], func=AF.Exp, scale=gsl), r=["F1", "B2"], w=["F0"])
                for tt in range(4):
                    ts = slice(tt * 512, (tt + 1) * 512)
                    pb, pk = proj64(wq, kq, h * Dk, Dk, tt)
                    V(lambda e, pb=pb, ts=ts: e.scalar_tensor_tensor(out=QS[0:Dk, ts], in0=pb[0:Dk, :], scalar=qsc, in1=L_[0:Dk, ts], op0=ALU.mult, op1=ALU.mult), r=[pk, "F0"], w=["B0"])
                vtm = to_tm(VB, "B3", TM[0], "TM0", 64, 64)
                ktm = to_tm(KT, "B2", TM[1], "TM1", 64, Dk)
                lin_loop(32, Dk, KS, "B1", QS, "B0", QS, "B0", vtm, ktm, cst["caus32"][:], ("c", "caus32"), decs_t[0:Dk, 0:64], F[2], "F2")
                nwc = hw[:, 2 + l:3 + l] if mixer == 2 else hw[:, 4 + l:5 + l]
                headnorm_gate(F[2], "F2", GB, "B4", nwc, h)
            out_proj(l, 512 if mixer == 2 else 768)

        def gdn(s, l):
            wq, kq = w_cols("w_in", l, BQ, 256)
            wk, kk = w_cols("w_in", l, BK, 256)
            wv, kv = w_cols("w_in", l, BV, 256)
            wg, kg = w_cols("w_in", l, BG, 256)
            wab, kab = w_small(l, BA, 8)
            pb, pk = nb()
            for n in range(32):
                for k in range(8):
                    TE(lambda e, pb=pb, n=n, k=k: e.matmul(pb[0:64, 8 * n:8 * n + 8], lhsT=xnT[:, k, n * 64:(n + 1) * 64], rhs=wab[:, k, :], start=(k == 0), stop=(k == 7)),
                       r=[kab, ("xnT", n // 8)], w=[pk])
            A(lambda e, pb=pb: e.activation(out=ab_all[:], in_=pb[0:64, 0:256], func=AF.Copy), r=[pk], w=["ab_all"])
            X1 = ROPE[0:64, 0:1024].bitcast(F32).rearrange("p (i c) -> p i c", c=64)
            X2 = ROPE[0:64, 1024:2048].bitcast(F32).rearrange("p (i c) -> p i c", c=64)
            Rr = ROPE[0:64, 2048:4096].bitcast(F32).rearrange("p (i c) -> p i c", c=128)
            X3 = ROPE[0:64, 2048:3072].bitcast(F32).rearrange("p (i c) -> p i c", c=64)
            for h in range(4):
                QB, KB, VB, GB, QG = Bf
                for ti, (w_, wk_, dstb, dk_) in enumerate(((wq, kq, QB, "B0"), (wk, kk, KB, "B1"), (wv, kv, VB, "B2"))):
                    for tt in range(4):
                        ts = slice(tt * 512, (tt + 1) * 512)
                        pb, pk = proj64(w_, wk_, h * 64, 64, tt)
                        A(lambda e, pb=pb, ts=ts: e.activation(out=F[0][:, ts], in_=pb[0:64, :], func=AF.Copy), r=[pk], w=["F0"])
                    cj = ti * 4 + h
                    V(lambda e, cj=cj: e.tensor_scalar(out=F[1], in0=F[0], scalar1=gcw[:, l, 3, cj:cj + 1], scalar2=None, op0=ALU.mult), r=["F0", "gcw"], w=["F1"])
                    for sh in (1, 2, 3):
                        V(lambda e, cj=cj, sh=sh: e.scalar_tensor_tensor(out=F[1][:, sh:T], in0=F[0][:, 0:T - sh], scalar=gcw[:, l, 3 - sh, cj:cj + 1], in1=F[1][:, sh:T], op0=ALU.mult, op1=ALU.add), r=["F0", "F1", "gcw"], w=["F1"])
                    if ti == 2:
                        A(lambda e: e.activation(out=VB, in_=F[1], func=AF.Silu), r=["F1"], w=["B2"])
                    else:
                        A(lambda e: e.activation(out=F[1], in_=F[1], func=AF.Silu), r=["F1"], w=["F1"])
                        tmp = reg_bf("TM0")
                        A(lambda e: e.activation(out=tmp, in_=F[1], func=AF.Square), r=["F1"], w=["TM0"])
                        for tt in range(4):
                            ts = slice(tt * 512, (tt + 1) * 512)
                            pb, pk = nb()
                            TE(lambda e, pb=pb, ts=ts: e.matmul(pb[0:64, :], lhsT=onesb[0:64, 0:64], rhs=tmp[:, ts], start=True, stop=True), r=["TM0", "onesb"], w=[pk])
                            A(lambda e, pb=pb: e.activation(out=rsb[0:64, :], in_=pb[0:64, :], func=AF.Ln, scale=1.0, bias=EPS), r=[pk], w=["rsb"])
                            A(lambda e: e.activation(out=rsb[0:64, :], in_=rsb[0:64, :], func=AF.Exp, scale=-0.5), r=["rsb"], w=["rsb"])
                            sc = 0.125 if ti == 0 else 1.0
                            V(lambda e, ts=ts, sc=sc, dstb=dstb: e.scalar_tensor_tensor(out=dstb[:, ts], in0=F[1][:, ts], scalar=sc, in1=rsb[0:64, :], op0=ALU.mult, op1=ALU.mult), r=["F1", "rsb"], w=[dk_])
                for tt in range(4):
                    ts = slice(tt * 512, (tt + 1) * 512)
                    pb, pk = proj64(wg, kg, h * 64, 64, tt)
                    A(lambda e, pb=pb, ts=ts: e.activation(out=GB[:, ts], in_=pb[0:64, :], func=AF.Silu), r=[pk], w=["B3"])
                abv = ab_all[:].rearrange("p (n j h) -> p j h n", j=2, h=4)
                A(lambda e: e.activation(out=small[:, 0:2, :], in_=abv[:, :, h, :], func=AF.Copy), r=["ab_all"], w=["small"])
                gi = l * 4 + h
                A(lambda e: e.activation(out=small[:, 1, :], in_=small[:, 1, :], func=AF.Sigmoid), r=["small"], w=["small"])
                A(lambda e: e.activation(out=small[:, 2, :], in_=small[:, 0, :], func=AF.Exp, bias=gsc[:, 8 + gi:9 + gi]), r=["small", "gsc"], w=["small"])
                A(lambda e: e.activation(out=small[:, 2, :], in_=small[:, 2, :], func=AF.Ln, bias=1.0), r=["small"], w=["small"])
                V(lambda e: e.tensor_scalar(out=small[:, 2, :], in0=small[:, 2, :], scalar1=gsc[:, gi:gi + 1], scalar2=None, op0=ALU.mult), r=["small", "gsc"], w=["small"])
                V(lambda e: e.tensor_scalar(out=small[:, 0, :], in0=small[:, 1, :], scalar1=-1.0, scalar2=None, op0=ALU.mult), r=["small"], w=["small"])
                pb, pk = nb()
                TE(lambda e, pb=pb: e.matmul(pb[0:64, 0:32], lhsT=cst["tri"][:], rhs=small[:, 2, :], start=True, stop=True), r=["small", ("c", "tri")], w=[pk])
                TE(lambda e, pb=pb: e.matmul(pb[0:64, 32:64], lhsT=ones64f[:], rhs=small[:, 2, :], start=True, stop=True), r=["small", "ones64f"], w=[pk])
                A(lambda e, pb=pb: e.activation(out=small[:, 3:5, :], in_=pb[0:64, 0:64].rearrange("p (j n) -> p j n", j=2), func=AF.Copy), r=[pk], w=["small"])
                A(lambda e: e.activation(out=small[:, 5, :], in_=small[:, 3, :], func=AF.Exp), r=["small"], w=["small"])
                V(lambda e: e.tensor_tensor(out=small[:, 6, :], in0=small[:, 1, :], in1=small[:, 5, :], op=ALU.mult), r=["small"], w=["small"])
                V(lambda e: e.tensor_tensor(out=small[:, 7, :], in0=small[:, 4, :], in1=small[:, 3, :], op=ALU.subtract), r=["small"], w=["small"])
                A(lambda e: e.activation(out=small[:, 7, :], in_=small[:, 7, :], func=AF.Exp), r=["small"], w=["small"])
                A(lambda e: e.activation(out=decs_t[:, 0:32], in_=small[:, 4, :], func=AF.Exp), r=["small"], w=["dec"])
                if h == 0:
                    tap(0, QB, ['B0']); tap(1, KB, ['B1']); tap(2, VB, ['B2'])
                    tap(0, small[:].rearrange('p a b -> p (a b)'), ['small'], n=256)
                ktm = to_tm(KB, "B1", TM[0], "TM0", 64, 64)
                vtm = to_tm(VB, "B2", TM[1], "TM1", 64, 64)
                qtm_t = reg_bf("F0")[:, 0:2048]
                qtm = to_tm(QB, "B0", qtm_t, "F0", 64, 64)
                V(lambda e: e.tensor_tensor(out=qtm, in0=qtm, in1=small[:, 5, :].unsqueeze(2).to_broadcast([64, 32, 64]), op=ALU.mult), r=["F0", "small"], w=["F0"])
                for g in range(8):
                    pb, pk = nb()
                    pbb = pb[:].bitcast(BF16)
                    for j in range(4):
                        n = g * 4 + j
                        TE(lambda e, pbb=pbb, j=j, n=n: e.transpose(out=pbb[0:64, j * 64:(j + 1) * 64], in_=qtm[:, n, :], identity=identb[0:64, 0:64]), r=["F0", "identb"], w=[pk])
                    A(lambda e, pbb=pbb, g=g: e.activation(out=QG[:, g * 256:(g + 1) * 256], in_=pbb[0:64, 0:256], func=AF.Copy), r=[pk], w=["B4"])
                Uv = F[1].rearrange("p (n v) -> p n v", v=64)
                WT = VB.rearrange("p (n c) -> p n c", c=64)
                QKT = reg_bf("F0")[:, 2048:4096].rearrange("p (n c) -> p n c", c=64)
                for G in range(4):
                    n0 = G * 8
                    V(lambda e, n0=n0: e.tensor_tensor(out=X1, in0=small[:, 2, n0:n0 + 8].unsqueeze(2).to_broadcast([64, 8, 64]), in1=cst["masku"][:].unsqueeze(1).to_broadcast([64, 8, 64]), op=ALU.mult),
                      r=["small", ("c", "masku")], w=["X1"])
                    X1f = ROPE[0:64, 0:1024].bitcast(F32)
                    pdt, kdt = nb()
                    TE(lambda e, pdt=pdt: e.matmul(pdt[0:64, :], lhsT=cst["tri"][:], rhs=X1f, start=True, stop=False), r=["X1", ("c", "tri")], w=[kdt])
                    for i in range(8):
                        TE(lambda e, pdt=pdt, i=i: e.matmul(pdt[0:64, i * 64:(i + 1) * 64], lhsT=ident64, rhs=cst["negs"][:], start=False, stop=(i == 7)), r=[("c", "negs"), "ident"], w=[kdt])
                    A(lambda e, pdt=pdt: e.activation(out=X2, in_=pdt[0:64, :].rearrange("p (i c) -> p i c", c=64), func=AF.Exp), r=[kdt], w=["X2"])
                    pd, kd = nb()
                    for i in range(8):
                        TE(lambda e, pd=pd, i=i: e.matmul(pd[0:64, i * 64:(i + 1) * 64], lhsT=X1[:, i, :], rhs=cst["tri"][:], start=True, stop=False), r=["X1", ("c", "tri")], w=[kd])
                        TE(lambda e, pd=pd, i=i: e.matmul(pd[0:64, i * 64:(i + 1) * 64], lhsT=ident64, rhs=cst["negi"][:], start=False, stop=True), r=[("c", "negi"), "ident"], w=[kd])
                    A(lambda e, pd=pd: e.activation(out=X3, in_=pd[0:64, :].rearrange("p (i c) -> p i c", c=64), func=AF.Exp), r=[kd], w=["RR"])
                    pkk, kkk = nb()
                    pqk, kqk = nb()
                    for i in range(8):
                        cs = slice((n0 + i) * 64, (n0 + i + 1) * 64)
                        TE(lambda e, pkk=pkk, i=i, cs=cs: e.matmul(pkk[0:64, i * 64:(i + 1) * 64], lhsT=KB[:, cs], rhs=KB[:, cs], start=True, stop=True), r=["B1"], w=[kkk])
                        TE(lambda e, pqk=pqk, i=i, cs=cs: e.matmul(pqk[0:64, i * 64:(i + 1) * 64], lhsT=KB[:, cs], rhs=QB[:, cs], start=True, stop=True), r=["B1", "B0"], w=[kqk])
                    V(lambda e, pkk=pkk: e.tensor_tensor(out=X2, in0=pkk[0:64, :].rearrange("p (i c) -> p i c", c=64), in1=X2, op=ALU.mult), r=[kkk, "X2"], w=["X2"])
                    V(lambda e, n0=n0: e.tensor_tensor(out=X2, in0=X2, in1=small[:, 0, n0:n0 + 8].unsqueeze(2).to_broadcast([64, 8, 64]), op=ALU.mult), r=["X2", "small"], w=["X2"])
                    V(lambda e, pqk=pqk, n0=n0: e.tensor_tensor(out=QKT[:, n0:n0 + 8, :], in0=pqk[0:64, :].rearrange("p (i c) -> p i c", c=64), in1=X3, op=ALU.mult), r=[kqk, "RR"], w=["F0"])
                    pq, kq_ = nb()
                    for i in range(8):
                        TE(lambda e, pq=pq, i=i: e.transpose(out=pq[0:64, i * 64:(i + 1) * 64], in_=X2[:, i, :], identity=ident64), r=["X2", "ident"], w=[kq_])
                    A(lambda e, pq=pq: e.activation(out=X1, in_=pq[0:64, :].rearrange("p (i c) -> p i c", c=64), func=AF.Copy), r=[kq_], w=["X1"])
                    V(lambda e, n0=n0: e.tensor_tensor(out=Rr[:, :, 0:64], in0=vtm[:, n0:n0 + 8, :], in1=small[:, 1, n0:n0 + 8].unsqueeze(2).to_broadcast([64, 8, 64]), op=ALU.mult), r=["TM1", "small"], w=["RR"])
                    V(lambda e, n0=n0: e.tensor_tensor(out=Rr[:, :, 64:128], in0=ktm[:, n0:n0 + 8, :], in1=small[:, 6, n0:n0 + 8].unsqueeze(2).to_broadcast([64, 8, 64]), op=ALU.mult), r=["TM0", "small"], w=["RR"])
                    for lev in range(6):
                        pa, ka = nb()
                        pa2, ka2 = nb()
                        for i in range(8):
                            pp = pa if i < 4 else pa2
                            TE(lambda e, pp=pp, i=i: e.matmul(pp[0:64, (i % 4) * 128:(i % 4 + 1) * 128], lhsT=X1[:, i, :], rhs=Rr[:, i, :], start=True, stop=True), r=["X1", "RR"], w=[ka if i < 4 else ka2])
                        V(lambda e, pa=pa: e.tensor_tensor(out=Rr[:, 0:4, :], in0=Rr[:, 0:4, :], in1=pa[0:64, :].rearrange("p (i c) -> p i c", c=128), op=ALU.add), r=[ka, "RR"], w=["RR"])
                        V(lambda e, pa2=pa2: e.tensor_tensor(out=Rr[:, 4:8, :], in0=Rr[:, 4:8, :], in1=pa2[0:64, :].rearrange("p (i c) -> p i c", c=128), op=ALU.add), r=[ka2, "RR"], w=["RR"])
                        if lev < 5:
                            pp_, kp_ = nb()
                            pq_, kq2 = nb()
                            for i in range(8):
                                TE(lambda e, pp_=pp_, i=i: e.matmul(pp_[0:64, i * 64:(i + 1) * 64], lhsT=X1[:, i, :], rhs=X2[:, i, :], start=True, stop=True), r=["X1", "X2"], w=[kp_])
                                TE(lambda e, pq_=pq_, i=i: e.matmul(pq_[0:64, i * 64:(i + 1) * 64], lhsT=X2[:, i, :], rhs=X1[:, i, :], start=True, stop=True), r=["X1", "X2"], w=[kq2])
                            A(lambda e, pp_=pp_: e.activation(out=X2, in_=pp_[0:64, :].rearrange("p (i c) -> p i c", c=64), func=AF.Copy), r=[kp_], w=["X2"])
                            V(lambda e, pq_=pq_: e.tensor_copy(out=X1, in_=pq_[0:64, :].rearrange("p (i c) -> p i c", c=64)), r=[kq2], w=["X1"])
                    A(lambda e, n0=n0: e.activation(out=Uv[:, n0:n0 + 8, :], in_=Rr[:, :, 0:64], func=AF.Copy), r=["RR"], w=["F1"])
                    pw, kw = nb()
                    for i in range(8):
                        TE(lambda e, pw=pw, i=i: e.transpose(out=pw[0:64, i * 64:(i + 1) * 64], in_=Rr[:, i, 64:128], identity=ident64), r=["RR", "ident"], w=[kw])
                    A(lambda e, pw=pw, n0=n0: e.activation(out=WT[:, n0:n0 + 8, :], in_=pw[0:64, :].rearrange("p (i c) -> p i c", c=64), func=AF.Copy), r=[kw], w=["B2"])
                if h == 0:
                    tap(1, F[1], ['F1']); tap(3, VB, ['B2']); tap(4, reg_bf('F0')[:, 2048:4096], ['F0']); tap(5, QG, ['B4'])
                    tap(6, TM[0], ['TM0']); tap(7, TM[1], ['TM1'])
                V(lambda e: e.tensor_tensor(out=ktm, in0=ktm, in1=small[:, 7, :].unsqueeze(2).to_broadcast([64, 32, 64]), op=ALU.mult), r=["TM0", "small"], w=["TM0"])
                state_init()
                for n in range(32):
                    cs = slice(n * 64, (n + 1) * 64)
                    p1, k1 = nb()
                    TE(lambda e, p1=p1, n=n: e.matmul(p1[0:64, 0:64], lhsT=WT[:, n, :], rhs=Sbf[:], start=True, stop=True), r=["B2", "Sbf"], w=[k1])
                    ub = PTb[n % 2]
                    V(lambda e, p1=p1, n=n, ub=ub: e.tensor_tensor(out=ub[:], in0=Uv[:, n, :], in1=p1[0:64, 0:64], op=ALU.subtract), r=[k1, "F1"], w=[("PTb", n % 2)])
                    p2, k2 = nb()
                    TE(lambda e, p2=p2, ub=ub, n=n: e.matmul(p2[0:64, 0:64], lhsT=ub[:], rhs=QKT[:, n, :], start=True, stop=False), r=[("PTb", n % 2), "F0"], w=[k2])
                    TE(lambda e, p2=p2, cs=cs: e.matmul(p2[0:64, 0:64], lhsT=Sbf[:], rhs=QG[:, cs], start=False, stop=True), r=["Sbf", "B4"], w=[k2])
                    A(lambda e, p2=p2, cs=cs: e.activation(out=F[2][:, cs], in_=p2[0:64, 0:64], func=AF.Copy), r=[k2], w=["F2"])
                    p3, k3 = nb()
                    TE(lambda e, p3=p3, n=n, ub=ub: e.matmul(p3[0:64, 0:64], lhsT=ktm[:, n, :], rhs=ub[:], start=True, stop=True), r=["TM0", ("PTb", n % 2)], w=[k3])
                    state_update(64, p3, k3, decs_t[:, n:n + 1])
                if h == 0:
                    tap(2, F[2], ['F2'])
                headnorm_gate(F[2], "F2", GB, "B3", hw[:, 0 + l:1 + l], h, tmpk="TM1")
            out_proj(l, 256)

        def xattn(s, l):
            QT = SCR[:, 0:16384].rearrange("p (k t) -> p k t", t=T)
            AO = SCR[:, 16384:32768].rearrange("p (k t) -> p k t", t=T)
            PR = SCR[:, 32768:33792].rearrange("p (m t) -> p m t", t=512)
            KTm = ROPE[:, 0:2048].rearrange("p (k m) -> p k m", m=256)
            Vtm = ROPE[:, 2048:4096].rearrange("p (c d) -> p c d", d=D)
            QTK = rk(0, 16384)
            AOK = rk(16384, 16384)
            PRK = rk(32768, 1024)
            specs = [(nm, l, q4 * 256, 256) for nm in ("xattn_wk", "xattn_wv", "xattn_wq", "xattn_wo") for q4 in range(4)]
            loaded = {}
            nxt = [0]

            def getw(i):
                while nxt[0] < len(specs) and nxt[0] <= i + 3:
                    loaded[nxt[0]] = w_cols(*specs[nxt[0]])
                    nxt[0] += 1
                return loaded[i]

            for q4 in range(4):
                wk_, kk_ = getw(q4)
                for j in range(2):
                    mtile = q4 * 2 + j
                    pb, pk = nb()
                    for k in range(8):
                        TE(lambda e, k=k, pb=pb, j=j, wk_=wk_: e.matmul(pb[:, 0:256], lhsT=wk_[:, k, j * 128:(j + 1) * 128], rhs=memT[:, k, :], start=(k == 0), stop=(k == 7)), r=[kk_, "memT"], w=[pk])
                    A(lambda e, pb=pb, mtile=mtile: e.activation(out=KTm[:, mtile, :], in_=pb[:, 0:256], func=AF.Copy), r=[pk], w=["ROPE"])
            for q4 in range(4):
                wv_, kv_ = getw(4 + q4)
                for mc in range(2):
                    pb, pk = nb()
                    for k in range(8):
                        TE(lambda e, k=k, pb=pb, mc=mc, wv_=wv_: e.matmul(pb[:, 0:256], lhsT=memT[:, k, mc * 128:(mc + 1) * 128], rhs=wv_[:, k, :], start=(k == 0), stop=(k == 7)), r=[kv_, "memT"], w=[pk])
                    A(lambda e, pb=pb, mc=mc, q4=q4: e.activation(out=Vtm[:, mc, q4 * 256:(q4 + 1) * 256], in_=pb[:, 0:256], func=AF.Copy), r=[pk], w=["ROPE"])
            for q4 in range(4):
                wq_, kq_ = getw(8 + q4)
                for j in range(2):
                    mt = q4 * 2 + j
                    for tt in range(4):
                        pb, pk = proj64(wq_, kq_, j * 128, 128, tt)
                        A(lambda e, pb=pb, mt=mt, tt=tt: e.activation(out=QT[:, mt, tt * 512:(tt + 1) * 512], in_=pb[:], func=AF.Copy), r=[pk], w=QTK)
            for tt in range(4):
                ts = slice(tt * 512, (tt + 1) * 512)
                for hh in range(4):
                    for mc in range(2):
                        pb, pk = nb()
                        for kk2 in range(2):
                            TE(lambda e, pb=pb, mc=mc, kk2=kk2, hh=hh, ts=ts: e.matmul(pb[:], lhsT=KTm[:, hh * 2 + kk2, mc * 128:(mc + 1) * 128], rhs=QT[:, hh * 2 + kk2, ts], start=(kk2 == 0), stop=(kk2 == 1)),
                               r=["ROPE"] + QTK, w=[pk])
                        A(lambda e, pb=pb, mc=mc: e.activation(out=PR[:, mc, :], in_=pb[:], func=AF.Exp, scale=1.0 / 16), r=[pk], w=PRK)
                    pden, kden = nb()
                    for mc in range(2):
                        TE(lambda e, pden=pden, mc=mc: e.matmul(pden[:], lhsT=onesb[:], rhs=PR[:, mc, :], start=(mc == 0), stop=(mc == 1)), r=PRK + ["onesb"], w=[kden])
                    V(lambda e, pden=pden: e.reciprocal(out=rsb[:], in_=pden[:]), r=[kden], w=["rsb"])
                    for dc in range(2):
                        pb, pk = nb()
                        for mc in range(2):
                            TE(lambda e, pb=pb, mc=mc, dc=dc, hh=hh: e.matmul(pb[:], lhsT=Vtm[:, mc, hh * 256 + dc * 128:hh * 256 + (dc + 1) * 128], rhs=PR[:, mc, :], start=(mc == 0), stop=(mc == 1)),
                               r=["ROPE"] + PRK, w=[pk])
                        V(lambda e, pb=pb, dc=dc, hh=hh, ts=ts: e.tensor_tensor(out=AO[:, hh * 2 + dc, ts], in0=pb[:], in1=rsb[:], op=ALU.mult), r=[pk, "rsb"], w=AOK)
            for q4 in range(4):
                wo_, ko_ = getw(12 + q4)
                for j in range(2):
                    mt = q4 * 2 + j
                    for tt in range(4):
                        ts = slice(tt * 512, (tt + 1) * 512)
                        pb, pk = nb()
                        for k in range(8):
                            TE(lambda e, pb=pb, k=k, j=j, ts=ts, wo_=wo_: e.matmul(pb[:], lhsT=wo_[:, k, j * 128:(j + 1) * 128], rhs=AO[:, k, ts], start=(k == 0), stop=(k == 7)), r=[ko_] + AOK, w=[pk])
                        V(lambda e, pb=pb, mt=mt, ts=ts: e.tensor_tensor(out=hT[:, mt, ts], in0=hT[:, mt, ts], in1=pb[:], op=ALU.add), r=[pk, "hT"], w=["hT"])

        def ffn(s, l):
            UG = SCR[:, 0:4100].bitcast(F32)
            UV = SCR[:, 4352:8452].bitcast(F32)
            CG_ = SCR[:, 8704:12800].bitcast(F32)
            CV_ = SCR[:, 12800:16896].bitcast(F32)
            ACT = SCR[:, 16896:20992].rearrange("p (j t) -> p j t", t=T)
            cw = SCR[:, 21056:21056 + 264].bitcast(F32).rearrange("p (k j) -> p k j", j=44)
            PTMP = ROPE[:, 0:4096].bitcast(F32)
            UGK, UVK, CGK, CVK, ACK, CWK = rk(0, 4100), rk(4352, 4100), rk(8704, 4096), rk(12800, 4096), rk(16896, 4096), rk(21056, 264)
            for k3 in range(3):
                DS(lambda e, k3=k3: e.dma_start(out=cw[:, k3, :], in_=dr["ffn_conv_w"][l, k3].rearrange("(j p) -> p j", p=128)), w=CWK)
            V(lambda e: e.memset(UG[:, 0:2], 0.0), w=UGK)
            V(lambda e: e.memset(UV[:, 0:2], 0.0), w=UVK)
            fslots = [(W[:, i, :], ("W", i)) for i in range(NSLOT)]
            for key in ("TM0", "TM1", ("OM", 0), ("OM", 1), ("OM", 2), ("OM", 3)):
                o_, m_ = REG[key]
                fslots.append((SCR[:, o_:o_ + m_], key))
            fctr = [0]

            def fslot():
                v = fslots[fctr[0] % len(fslots)]
                fctr[0] += 1
                return v

            loaded = {}

            def issue(g):
                res = []
                for c0 in (g * 256, DFF + g * 256):
                    buf, key = fslot()
                    dst = buf.rearrange("p (k m) -> p k m", m=256)
                    src = dr["ffn_up"][l][:, c0:c0 + 256].rearrange("(k p) m -> p k m", p=128)
                    DG_(lambda e, dst=dst, src=src: e.dma_start(out=dst, in_=src), w=[key])
                    res.append((dst, key))
                buf, key = fslot()
                dst = buf.rearrange("p (j m) -> p j m", m=1024)
                src = dr["ffn_down"][l][g * 256:(g + 1) * 256, :].rearrange("(j p) m -> p j m", p=128)
                DG_(lambda e, dst=dst, src=src: e.dma_start(out=dst, in_=src), w=[key])
                res.append((dst, key))
                loaded[g] = res

            PF = 2
            for g in range(PF):
                issue(g)
            for g in range(11):
                if g + PF < 11:
                    issue(g + PF)
                (wg_, kg_), (wv_, kv_), (wd_, kd_) = loaded[g]
                for j in range(2):
                    ch = g * 2 + j
                    for tt in range(4):
                        pb, pk = proj64(wg_, kg_, j * 128, 128, tt)
                        A(lambda e, pb=pb, tt=tt: e.activation(out=UG[:, 2 + tt * 512:2 + (tt + 1) * 512], in_=pb[:], func=AF.Copy), r=[pk], w=UGK)
                    V(lambda e, ch=ch: e.tensor_scalar(out=CG_, in0=UG[:, 0:T], scalar1=cw[:, 0, ch:ch + 1], scalar2=None, op0=ALU.mult), r=UGK + CWK, w=CGK)
                    V(lambda e, ch=ch: e.scalar_tensor_tensor(out=CG_, in0=UG[:, 1:T + 1], scalar=cw[:, 1, ch:ch + 1], in1=CG_, op0=ALU.mult, op1=ALU.add), r=UGK + CWK + CGK, w=CGK)
                    V(lambda e, ch=ch: e.scalar_tensor_tensor(out=CG_, in0=UG[:, 2:T + 2], scalar=cw[:, 2, ch:ch + 1], in1=CG_, op0=ALU.mult, op1=ALU.add), r=UGK + CWK + CGK, w=CGK)
                    A(lambda e: e.activation(out=CG_, in_=CG_, func=AF.Silu), r=CGK, w=CGK)
                    for tt in range(4):
                        pb, pk = proj64(wv_, kv_, j * 128, 128, tt)
                        A(lambda e, pb=pb, tt=tt: e.activation(out=UV[:, 2 + tt * 512:2 + (tt + 1) * 512], in_=pb[:], func=AF.Copy), r=[pk], w=UVK)
                    cv = 22 + ch
                    GP(lambda e, cv=cv: e.tensor_scalar(out=CV_, in0=UV[:, 0:T], scalar1=cw[:, 0, cv:cv + 1], scalar2=None, op0=ALU.mult), r=UVK + CWK, w=CVK)
                    for k3 in (1, 2):
                        GP(lambda e, cv=cv, k3=k3: e.tensor_scalar(out=PTMP, in0=UV[:, k3:T + k3], scalar1=cw[:, k3, cv:cv + 1], scalar2=None, op0=ALU.mult), r=UVK + CWK, w=["ROPE"])
                        GP(lambda e: e.tensor_tensor(out=CV_, in0=CV_, in1=PTMP, op=ALU.add), r=CVK + ["ROPE"], w=CVK)
                    GP(lambda e, j=j: e.tensor_tensor(out=ACT[:, j, :], in0=CG_, in1=CV_, op=ALU.mult), r=CGK + CVK, w=ACK)
                for mt in range(8):
                    for tt in range(4):
                        ts = slice(tt * 512, (tt + 1) * 512)
                        pb, pk = nb()
                        for j in range(2):
                            TE(lambda e, pb=pb, j=j, mt=mt, ts=ts, wd_=wd_: e.matmul(pb[:], lhsT=wd_[:, j, mt * 128:(mt + 1) * 128], rhs=ACT[:, j, ts], start=(j == 0), stop=(j == 1)), r=[kd_] + ACK, w=[pk])
                        V(lambda e, pb=pb, mt=mt, ts=ts: e.tensor_tensor(out=hT[:, mt, ts], in0=hT[:, mt, ts], in1=pb[:], op=ALU.add), r=[pk, "hT"], w=["hT"])

        XNK = [("xnT", i) for i in range(4)]
        xs = xnT[:].rearrange("p k t -> p (k t)").bitcast(F32)
        out_ops = []
        for s in (seqs if seqs is not None else range(NSEQ)):
            for t16 in range(16):
                xt_ = xs[:, (t16 % 4) * 1024:(t16 % 4 + 1) * 1024]
                DS(lambda e, xt_=xt_, t16=t16: e.dma_start(out=xt_, in_=dr["x"][s, t16 * 128:(t16 + 1) * 128, :]), w=XNK)
                for g in range(2):
                    pb, pk = nb()
                    for j in range(4):
                        k = g * 4 + j
                        TE(lambda e, pb=pb, j=j, k=k, xt_=xt_: e.transpose(out=pb[:, j * 128:(j + 1) * 128], in_=xt_[:, k * 128:(k + 1) * 128], identity=ident[:]), r=XNK + ["ident"], w=[pk])
                    A(lambda e, pb=pb, g=g, t16=t16: e.activation(out=hT[:, g * 4:(g + 1) * 4, t16 * 128:(t16 + 1) * 128], in_=pb[:].rearrange("p (k t) -> p k t", t=128), func=AF.Copy), r=[pk], w=["hT"])
            for mt in range(2):
                mt_ = xs[:, 4096 + mt * 1024:4096 + (mt + 1) * 1024]
                DS(lambda e, mt_=mt_, mt=mt: e.dma_start(out=mt_, in_=dr["mem"][s, mt * 128:(mt + 1) * 128, :]), w=XNK)
                A(lambda e, mt_=mt_: e.activation(out=xs[:, 6144:7168], in_=mt_, func=AF.Square, accum_out=rsb[:, 0:1]), r=XNK, w=XNK + ["rsb"])
                A(lambda e: e.activation(out=rsb[:, 1:2], in_=rsb[:, 0:1], func=AF.Sqrt, scale=1.0 / D, bias=EPS), r=["rsb"], w=["rsb"])
                V(lambda e: e.reciprocal(out=rsb[:, 2:3], in_=rsb[:, 1:2]), r=["rsb"], w=["rsb"])
                V(lambda e, mt_=mt_: e.tensor_scalar(out=mt_, in0=mt_, scalar1=rsb[:, 2:3], scalar2=None, op0=ALU.mult), r=XNK + ["rsb"], w=XNK)
                for g in range(2):
                    pb, pk = nb()
                    for j in range(4):
                        k = g * 4 + j
                        TE(lambda e, pb=pb, j=j, k=k, mt_=mt_: e.transpose(out=pb[:, j * 128:(j + 1) * 128], in_=mt_[:, k * 128:(k + 1) * 128], identity=ident[:]), r=XNK + ["ident"], w=[pk])
                    for j in range(4):
                        k = g * 4 + j
                        V(lambda e, pb=pb, j=j, k=k, mt=mt: e.tensor_scalar(out=memT[:, k, mt * 128:(mt + 1) * 128], in0=pb[:, j * 128:(j + 1) * 128], scalar1=memnw[:, k:k + 1], scalar2=None, op0=ALU.mult),
                          r=[pk, "memnw"], w=["memT"])
            for l in (layers if layers is not None else range(NLAYER)):
                if "mix" in stages:
                    norm(0 + l)
                    if 0 in mixers:
                        retention(s, l)
                    if 1 in mixers:
                        gdn(s, l)
                    if 2 in mixers:
                        diag_gated(s, l, 2)
                    if 3 in mixers:
                        diag_gated(s, l, 3)
                if "xat" in stages:
                    norm(2 + l)
                    xattn(s, l)
                if "ffn" in stages:
                    norm(4 + l)
                    ffn(s, l)
            yn = xs[:, 0:4096].rearrange("p (k t) -> p k t", t=512)
            for tt in range(4):
                ts = slice(tt * 512, (tt + 1) * 512)
                A(lambda e, ts=ts: e.activation(out=sq, in_=hT[:, :, ts], func=AF.Square), r=["hT"], w=SQK)
                pb, pk = nb()
                for k in range(8):
                    TE(lambda e, k=k, pb=pb: e.matmul(pb[:], lhsT=onesb[:], rhs=sq[:, k, :], start=(k == 0), stop=(k == 7)), r=SQK + ["onesb"], w=[pk])
                A(lambda e, pb=pb: e.activation(out=rsb[:], in_=pb[:], func=AF.Ln, scale=1.0 / D, bias=EPS), r=[pk], w=["rsb"])
                A(lambda e: e.activation(out=rsb[:], in_=rsb[:], func=AF.Exp, scale=-0.5), r=["rsb"], w=["rsb"])
                for k in range(8):
                    V(lambda e, k=k, ts=ts: e.scalar_tensor_tensor(out=yn[:, k, :], in0=hT[:, k, ts], scalar=normw[:, 6, k:k + 1], in1=rsb[:], op0=ALU.mult, op1=ALU.mult), r=["hT", "rsb", "normw"], w=XNK)
                for t4 in range(4):
                    stg = xs[:, 4096 + t4 * 1024:4096 + (t4 + 1) * 1024]
                    for g in range(2):
                        pb, pk = nb()
                        for j in range(4):
                            k = g * 4 + j
                            TE(lambda e, pb=pb, j=j, k=k, t4=t4: e.transpose(out=pb[:, j * 128:(j + 1) * 128], in_=yn[:, k, t4 * 128:(t4 + 1) * 128], identity=ident[:]), r=XNK + ["ident"], w=[pk])
                        A(lambda e, pb=pb, g=g, stg=stg: e.activation(out=stg[:, g * 512:(g + 1) * 512], in_=pb[:], func=AF.Copy), r=[pk], w=XNK)
                    t0 = tt * 512 + t4 * 128
                    out_ops.append(DS(lambda e, stg=stg, t0=t0: e.dma_start(out=y_d[s, t0:t0 + 128, :], in_=stg), r=XNK))
        P.emit(final_wait_ops=out_ops[-8:] + tap_ops)
    return nc


_CACHE = {}


def kernel(**inputs):
    if "nc" not in _CACHE:
        _CACHE["nc"] = build()
    nc = _CACHE["nc"]
    consts = make_consts()
    in_maps = []
    for c in range(8):
        m = {}
        m["x"] = np.ascontiguousarray(inputs["x"][2 * c:2 * c + 2]).astype(np.float32, copy=False)
        m["mem"] = np.ascontiguousarray(inputs["mem"][2 * c:2 * c + 2]).astype(np.float32, copy=False)
        m["positions"] = np.ascontiguousarray(inputs["positions"][2 * c:2 * c + 2]).astype(np.int32, copy=False)
        for k in WEIGHTS:
            m[k] = np.ascontiguousarray(np.asarray(inputs[k], dtype=np.float32))
        for k in CONST_SHAPES:
            m["c_" + k] = np.ascontiguousarray(consts[k])
        in_maps.append(m)
    res = run_bass_kernel_spmd(nc, in_maps, core_ids=list(range(8)))
    return np.concatenate([np.asarray(r["y"]) for r in res.results], axis=0)
```

```python
import contextlib
import math
import types
import numpy as np
import concourse.bass as bass
import concourse.mybir as mybir
from concourse.bass_utils import run_bass_kernel_spmd

F32 = mybir.dt.float32
BF16 = mybir.dt.bfloat16
I32 = mybir.dt.int32
AF = mybir.ActivationFunctionType
ALU = mybir.AluOpType

SEM_LIMIT = 30000
T = 2048
D = 1024
DFF = 2816
EPS = 1e-6
NEG = -30000.0


class Prog:
    ENGS = ("tensor", "vector", "scalar", "gpsimd", "sync")

    def __init__(self, nc):
        self.nc = nc
        self.ops = []
        self.last_w = {}
        self.readers = {}

    @staticmethod
    def _freeze(fn):
        if fn.__closure__ is None:
            return fn
        cells = []
        for c in fn.__closure__:
            try:
                cells.append(types.CellType(c.cell_contents))
            except ValueError:
                cells.append(c)
        return types.FunctionType(fn.__code__, fn.__globals__, fn.__name__, fn.__defaults__, tuple(cells))

    def op(self, eng, fn, reads=(), writes=(), dma=False):
        fn = self._freeze(fn)
        i = len(self.ops)
        deps = set()
        for k in reads:
            w = self.last_w.get(k)
            if w is not None:
                deps.add(w)
        for k in writes:
            w = self.last_w.get(k)
            if w is not None:
                deps.add(w)
            for r in self.readers.get(k, ()):
                deps.add(r)
        deps.discard(i)
        for k in reads:
            self.readers.setdefault(k, []).append(i)
        for k in writes:
            self.last_w[k] = i
            self.readers[k] = []
        self.ops.append(dict(eng=eng, fn=fn, deps=deps, dma=dma))
        return i

    def emit(self, final_wait_ops=()):
        nc = self.nc
        ops = self.ops
        needed = set()
        for o in ops:
            for d in o["deps"]:
                if ops[d]["eng"] == "tensor" and o["eng"] == "tensor" and not ops[d]["dma"] and not o["dma"]:
                    continue
                needed.add(d)
        for d in final_wait_ops:
            needed.add(d)
        DR = 8
        counters = {}
        sig = {}
        for i, o in enumerate(ops):
            if o["dma"]:
                n = counters.get(("dma", o["eng"]), 0)
                counters[("dma", o["eng"])] = n + 1
                sig[i] = (("d", o["eng"], n % DR), n // DR + 1, 16)
                continue
            if i not in needed:
                continue
            c = counters.get(o["eng"], 0)
            counters[o["eng"]] = c + 1
            sig[i] = (("c", o["eng"], c // SEM_LIMIT), c % SEM_LIMIT + 1, 1)
        with contextlib.ExitStack() as es:
            sems = {}
            for i in sorted(sig):
                key = sig[i][0]
                if key not in sems:
                    sems[key] = es.enter_context(nc.semaphore("s_%s_%s_%d" % key))
            block = es.enter_context(nc.Block())

            def make_body(engname):
                def body(e):
                    waited = {}

                    def wait(key, v, mult):
                        if waited.get(key, 0) >= v:
                            return
                        e.wait_ge(sems[key], v * mult)
                        waited[key] = v
                        if key[0] == "c":
                            for jj in range(key[2]):
                                waited[(key[0], key[1], jj)] = SEM_LIMIT

                    for i, o in enumerate(ops):
                        if o["eng"] != engname:
                            continue
                        for d in sorted(o["deps"]):
                            if d not in sig:
                                continue
                            if ops[d]["eng"] == "tensor" and engname == "tensor" and not ops[d]["dma"] and not o["dma"]:
                                continue
                            key, v, mult = sig[d]
                            wait(key, v, mult)
                        if o["dma"]:
                            key, v, mult = sig[i]
                            if v > 1:
                                wait(key, v - 1, mult)
                        ins = o["fn"](e)
                        if i in sig:
                            key, v, mult = sig[i]
                            ins.then_inc(sems[key], mult)
                    if engname == "sync":
                        for d in final_wait_ops:
                            key, v, mult = sig[d]
                            wait(key, v, mult)
                return body

            for engname in self.ENGS:
                if any(o["eng"] == engname for o in ops) or engname == "sync":
                    getattr(block, engname)(make_body(engname))


def make_consts():
    c = {}
    c["ident"] = np.eye(128, dtype=np.float32)
    j = np.arange(64)
    c["tri"] = (j[:, None] <= j[None, :]).astype(np.float32)
    c["masku"] = (j[:, None] > j[None, :]).astype(np.float32)
    c["negs"] = np.where(j[None, :] >= j[:, None], NEG, 0.0).astype(np.float32)
    c["negi"] = np.where(j[:, None] > j[None, :], NEG, 0.0).astype(np.float32)
    c["caus32"] = ((j[:, None] % 32) <= j[None, :32]).astype(np.float32)
    gam = 1.0 - 2.0 ** (-5.0 - np.arange(4))
    lg = np.log(gam)
    maskr = np.zeros((64, 4, 64), np.float64)
    gq = np.zeros((64, 4, 64), np.float64)
    gk = np.zeros((64, 4), np.float64)
    for h in range(4):
        rel = j[None, :] - j[:, None]
        maskr[:, h, :] = np.where(rel >= 0, np.exp(lg[h] * np.maximum(rel, 0)), 0.0) / 8.0
        gq[:, h, :] = np.exp(lg[h] * (j[None, :] + 1.0))
        gk[:, h] = np.exp(lg[h] * (63.0 - j)) / 8.0
    c["maskr"] = maskr.astype(np.float32)
    c["gq"] = gq.astype(np.float32)
    c["gk"] = gk.astype(np.float32)
    c["retdec"] = [float(np.exp(lg[h] * 64.0)) for h in range(4)]
    p = np.arange(64)
    freq = 10000.0 ** (-(np.arange(0, 64, 2, dtype=np.float32)) / 64.0)
    c["freq"] = freq.astype(np.float32)[p % 32].reshape(64, 1).astype(np.float32)
    c["sgn"] = np.where(p < 32, -1.0, 1.0).reshape(64, 1).astype(np.float32)
    perm = np.zeros((64, 64), np.float32)
    perm[(p + 32) % 64, p] = 1.0
    c["perm"] = perm
    t = np.arange(T)
    c["rst"] = np.stack([(t % 64 != 0), (t % 32 != 0)]).astype(np.float32)
    return c


CONST_SHAPES = dict(ident=[128, 128], tri=[64, 64], masku=[64, 64], negs=[64, 64], negi=[64, 64],
                    caus32=[64, 32], maskr=[64, 4, 64], gq=[64, 4, 64], gk=[64, 4], freq=[64, 1],
                    sgn=[64, 1], perm=[64, 64], rst=[2, T])

WEIGHTS = dict(
    mix_norm_w=[2, D], w_in=[2, D, 3864], gdn_conv_w=[2, 4, 768], gdn_a_log=[2, 4], gdn_dt_bias=[2, 4],
    gdn_norm_w=[2, 64], gla_gk_up=[2, 16, 128], gla_gk_bias=[2, 128], gla_norm_w=[2, 64],
    hgrn_lb_logits=[2, 256], hgrn_norm_w=[2, 64], w_out=[2, D, D], xattn_norm_w=[2, D], mem_norm_w=[D],
    xattn_wq=[2, D, D], xattn_wk=[2, D, D], xattn_wv=[2, D, D], xattn_wo=[2, D, D], ffn_norm_w=[2, D],
    ffn_up=[2, D, 2 * DFF], ffn_conv_w=[2, 3, 2 * DFF], ffn_down=[2, DFF, D], final_norm_w=[D])

RQ, RK, RV, RG = 0, 256, 512, 768
BQ, BK, BV, BA, BB, BG = 1024, 1280, 1536, 1792, 1796, 1800
CQ, CK, CV, CLR, CG = 2056, 2184, 2312, 2568, 2584
DQ, DF_, DI, DG = 2840, 3096, 3352, 3608


def build(NSEQ=2, NLAYER=2, stages=("mix", "xat", "ffn"), mixers=(0, 1, 2, 3), dbg=False, seqs=None, layers=None):
    nc = bass.Bass("TRN2", target_bir_lowering=False)
    dr = {}
    dr["x"] = nc.dram_tensor("x", [2, T, D], F32, kind="ExternalInput").ap()
    dr["mem"] = nc.dram_tensor("mem", [2, 256, D], F32, kind="ExternalInput").ap()
    dr["positions"] = nc.dram_tensor("positions", [2, T], I32, kind="ExternalInput").ap()
    for k, shp in WEIGHTS.items():
        dr[k] = nc.dram_tensor(k, shp, F32, kind="ExternalInput").ap()
    for k, shp in CONST_SHAPES.items():
        dr["c_" + k] = nc.dram_tensor("c_" + k, shp, F32, kind="ExternalInput").ap()
    y_d = nc.dram_tensor("y", [2, T, D], F32, kind="ExternalOutput").ap()
    if dbg:
        dbg_f = nc.dram_tensor("dbg_f", [64, 8, T], F32, kind="ExternalOutput").ap()
        dbg_b = nc.dram_tensor("dbg_b", [64, 8, T], BF16, kind="ExternalOutput").ap()
    C = make_consts()

    with contextlib.ExitStack() as es:
        def sb(name, shape, dt):
            return es.enter_context(nc.sbuf_tensor(name, shape, dt))

        P = Prog(nc)
        es.enter_context(nc.allow_non_contiguous_dma(reason='small strided parameter loads'))

        def V(fn, r=(), w=()):
            return P.op("vector", fn, r, w)

        def A(fn, r=(), w=()):
            return P.op("scalar", fn, r, w)

        def TE(fn, r=(), w=()):
            return P.op("tensor", fn, r, w)

        def GP(fn, r=(), w=()):
            return P.op("gpsimd", fn, r, w)

        def DS(fn, r=(), w=()):
            return P.op("sync", fn, r, w, dma=True)

        def DG_(fn, r=(), w=()):
            return P.op("gpsimd", fn, r, w, dma=True)

        tap_ops = []

        def tap(idx, ap, keys, n=T):
            if not dbg:
                return
            dst = (dbg_f if ap.dtype == F32 else dbg_b)[0:ap.shape[0], idx, 0:n]
            tap_ops.append(DS(lambda e: e.dma_start(out=dst, in_=ap), r=keys))

        banks = [es.enter_context(nc.psum_tensor(f"pb{i}", [128, 512], F32)) for i in range(8)]
        bank_ctr = [0]

        def nb():
            i = bank_ctr[0] % 8
            bank_ctr[0] += 1
            return banks[i], ("pb", i)

        hT = sb("hT", [128, 8, T], F32)
        xnT = sb("xnT", [128, 8, T], BF16)
        NSLOT = 4
        W = sb("W", [128, NSLOT, 2048], BF16)
        slot_ctr = [0]

        def wslot():
            i = slot_ctr[0] % NSLOT
            slot_ctr[0] += 1
            return i, ("W", i)

        REG = {"F0": (0, 4096), "F1": (4096, 4096), "F2": (8192, 4096)}
        for i in range(5):
            REG["B%d" % i] = (12288 + i * 2048, 2048)
        REG["TM0"] = (22528, 2048)
        REG["TM1"] = (24576, 2048)
        for h in range(4):
            REG[("OM", h)] = (26624 + h * 2048, 2048)
        SCRN = 34816
        SCR = sb("SCR", [128, SCRN], BF16)
        ROPE = sb("ROPE", [128, 4096], BF16)

        def rk(off, n):
            return [k for k, (o, m) in REG.items() if o < off + n and off < o + m]

        def reg_bf(key, parts=64):
            o, m = REG[key]
            return SCR[0:parts, o:o + m]

        def reg_f32(key, parts=64):
            o, m = REG[key]
            return SCR[0:parts, o:o + m].bitcast(F32)

        F = [reg_f32("F%d" % i) for i in range(3)]
        Bf = [reg_bf("B%d" % i) for i in range(5)]
        TM = [reg_bf("TM0"), reg_bf("TM1")]
        OMv = [reg_bf(("OM", h)) for h in range(4)]
        cosT = ROPE[0:64, 0:2048]
        sinS = ROPE[0:64, 2048:4096]
        sq = SCR[:, 26624:26624 + 4096].rearrange("p (k t) -> p k t", t=512)
        SQK = [("OM", 0), ("OM", 1)]
        rsb = sb("rsb", [128, 512], F32)
        ident = sb("ident", [128, 128], F32)
        identb = sb("identb", [128, 128], BF16)
        onesb = sb("onesb", [128, 128], BF16)
        ones64f = sb("ones64f", [64, 64], F32)
        onecol = sb("onecol", [64, 1], F32)
        cst = {}
        for k in ("tri", "masku", "negs", "negi", "caus32", "maskr", "gq", "gk", "freq", "sgn", "perm"):
            cst[k] = sb("k_" + k, CONST_SHAPES[k], F32)
        permb = sb("permb", [64, 64], BF16)
        ident64 = ident[0:64, 0:64]
        normw = sb("normw", [128, 7, 8], F32)
        memnw = sb("memnw", [128, 8], F32)
        hw = sb("hw", [64, 16], F32)
        gsc = sb("gsc", [64, 24], F32)
        glab = sb("glab", [32, 8], F32)
        glup = sb("glup", [16, 2, 128], F32)
        gcw = sb("gcw", [64, 2, 4, 12], F32)
        S32 = sb("S32", [64, 64], F32)
        ab_all = sb("ab_all", [64, 256], F32)
        Sbz = sb("Sbz", [64, 64], BF16)
        Sbf = sb("Sbf", [64, 64], BF16)
        PTb = [sb(f"PTb{i}", [64, 64], BF16) for i in range(2)]
        small = sb("small", [64, 8, 32], F32)
        decs_t = sb("decs_t", [64, 64], F32)
        memT = sb("memT", [128, 8, 256], BF16)
        wsmall = sb("wsmall", [128, 8, 16], BF16)

        for k in cst:
            DS(lambda e, k=k: e.dma_start(out=cst[k][:], in_=dr["c_" + k]), w=[("c", k)])
        DS(lambda e: e.dma_start(out=ident[:], in_=dr["c_ident"]), w=["ident"])
        V(lambda e: e.tensor_copy(out=identb[:], in_=ident[:]), r=["ident"], w=["identb"])
        V(lambda e: e.memset(onesb[:], 1.0), w=["onesb"])
        V(lambda e: e.memset(ones64f[:], 1.0), w=["ones64f"])
        V(lambda e: e.memset(onecol[:], 1.0), w=["onecol"])
        V(lambda e: e.memset(Sbz[:], 0.0), w=["Sbf"])
        V(lambda e: e.tensor_copy(out=permb[:], in_=cst["perm"][:]), r=[("c", "perm")], w=["permb"])
        for a, nm in enumerate(["mix_norm_w", "xattn_norm_w", "ffn_norm_w"]):
            for l in range(2):
                DS(lambda e, a=a, l=l, nm=nm: e.dma_start(out=normw[:, 2 * a + l, :], in_=dr[nm][l].rearrange("(k p) -> p k", p=128)), w=["normw"])
        DS(lambda e: e.dma_start(out=normw[:, 6, :], in_=dr["final_norm_w"].rearrange("(k p) -> p k", p=128)), w=["normw"])
        DS(lambda e: e.dma_start(out=memnw[:], in_=dr["mem_norm_w"].rearrange("(k p) -> p k", p=128)), w=["memnw"])
        for a, nm in enumerate(["gdn_norm_w", "gla_norm_w", "hgrn_norm_w"]):
            DS(lambda e, a=a, nm=nm: e.dma_start(out=hw[:, 2 * a:2 * a + 2], in_=dr[nm].rearrange("l p -> p l")), w=["hw"])
        for l in range(2):
            DS(lambda e, l=l: e.dma_start(out=gsc[:, 16 + 4 * l:20 + 4 * l], in_=dr["hgrn_lb_logits"][l].rearrange("(h p) -> p h", p=64)), w=["gsc"])
        A(lambda e: e.activation(out=gsc[:, 16:24], in_=gsc[:, 16:24], func=AF.Exp), r=["gsc"], w=["gsc"])
        V(lambda e: e.tensor_tensor(out=hw[:, 10:14], in0=gsc[:, 16:20], in1=gsc[:, 20:24], op=ALU.add), r=["gsc"], w=["hw"])
        V(lambda e: e.reciprocal(out=hw[:, 10:14], in_=hw[:, 10:14]), r=["hw"], w=["hw"])
        V(lambda e: e.tensor_tensor(out=hw[:, 6:10], in0=gsc[:, 20:24], in1=hw[:, 10:14], op=ALU.mult), r=["hw", "gsc"], w=["hw"])
        V(lambda e: e.tensor_tensor(out=hw[:, 10:14], in0=gsc[:, 16:20], in1=hw[:, 10:14], op=ALU.mult), r=["hw", "gsc"], w=["hw"])
        V(lambda e: e.memset(hw[:, 14:15], 0.0), w=["hw"])
        V(lambda e: e.memset(hw[:, 15:16], 1.0), w=["hw"])
        DS(lambda e: e.dma_start(out=gsc[:, 0:8], in_=dr["gdn_a_log"].rearrange("l h -> (l h)").unsqueeze(0).broadcast_to([64, 8])), w=["gsc"])
        DS(lambda e: e.dma_start(out=gsc[:, 8:16], in_=dr["gdn_dt_bias"].rearrange("l h -> (l h)").unsqueeze(0).broadcast_to([64, 8])), w=["gsc"])
        A(lambda e: e.activation(out=gsc[:, 0:8], in_=gsc[:, 0:8], func=AF.Exp), r=["gsc"], w=["gsc"])
        V(lambda e: e.tensor_scalar(out=gsc[:, 0:8], in0=gsc[:, 0:8], scalar1=-1.0, scalar2=None, op0=ALU.mult), r=["gsc"], w=["gsc"])
        for l in range(2):
            DS(lambda e, l=l: e.dma_start(out=glab[:, 4 * l:4 * l + 4], in_=dr["gla_gk_bias"][l].rearrange("(h p) -> p h", p=32)), w=["glab"])
            DS(lambda e, l=l: e.dma_start(out=glup[:, l, :], in_=dr["gla_gk_up"][l]), w=["glup"])
            for k4 in range(4):
                DS(lambda e, l=l, k4=k4: e.dma_start(out=gcw[:, l, k4, :], in_=dr["gdn_conv_w"][l, k4].rearrange("(j p) -> p j", p=64)), w=["gcw"])
        V(lambda e: e.tensor_scalar(out=glab[:], in0=glab[:], scalar1=-1.0, scalar2=None, op0=ALU.mult), r=["glab"], w=["glab"])

        def w_cols(name, l, c0, n):
            src = dr[name][l][:, c0:c0 + n].rearrange("(k p) m -> p k m", p=128)
            i, key = wslot()
            dst = W[:, i, 0:8 * n].rearrange("p (k m) -> p k m", m=n)
            DG_(lambda e, dst=dst, src=src: e.dma_start(out=dst, in_=src), w=[key])
            return dst, key

        def w_small(l, c0, n):
            src = dr["w_in"][l][:, c0:c0 + n].rearrange("(k p) m -> p k m", p=128)
            dst = wsmall[:, :, 0:n]
            DG_(lambda e: e.dma_start(out=dst, in_=src), w=["wsmall"])
            return dst, "wsmall"

        def norm(widx):
            for tt in range(4):
                ts = slice(tt * 512, (tt + 1) * 512)
                A(lambda e, ts=ts: e.activation(out=sq, in_=hT[:, :, ts], func=AF.Square), r=["hT"], w=SQK)
                pb, pk = nb()
                for k in range(8):
                    TE(lambda e, k=k, pb=pb: e.matmul(pb[:], lhsT=onesb[:], rhs=sq[:, k, :], start=(k == 0), stop=(k == 7)), r=SQK + ["onesb"], w=[pk])
                A(lambda e, pb=pb: e.activation(out=rsb[:], in_=pb[:], func=AF.Ln, scale=1.0 / D, bias=EPS), r=[pk], w=["rsb"])
                A(lambda e: e.activation(out=rsb[:], in_=rsb[:], func=AF.Exp, scale=-0.5), r=["rsb"], w=["rsb"])
                for k in range(8):
                    V(lambda e, k=k, ts=ts: e.scalar_tensor_tensor(out=xnT[:, k, ts], in0=hT[:, k, ts], scalar=normw[:, widx, k:k + 1], in1=rsb[:], op0=ALU.mult, op1=ALU.mult),
                      r=["hT", "rsb", "normw"], w=[("xnT", tt)])

        def proj64(wv, wkey, c0, M, tt):
            pb, pk = nb()
            for k in range(8):
                TE(lambda e, k=k, pb=pb: e.matmul(pb[0:M, :], lhsT=wv[:, k, c0:c0 + M], rhs=xnT[:, k, tt * 512:(tt + 1) * 512], start=(k == 0), stop=(k == 7)),
                   r=[wkey, ("xnT", tt)], w=[pk])
            return pb, pk

        def to_tm(src, skey, dst, dkey, Cc, Dd, scale_ap=None):
            nch = T // Cc
            per = 1024 // Dd
            dv = dst[0:Cc, 0:nch * Dd].rearrange("p (n d) -> p n d", d=Dd)
            for g in range(nch // per):
                pb, pk = nb()
                pbb = pb[:].bitcast(BF16)
                for j in range(per):
                    n = g * per + j
                    TE(lambda e, n=n, j=j, pbb=pbb: e.transpose(out=pbb[0:Cc, j * Dd:(j + 1) * Dd], in_=src[0:Dd, n * Cc:(n + 1) * Cc], identity=identb[0:Dd, 0:Dd]),
                       r=[skey, "identb"], w=[pk])
                o = dv[:, g * per:(g + 1) * per, :]
                i_ = pbb[0:Cc, :].rearrange("p (n d) -> p n d", d=Dd)
                A(lambda e, o=o, i_=i_: e.activation(out=o, in_=i_, func=AF.Copy), r=[pk], w=[dkey])
                if scale_ap is not None:
                    V(lambda e, o=o: e.tensor_scalar(out=o, in0=o, scalar1=scale_ap, scalar2=None, op0=ALU.mult), r=[dkey], w=[dkey])
            return dv

        def headnorm_gate(osrc, okey, gate, gkey, nw_ap, h, tmpk="TM0"):
            tmp = reg_bf(tmpk)
            A(lambda e: e.activation(out=tmp, in_=osrc, func=AF.Square), r=[okey], w=[tmpk])
            for tt in range(4):
                ts = slice(tt * 512, (tt + 1) * 512)
                pb, pk = nb()
                TE(lambda e, pb=pb, ts=ts: e.matmul(pb[0:64, :], lhsT=onesb[0:64, 0:64], rhs=tmp[:, ts], start=True, stop=True), r=[tmpk, "onesb"], w=[pk])
                A(lambda e, pb=pb, ts=ts: e.activation(out=rsb[0:64, :], in_=pb[0:64, :], func=AF.Ln, scale=1.0 / 64, bias=EPS), r=[pk], w=["rsb"])
                A(lambda e: e.activation(out=rsb[0:64, :], in_=rsb[0:64, :], func=AF.Exp, scale=-0.5), r=["rsb"], w=["rsb"])
                V(lambda e, ts=ts: e.scalar_tensor_tensor(out=osrc[:, ts], in0=osrc[:, ts], scalar=nw_ap, in1=rsb[0:64, :], op0=ALU.mult, op1=ALU.mult), r=[okey, "rsb", "hw"], w=[okey])
            V(lambda e: e.tensor_tensor(out=OMv[h], in0=osrc, in1=gate, op=ALU.mult), r=[okey, gkey], w=[("OM", h)])

        def out_proj(l, row0):
            src = dr["w_out"][l][row0:row0 + 256, :].rearrange("(h p) m -> p h m", p=64)
            i, key = wslot()
            i2, key2 = wslot()
            dsts = [W[0:64, i, :].rearrange("p (h m) -> p h m", m=1024), W[0:64, i2, :].rearrange("p (h m) -> p h m", m=1024)]
            DG_(lambda e: e.dma_start(out=dsts[0], in_=src[:, 0:2, :]), w=[key])
            DG_(lambda e: e.dma_start(out=dsts[1], in_=src[:, 2:4, :]), w=[key2])
            for mt in range(8):
                for tt in range(4):
                    ts = slice(tt * 512, (tt + 1) * 512)
                    pb, pk = nb()
                    for h in range(4):
                        TE(lambda e, h=h, pb=pb, mt=mt, ts=ts: e.matmul(pb[:], lhsT=dsts[h // 2][:, h % 2, mt * 128:(mt + 1) * 128], rhs=OMv[h][:, ts], start=(h == 0), stop=(h == 3)),
                           r=[key, key2, ("OM", h)], w=[pk])
                    V(lambda e, pb=pb, mt=mt, ts=ts: e.tensor_tensor(out=hT[:, mt, ts], in0=hT[:, mt, ts], in1=pb[:], op=ALU.add), r=[pk, "hT"], w=["hT"])

        def state_init():
            V(lambda e: e.memset(S32[:], 0.0), w=["S32"])
            V(lambda e: e.memset(Sbf[:], 0.0), w=["Sbf"])

        def state_update(Dk, p3, k3, dec):
            V(lambda e: e.scalar_tensor_tensor(out=S32[0:Dk, :], in0=S32[0:Dk, :], scalar=dec, in1=p3[0:Dk, 0:64], op0=ALU.mult, op1=ALU.add), r=["S32", k3, "dec"], w=["S32"])
            A(lambda e: e.activation(out=Sbf[0:Dk, :], in_=S32[0:Dk, :], func=AF.Copy), r=["S32"], w=["Sbf"])

        SBK = [("Sb", v) for v in range(64)]

        def lin_loop(Cc, Dk, ks, kskey, qs, qskey, qi, qikey, vtm, ktm, mask_ap, mkey, dec_ap, odst, okey):
            nblk = T // Cc
            npass = nblk // 32
            PTall = reg_bf("B2")
            SKV = F[0].rearrange("p (n v) -> p n v", v=64)
            Sball = reg_bf("F1")

            def blk(n):
                if Cc == 64:
                    return slice(0, 64), n, slice(n * 64, (n + 1) * 64)
                return slice((n % 2) * 32, (n % 2) * 32 + 32), n // 2, slice(n * 32, (n + 1) * 32)

            def phase_kv(ps_i):
                if Cc == 64:
                    for g in range(4):
                        p3, k3 = nb()
                        for j in range(8):
                            n = ps_i * 32 + g * 8 + j
                            pp, c64, cs = blk(n)
                            TE(lambda e, p3=p3, j=j, pp=pp, c64=c64: e.matmul(p3[0:Dk, j * 64:(j + 1) * 64], lhsT=ktm[pp, c64, :], rhs=vtm[pp, c64, :], start=True, stop=True), r=["TM1", "TM0"], w=[k3])
                        A(lambda e, p3=p3, g=g: e.activation(out=SKV[0:Dk, g * 8:(g + 1) * 8, :], in_=p3[0:Dk, :].rearrange("p (n v) -> p n v", v=64), func=AF.Copy), r=[k3], w=["F0"])
                    return
                SKVp = F[0].rearrange("p (m two v) -> p two m v", two=2, v=64)
                for g in range(2):
                    pbs = [nb(), nb()]
                    for j in range(16):
                        n = ps_i * 32 + g * 16 + j
                        pp, c64, cs = blk(n)
                        p3, k3 = pbs[n % 2]
                        jj = j // 2
                        TE(lambda e, p3=p3, jj=jj, pp=pp, c64=c64: e.matmul(p3[0:Dk, jj * 64:(jj + 1) * 64], lhsT=ktm[pp, c64, :], rhs=vtm[pp, c64, :], start=True, stop=True), r=["TM1", "TM0"], w=[k3])
                    for par in range(2):
                        p3, k3 = pbs[par]
                        A(lambda e, p3=p3, g=g, par=par: e.activation(out=SKVp[0:Dk, par, g * 8:(g + 1) * 8, :], in_=p3[0:Dk, :].rearrange("p (n v) -> p n v", v=64), func=AF.Copy), r=[k3], w=["F0"])

            def phase_scores():
                if Cc == 64:
                    PTv = PTall.rearrange("p (n c) -> p n c", c=64)
                    for g in range(4):
                        p1, k1 = nb()
                        for j in range(8):
                            n = g * 8 + j
                            pp, c64, cs = blk(n)
                            TE(lambda e, p1=p1, j=j, cs=cs: e.matmul(p1[0:64, j * 64:(j + 1) * 64], lhsT=ks[0:Dk, cs], rhs=qs[0:Dk, cs], start=True, stop=True), r=[kskey, qskey], w=[k1])
                        V(lambda e, p1=p1, g=g: e.tensor_tensor(out=PTv[:, g * 8:(g + 1) * 8, :], in0=p1[0:64, :].rearrange("p (n c) -> p n c", c=64), in1=mask_ap.unsqueeze(1).to_broadcast([64, 8, 64]), op=ALU.mult), r=[k1, mkey], w=["B2"])
                    return PTv
                PTv = PTall[:, 0:1024].rearrange("p (n c) -> p n c", c=32)
                for g in range(2):
                    pbs = [nb(), nb()]
                    for j in range(32):
                        n = g * 32 + j
                        pp, c64, cs = blk(n)
                        cl = c64 - g * 16
                        p1, k1 = pbs[n % 2]
                        TE(lambda e, p1=p1, pp=pp, cl=cl, cs=cs: e.matmul(p1[pp, cl * 32:(cl + 1) * 32], lhsT=ks[0:Dk, cs], rhs=qs[0:Dk, cs], start=True, stop=True), r=[kskey, qskey], w=[k1])
                    for par in range(2):
                        p1, k1 = pbs[par]
                        pp = slice(par * 32, par * 32 + 32)
                        V(lambda e, p1=p1, g=g, pp=pp: e.tensor_tensor(out=PTv[pp, g * 16:(g + 1) * 16, :], in0=p1[pp, :].rearrange("p (n c) -> p n c", c=32), in1=mask_ap[pp, :].unsqueeze(1).to_broadcast([32, 16, 32]), op=ALU.mult), r=[k1, mkey], w=["B2"])
                return PTv

            def phase_scan(ps_i):
                Sb = Sball[:, ps_i * 2048:(ps_i + 1) * 2048].rearrange("p (n v) -> p n v", v=64)
                for v in range(64):
                    init = 0.0 if ps_i == 0 else S32[0:Dk, v:v + 1]
                    V(lambda e, v=v, init=init, Sb=Sb: e.tensor_tensor_scan(out=Sb[0:Dk, :, v], data0=dec_ap[:, ps_i * 32:(ps_i + 1) * 32], data1=SKV[0:Dk, :, v], initial=init, op0=ALU.mult, op1=ALU.add),
                      r=["F0", "dec", "S32"], w=([("Sb", ps_i, v), "F1"] if v == 0 else [("Sb", ps_i, v)]))
                return Sb

            def phase_out(ps_i, PTv, Sb, carry):
                SBKp = [("Sb", ps_i, v) for v in range(64)]
                if Cc == 64:
                    for g in range(4):
                        p2, k2 = nb()
                        for j in range(8):
                            nl = g * 8 + j
                            n = ps_i * 32 + nl
                            pp, c64, cs = blk(n)
                            first = (nl == 0 and carry is None)
                            TE(lambda e, p2=p2, j=j, pp=pp, c64=c64, first=first: e.matmul(p2[0:64, j * 64:(j + 1) * 64], lhsT=vtm[pp, c64, :], rhs=PTv[pp, c64, :], start=True, stop=first), r=["TM0", "B2"], w=[k2])
                            if not first:
                                sprev = carry if nl == 0 else Sb[0:Dk, nl - 1, :]
                                rk_ = ["Sbf"] if nl == 0 else SBKp + ["F1"]
                                TE(lambda e, p2=p2, j=j, cs=cs, sprev=sprev: e.matmul(p2[0:64, j * 64:(j + 1) * 64], lhsT=sprev, rhs=qi[0:Dk, cs], start=False, stop=True), r=rk_ + [qikey], w=[k2])
                        t0 = (ps_i * 32 + g * 8) * 64
                        A(lambda e, p2=p2, t0=t0: e.activation(out=odst[:, t0:t0 + 512], in_=p2[0:64, :], func=AF.Copy), r=[k2], w=[okey])
                    return
                for g in range(2):
                    pbs = [nb(), nb()]
                    pI, kI = nb()
                    for j in range(16):
                        nl = g * 16 + j
                        n = ps_i * 32 + nl
                        pp, c64, cs = blk(n)
                        p2, k2 = pbs[j % 2]
                        jj = j // 2
                        TE(lambda e, p2=p2, jj=jj, pp=pp, c64=c64: e.matmul(p2[0:64, jj * 32:(jj + 1) * 32], lhsT=vtm[pp, c64, :], rhs=PTv[pp, c64, :], start=True, stop=True), r=["TM0", "B2"], w=[k2])
                        sprev = carry if nl == 0 else Sb[0:Dk, nl - 1, :]
                        rk_ = ["Sbf"] if nl == 0 else SBKp + ["F1"]
                        TE(lambda e, pI=pI, j=j, cs=cs, sprev=sprev: e.matmul(pI[0:64, j * 32:(j + 1) * 32], lhsT=sprev, rhs=qi[0:Dk, cs], start=True, stop=True), r=rk_ + [qikey], w=[kI])
                    t0 = (ps_i * 32 + g * 16) * 32
                    A(lambda e, pI=pI, t0=t0: e.activation(out=odst[:, t0:t0 + 512], in_=pI[0:64, :], func=AF.Copy), r=[kI], w=[okey])
                    ov = odst[:, t0:t0 + 512].rearrange("p (m two c) -> p two m c", two=2, c=32)
                    for par in range(2):
                        p2, k2 = pbs[par]
                        V(lambda e, p2=p2, ov=ov, par=par: e.tensor_tensor(out=ov[:, par, :, :], in0=ov[:, par, :, :], in1=p2[0:64, 0:256].rearrange("p (m c) -> p m c", c=32), op=ALU.add), r=[k2, okey], w=[okey])

            import os
            cut = int(os.environ.get("LL_CUT", "99"))
            phase_kv(0)
            if cut == 1:
                return
            PTv = phase_scores()
            if cut == 2:
                return
            Sb0 = phase_scan(0)
            if cut == 3:
                return
            if npass == 1:
                phase_out(0, PTv, Sb0, None)
                return
            SBK0 = [("Sb", 0, v) for v in range(64)]
            V(lambda e: e.tensor_copy(out=S32[0:Dk, :], in_=Sb0[0:Dk, 31, :]), r=SBK0 + ["F1"], w=["S32"])
            V(lambda e: e.tensor_copy(out=Sbf[0:Dk, :], in_=Sb0[0:Dk, 31, :]), r=SBK0 + ["F1"], w=["Sbf"])
            phase_out(0, PTv, Sb0, Sbz[0:Dk, :])
            if cut == 4:
                return
            phase_kv(1)
            Sb1 = phase_scan(1)
            phase_out(1, PTv, Sb1, Sbf[0:Dk, :])

        def rope_tables(s):
            pos_i = F[2].bitcast(I32)
            DS(lambda e: e.dma_start(out=pos_i, in_=dr["positions"][s:s + 1, :].broadcast_to([64, T])), w=["F2"])
            V(lambda e: e.tensor_copy(out=F[0], in_=pos_i), r=["F2"], w=["F0"])
            V(lambda e: e.tensor_scalar(out=F[0], in0=F[0], scalar1=cst["freq"][:, 0:1], scalar2=None, op0=ALU.mult), r=["F0", ("c", "freq")], w=["F0"])
            MAGIC = 12582912.0
            C1 = 6.28125
            C2 = 2 * math.pi - 6.28125
            for which, dstt in ((0, sinS), (1, cosT)):
                shift = 0.0 if which == 0 else math.pi / 2
                V(lambda e, shift=shift: e.tensor_scalar(out=F[1], in0=F[0], scalar1=shift, scalar2=None, op0=ALU.add), r=["F0"], w=["F1"])
                V(lambda e: e.tensor_scalar(out=F[2], in0=F[1], scalar1=1.0 / (2 * math.pi), scalar2=MAGIC, op0=ALU.mult, op1=ALU.add), r=["F1"], w=["F2"])
                V(lambda e: e.tensor_scalar(out=F[2], in0=F[2], scalar1=MAGIC, scalar2=None, op0=ALU.subtract), r=["F2"], w=["F2"])
                V(lambda e: e.scalar_tensor_tensor(out=F[1], in0=F[2], scalar=-C1, in1=F[1], op0=ALU.mult, op1=ALU.add), r=["F1", "F2"], w=["F1"])
                V(lambda e: e.scalar_tensor_tensor(out=F[1], in0=F[2], scalar=-C2, in1=F[1], op0=ALU.mult, op1=ALU.add), r=["F1", "F2"], w=["F1"])
                V(lambda e: e.tensor_scalar(out=F[1], in0=F[1], scalar1=-math.pi, scalar2=math.pi, op0=ALU.max, op1=ALU.min), r=["F1"], w=["F1"])
                if which == 0:
                    A(lambda e: e.activation(out=F[2], in_=F[1], func=AF.Sin), r=["F1"], w=["F2"])
                    V(lambda e: e.tensor_scalar(out=sinS, in0=F[2], scalar1=cst["sgn"][:, 0:1], scalar2=None, op0=ALU.mult), r=["F2", ("c", "sgn")], w=["ROPE"])
                else:
                    A(lambda e: e.activation(out=cosT, in_=F[1], func=AF.Sin), r=["F1"], w=["ROPE"])

        def retention(s, l):
            cut = 9
            rope_tables(s)
            if cut == 0:
                return
            wq, kq = w_cols("w_in", l, RQ, 256)
            wk, kk = w_cols("w_in", l, RK, 256)
            wv, kv = w_cols("w_in", l, RV, 256)
            wg, kg = w_cols("w_in", l, RG, 256)
            tmpb = reg_bf("TM1")
            for h in range(4):
                QB, KB, VB, GB, QI = Bf
                for tt in range(4):
                    ts = slice(tt * 512, (tt + 1) * 512)
                    for (w_, wk_, dstb, dk_) in ((wq, kq, QB, "B0"), (wk, kk, KB, "B1")):
                        pb, pk = proj64(w_, wk_, h * 64, 64, tt)
                        A(lambda e, pb=pb: e.activation(out=tmpb[:, 0:512], in_=pb[0:64, :], func=AF.Copy), r=[pk], w=["TM1"])
                        p2, k2 = nb()
                        TE(lambda e, p2=p2: e.matmul(p2[0:64, :], lhsT=permb[:], rhs=tmpb[:, 0:512], start=True, stop=True), r=["TM1", "permb"], w=[k2])
                        A(lambda e, pb=pb, ts=ts: e.activation(out=F[0][:, ts], in_=pb[0:64, :], func=AF.Copy), r=[pk], w=["F0"])
                        A(lambda e, p2=p2, ts=ts: e.activation(out=F[1][:, ts], in_=p2[0:64, :], func=AF.Copy), r=[k2], w=["F1"])
                        V(lambda e, ts=ts: e.tensor_tensor(out=F[0][:, ts], in0=F[0][:, ts], in1=cosT[:, ts], op=ALU.mult), r=["F0", "ROPE"], w=["F0"])
                        V(lambda e, ts=ts: e.tensor_tensor(out=F[1][:, ts], in0=F[1][:, ts], in1=sinS[:, ts], op=ALU.mult), r=["F1", "ROPE"], w=["F1"])
                        V(lambda e, ts=ts, dstb=dstb: e.tensor_tensor(out=dstb[:, ts], in0=F[0][:, ts], in1=F[1][:, ts], op=ALU.add), r=["F0", "F1"], w=[dk_])
                    pb, pk = proj64(wv, kv, h * 64, 64, tt)
                    A(lambda e, pb=pb, ts=ts: e.activation(out=VB[:, ts], in_=pb[0:64, :], func=AF.Copy), r=[pk], w=["B2"])
                    pb, pk = proj64(wg, kg, h * 64, 64, tt)
                    A(lambda e, pb=pb, ts=ts: e.activation(out=GB[:, ts], in_=pb[0:64, :], func=AF.Silu), r=[pk], w=["B3"])
                if cut == 1:
                    continue
                V(lambda e, h=h: e.tensor_tensor(out=QI.rearrange("p (n c) -> p n c", c=64), in0=QB.rearrange("p (n c) -> p n c", c=64),
                                                 in1=cst["gq"][:, h, :].unsqueeze(1).to_broadcast([64, 32, 64]), op=ALU.mult), r=["B0", ("c", "gq")], w=["B4"])
                vtm = to_tm(VB, "B2", TM[0], "TM0", 64, 64)
                ktm = to_tm(KB, "B1", TM[1], "TM1", 64, 64, scale_ap=cst["gk"][:, h:h + 1])
                if cut == 2:
                    continue
                dec = C["retdec"][h]
                V(lambda e: e.memset(decs_t[:, 0:32], dec), w=["dec"])
                lin_loop(64, 64, KB, "B1", QB, "B0", QI, "B4", vtm, ktm, cst["maskr"][:, h, :], ("c", "maskr"), decs_t[0:64, 0:32], F[2], "F2")
                headnorm_gate(F[2], "F2", GB, "B3", hw[:, 15:16], h)
            out_proj(l, 0)

        def diag_gated(s, l, mixer):
            Dk = 32 if mixer == 2 else 64
            if mixer == 2:
                wq, kq = w_cols("w_in", l, CQ, 128)
                wk, kk = w_cols("w_in", l, CK, 128)
                wv, kv = w_cols("w_in", l, CV, 256)
                wg, kg = w_cols("w_in", l, CG, 256)
                wl, kl = w_small(l, CLR, 16)
            else:
                wq, kq = w_cols("w_in", l, DQ, 256)
                wf, kf = w_cols("w_in", l, DF_, 256)
                wv, kv = w_cols("w_in", l, DI, 256)
                wg, kg = w_cols("w_in", l, DG, 256)
            gsl = -1.0 / 16 if mixer == 2 else -1.0
            qsc = Dk ** -0.5 if mixer == 2 else 1.0
            for h in range(4):
                QS, KS, KT, VB, GB = Bf
                L_, E_, K_ = F
                for tt in range(4):
                    ts = slice(tt * 512, (tt + 1) * 512)
                    if mixer == 2:
                        pb, pk = proj64(wl, kl, 0, 16, tt)
                        A(lambda e, pb=pb: e.activation(out=rsb[0:16, :], in_=pb[0:16, :], func=AF.Copy), r=[pk], w=["rsb"])
                        pb, pk = nb()
                        TE(lambda e, pb=pb, h=h: e.matmul(pb[0:32, :], lhsT=glup[:, l, h * 32:(h + 1) * 32], rhs=rsb[0:16, :], start=True, stop=True), r=["rsb", "glup"], w=[pk])
                        A(lambda e, pb=pb, ts=ts, h=h: e.activation(out=L_[0:32, ts], in_=pb[0:32, :], func=AF.Exp, scale=-1.0, bias=glab[:, l * 4 + h:l * 4 + h + 1]), r=[pk, "glab"], w=["F0"])
                        A(lambda e, ts=ts: e.activation(out=L_[0:32, ts], in_=L_[0:32, ts], func=AF.Ln, bias=1.0), r=["F0"], w=["F0"])
                        pb, pk = proj64(wk, kk, h * 32, 32, tt)
                        A(lambda e, pb=pb, ts=ts: e.activation(out=K_[0:32, ts], in_=pb[0:32, :], func=AF.Copy), r=[pk], w=["F2"])
                    else:
                        lbc = hw[:, 6 + h:7 + h] if l == 1 else hw[:, 14:15]
                        omc = hw[:, 10 + h:11 + h] if l == 1 else hw[:, 15:16]
                        pb, pk = proj64(wf, kf, h * 64, 64, tt)
                        A(lambda e, pb=pb, ts=ts: e.activation(out=E_[:, ts], in_=pb[0:64, :], func=AF.Sigmoid), r=[pk], w=["F1"])
                        V(lambda e, ts=ts, lbc=lbc, omc=omc: e.tensor_scalar(out=L_[:, ts], in0=E_[:, ts], scalar1=omc, scalar2=lbc, op0=ALU.mult, op1=ALU.add), r=["F1", "hw"], w=["F0"])
                        A(lambda e, ts=ts: e.activation(out=L_[:, ts], in_=L_[:, ts], func=AF.Ln), r=["F0"], w=["F0"])
                        V(lambda e, ts=ts: e.tensor_scalar(out=L_[:, ts], in0=L_[:, ts], scalar1=-1.0, scalar2=None, op0=ALU.mult), r=["F0"], w=["F0"])
                        V(lambda e, ts=ts: e.tensor_scalar(out=K_[:, ts], in0=E_[:, ts], scalar1=-1.0, scalar2=1.0, op0=ALU.mult, op1=ALU.add), r=["F1"], w=["F2"])
                        V(lambda e, ts=ts, omc=omc: e.tensor_scalar(out=K_[:, ts], in0=K_[:, ts], scalar1=omc, scalar2=None, op0=ALU.mult), r=["F2", "hw"], w=["F2"])
                    pb, pk = proj64(wv, kv, h * 64, 64, tt)
                    A(lambda e, pb=pb, ts=ts: e.activation(out=VB[:, ts], in_=pb[0:64, :], func=AF.Copy), r=[pk], w=["B3"])
                    pb, pk = proj64(wg, kg, h * 64, 64, tt)
                    A(lambda e, pb=pb, ts=ts: e.activation(out=GB[:, ts], in_=pb[0:64, :], func=AF.Silu), r=[pk], w=["B4"])
                V(lambda e: e.tensor_tensor_scan(out=E_[0:Dk, :], data0=onecol[0:Dk, 0:1].to_broadcast([Dk, T]), data1=L_[0:Dk, :], initial=0.0, op0=ALU.mult, op1=ALU.add), r=["F0", "onecol"], w=["F1"])
                Ev = E_[0:Dk, :].rearrange("p (m c) -> p m c", c=32)
                Lv = L_[0:Dk, :].rearrange("p (m c) -> p m c", c=32)
                V(lambda e: e.memset(decs_t[0:Dk, 0:1], 0.0), w=["dec"])
                V(lambda e: e.tensor_copy(out=decs_t[0:Dk, 1:64], in_=Ev[:, 0:63, 31]), r=["F1"], w=["dec"])
                V(lambda e: e.tensor_tensor(out=Ev, in0=Ev, in1=decs_t[0:Dk, 0:64].unsqueeze(2).to_broadcast([Dk, 64, 32]), op=ALU.subtract), r=["F1", "dec"], w=["F1"])
                A(lambda e: e.activation(out=L_[0:Dk, :], in_=E_[0:Dk, :], func=AF.Exp, scale=-gsl), r=["F1"], w=["F0"])
                V(lambda e: e.tensor_tensor(out=KS[0:Dk, :], in0=K_[0:Dk, :], in1=L_[0:Dk, :], op=ALU.mult), r=["F0", "F2"], w=["B1"])
                V(lambda e: e.tensor_tensor(out=Lv, in0=Ev, in1=Ev[:, :, 31:32].to_broadcast([Dk, 64, 32]), op=ALU.subtract), r=["F1", "B1"], w=["F0"])
                A(lambda e: e.activation(out=L_[0:Dk, :], in_=L_[0:Dk, :], func=AF.Exp, scale=-gsl), r=["F0"], w=["F0"])
                V(lambda e: e.tensor_tensor(out=KT[0:Dk, :], in0=K_[0:Dk, :], in1=L_[0:Dk, :], op=ALU.mult), r=["F0", "F2"], w=["B2"])
                A(lambda e: e.activation(out=decs_t[0:Dk, 0:64], in_=Ev[:, :, 31], func=AF.Exp, scale=gsl), r=["F1"], w=["dec"])
                A(lambda e: e.activation(out=L_[0:Dk, :], in_=E_[0:Dk, :], func=AF.Exp, scale=gsl), r=["F1", "B2"], w=["F0"])
                for tt in range(4):
                    ts = slice(tt * 512, (tt + 1) * 512)
                    pb, pk = proj64(wq, kq, h * Dk, Dk, tt)
                    V(lambda e, pb=pb, ts=ts: e.scalar_tensor_tensor(out=QS[0:Dk, ts], in0=pb[0:Dk, :], scalar=qsc, in1=L_[0:Dk, ts], op0=ALU.mult, op1=ALU.mult), r=[pk, "F0"], w=["B0"])
                vtm = to_tm(VB, "B3", TM[0], "TM0", 64, 64)
                ktm = to_tm(KT, "B2", TM[1], "TM1", 64, Dk)
                lin_loop(32, Dk, KS, "B1", QS, "B0", QS, "B0", vtm, ktm, cst["caus32"][:], ("c", "caus32"), decs_t[0:Dk, 0:64], F[2], "F2")
                nwc = hw[:, 2 + l:3 + l] if mixer == 2 else hw[:, 4 + l:5 + l]
                headnorm_gate(F[2], "F2", GB, "B4", nwc, h)
            out_proj(l, 512 if mixer == 2 else 768)

        def gdn(s, l):
            wq, kq = w_cols("w_in", l, BQ, 256)
            wk, kk = w_cols("w_in", l, BK, 256)
            wv, kv = w_cols("w_in", l, BV, 256)
            wg, kg = w_cols("w_in", l, BG, 256)
            wab, kab = w_small(l, BA, 8)
            pb, pk = nb()
            for n in range(32):
                for k in range(8):
                    TE(lambda e, pb=pb, n=n, k=k: e.matmul(pb[0:64, 8 * n:8 * n + 8], lhsT=xnT[:, k, n * 64:(n + 1) * 64], rhs=wab[:, k, :], start=(k == 0), stop=(k == 7)),
                       r=[kab, ("xnT", n // 8)], w=[pk])
            A(lambda e, pb=pb: e.activation(out=ab_all[:], in_=pb[0:64, 0:256], func=AF.Copy), r=[pk], w=["ab_all"])
            X1 = ROPE[0:64, 0:1024].bitcast(F32).rearrange("p (i c) -> p i c", c=64)
            X2 = ROPE[0:64, 1024:2048].bitcast(F32).rearrange("p (i c) -> p i c", c=64)
            Rr = ROPE[0:64, 2048:4096].bitcast(F32).rearrange("p (i c) -> p i c", c=128)
            X3 = ROPE[0:64, 2048:3072].bitcast(F32).rearrange("p (i c) -> p i c", c=64)
            for h in range(4):
                QB, KB, VB, GB, QG = Bf
                for ti, (w_, wk_, dstb, dk_) in enumerate(((wq, kq, QB, "B0"), (wk, kk, KB, "B1"), (wv, kv, VB, "B2"))):
                    for tt in range(4):
                        ts = slice(tt * 512, (tt + 1) * 512)
                        pb, pk = proj64(w_, wk_, h * 64, 64, tt)
                        A(lambda e, pb=pb, ts=ts: e.activation(out=F[0][:, ts], in_=pb[0:64, :], func=AF.Copy), r=[pk], w=["F0"])
                    cj = ti * 4 + h
                    V(lambda e, cj=cj: e.tensor_scalar(out=F[1], in0=F[0], scalar1=gcw[:, l, 3, cj:cj + 1], scalar2=None, op0=ALU.mult), r=["F0", "gcw"], w=["F1"])
                    for sh in (1, 2, 3):
                        V(lambda e, cj=cj, sh=sh: e.scalar_tensor_tensor(out=F[1][:, sh:T], in0=F[0][:, 0:T - sh], scalar=gcw[:, l, 3 - sh, cj:cj + 1], in1=F[1][:, sh:T], op0=ALU.mult, op1=ALU.add), r=["F0", "F1", "gcw"], w=["F1"])
                    if ti == 2:
                        A(lambda e: e.activation(out=VB, in_=F[1], func=AF.Silu), r=["F1"], w=["B2"])
                    else:
                        A(lambda e: e.activation(out=F[1], in_=F[1], func=AF.Silu), r=["F1"], w=["F1"])
                        tmp = reg_bf("TM0")
                        A(lambda e: e.activation(out=tmp, in_=F[1], func=AF.Square), r=["F1"], w=["TM0"])
                        for tt in range(4):
                            ts = slice(tt * 512, (tt + 1) * 512)
                            pb, pk = nb()
                            TE(lambda e, pb=pb, ts=ts: e.matmul(pb[0:64, :], lhsT=onesb[0:64, 0:64], rhs=tmp[:, ts], start=True, stop=True), r=["TM0", "onesb"], w=[pk])
                            A(lambda e, pb=pb: e.activation(out=rsb[0:64, :], in_=pb[0:64, :], func=AF.Ln, scale=1.0, bias=EPS), r=[pk], w=["rsb"])
                            A(lambda e: e.activation(out=rsb[0:64, :], in_=rsb[0:64, :], func=AF.Exp, scale=-0.5), r=["rsb"], w=["rsb"])
                            sc = 0.125 if ti == 0 else 1.0
                            V(lambda e, ts=ts, sc=sc, dstb=dstb: e.scalar_tensor_tensor(out=dstb[:, ts], in0=F[1][:, ts], scalar=sc, in1=rsb[0:64, :], op0=ALU.mult, op1=ALU.mult), r=["F1", "rsb"], w=[dk_])
                for tt in range(4):
                    ts = slice(tt * 512, (tt + 1) * 512)
                    pb, pk = proj64(wg, kg, h * 64, 64, tt)
                    A(lambda e, pb=pb, ts=ts: e.activation(out=GB[:, ts], in_=pb[0:64, :], func=AF.Silu), r=[pk], w=["B3"])
                abv = ab_all[:].rearrange("p (n j h) -> p j h n", j=2, h=4)
                A(lambda e: e.activation(out=small[:, 0:2, :], in_=abv[:, :, h, :], func=AF.Copy), r=["ab_all"], w=["small"])
                gi = l * 4 + h
                A(lambda e: e.activation(out=small[:, 1, :], in_=small[:, 1, :], func=AF.Sigmoid), r=["small"], w=["small"])
                A(lambda e: e.activation(out=small[:, 2, :], in_=small[:, 0, :], func=AF.Exp, bias=gsc[:, 8 + gi:9 + gi]), r=["small", "gsc"], w=["small"])
                A(lambda e: e.activation(out=small[:, 2, :], in_=small[:, 2, :], func=AF.Ln, bias=1.0), r=["small"], w=["small"])
                V(lambda e: e.tensor_scalar(out=small[:, 2, :], in0=small[:, 2, :], scalar1=gsc[:, gi:gi + 1], scalar2=None, op0=ALU.mult), r=["small", "gsc"], w=["small"])
                V(lambda e: e.tensor_scalar(out=small[:, 0, :], in0=small[:, 1, :], scalar1=-1.0, scalar2=None, op0=ALU.mult), r=["small"], w=["small"])
                pb, pk = nb()
                TE(lambda e, pb=pb: e.matmul(pb[0:64, 0:32], lhsT=cst["tri"][:], rhs=small[:, 2, :], start=True, stop=True), r=["small", ("c", "tri")], w=[pk])
                TE(lambda e, pb=pb: e.matmul(pb[0:64, 32:64], lhsT=ones64f[:], rhs=small[:, 2, :], start=True, stop=True), r=["small", "ones64f"], w=[pk])
                A(lambda e, pb=pb: e.activation(out=small[:, 3:5, :], in_=pb[0:64, 0:64].rearrange("p (j n) -> p j n", j=2), func=AF.Copy), r=[pk], w=["small"])
                A(lambda e: e.activation(out=small[:, 5, :], in_=small[:, 3, :], func=AF.Exp), r=["small"], w=["small"])
                V(lambda e: e.tensor_tensor(out=small[:, 6, :], in0=small[:, 1, :], in1=small[:, 5, :], op=ALU.mult), r=["small"], w=["small"])
                V(lambda e: e.tensor_tensor(out=small[:, 7, :], in0=small[:, 4, :], in1=small[:, 3, :], op=ALU.subtract), r=["small"], w=["small"])
                A(lambda e: e.activation(out=small[:, 7, :], in_=small[:, 7, :], func=AF.Exp), r=["small"], w=["small"])
                A(lambda e: e.activation(out=decs_t[:, 0:32], in_=small[:, 4, :], func=AF.Exp), r=["small"], w=["dec"])
                if h == 0:
                    tap(0, QB, ['B0']); tap(1, KB, ['B1']); tap(2, VB, ['B2'])
                    tap(0, small[:].rearrange('p a b -> p (a b)'), ['small'], n=256)
                ktm = to_tm(KB, "B1", TM[0], "TM0", 64, 64)
                vtm = to_tm(VB, "B2", TM[1], "TM1", 64, 64)
                qtm_t = reg_bf("F0")[:, 0:2048]
                qtm = to_tm(QB, "B0", qtm_t, "F0", 64, 64)
                V(lambda e: e.tensor_tensor(out=qtm, in0=qtm, in1=small[:, 5, :].unsqueeze(2).to_broadcast([64, 32, 64]), op=ALU.mult), r=["F0", "small"], w=["F0"])
                for g in range(8):
                    pb, pk = nb()
                    pbb = pb[:].bitcast(BF16)
                    for j in range(4):
                        n = g * 4 + j
                        TE(lambda e, pbb=pbb, j=j, n=n: e.transpose(out=pbb[0:64, j * 64:(j + 1) * 64], in_=qtm[:, n, :], identity=identb[0:64, 0:64]), r=["F0", "identb"], w=[pk])
                    A(lambda e, pbb=pbb, g=g: e.activation(out=QG[:, g * 256:(g + 1) * 256], in_=pbb[0:64, 0:256], func=AF.Copy), r=[pk], w=["B4"])
                Uv = F[1].rearrange("p (n v) -> p n v", v=64)
                WT = VB.rearrange("p (n c) -> p n c", c=64)
                QKT = reg_bf("F0")[:, 2048:4096].rearrange("p (n c) -> p n c", c=64)
                for G in range(4):
                    n0 = G * 8
                    V(lambda e, n0=n0: e.tensor_tensor(out=X1, in0=small[:, 2, n0:n0 + 8].unsqueeze(2).to_broadcast([64, 8, 64]), in1=cst["masku"][:].unsqueeze(1).to_broadcast([64, 8, 64]), op=ALU.mult),
                      r=["small", ("c", "masku")], w=["X1"])
                    X1f = ROPE[0:64, 0:1024].bitcast(F32)
                    pdt, kdt = nb()
                    TE(lambda e, pdt=pdt: e.matmul(pdt[0:64, :], lhsT=cst["tri"][:], rhs=X1f, start=True, stop=False), r=["X1", ("c", "tri")], w=[kdt])
                    for i in range(8):
                        TE(lambda e, pdt=pdt, i=i: e.matmul(pdt[0:64, i * 64:(i + 1) * 64], lhsT=ident64, rhs=cst["negs"][:], start=False, stop=(i == 7)), r=[("c", "negs"), "ident"], w=[kdt])
                    A(lambda e, pdt=pdt: e.activation(out=X2, in_=pdt[0:64, :].rearrange("p (i c) -> p i c", c=64), func=AF.Exp), r=[kdt], w=["X2"])
                    pd, kd = nb()
                    for i in range(8):
                        TE(lambda e, pd=pd, i=i: e.matmul(pd[0:64, i * 64:(i + 1) * 64], lhsT=X1[:, i, :], rhs=cst["tri"][:], start=True, stop=False), r=["X1", ("c", "tri")], w=[kd])
                        TE(lambda e, pd=pd, i=i: e.matmul(pd[0:64, i * 64:(i + 1) * 64], lhsT=ident64, rhs=cst["negi"][:], start=False, stop=True), r=[("c", "negi"), "ident"], w=[kd])
                    A(lambda e, pd=pd: e.activation(out=X3, in_=pd[0:64, :].rearrange("p (i c) -> p i c", c=64), func=AF.Exp), r=[kd], w=["RR"])
                    pkk, kkk = nb()
                    pqk, kqk = nb()
                    for i in range(8):
                        cs = slice((n0 + i) * 64, (n0 + i + 1) * 64)
                        TE(lambda e, pkk=pkk, i=i, cs=cs: e.matmul(pkk[0:64, i * 64:(i + 1) * 64], lhsT=KB[:, cs], rhs=KB[:, cs], start=True, stop=True), r=["B1"], w=[kkk])
                        TE(lambda e, pqk=pqk, i=i, cs=cs: e.matmul(pqk[0:64, i * 64:(i + 1) * 64], lhsT=KB[:, cs], rhs=QB[:, cs], start=True, stop=True), r=["B1", "B0"], w=[kqk])
                    V(lambda e, pkk=pkk: e.tensor_tensor(out=X2, in0=pkk[0:64, :].rearrange("p (i c) -> p i c", c=64), in1=X2, op=ALU.mult), r=[kkk, "X2"], w=["X2"])
                    V(lambda e, n0=n0: e.tensor_tensor(out=X2, in0=X2, in1=small[:, 0, n0:n0 + 8].unsqueeze(2).to_broadcast([64, 8, 64]), op=ALU.mult), r=["X2", "small"], w=["X2"])
                    V(lambda e, pqk=pqk, n0=n0: e.tensor_tensor(out=QKT[:, n0:n0 + 8, :], in0=pqk[0:64, :].rearrange("p (i c) -> p i c", c=64), in1=X3, op=ALU.mult), r=[kqk, "RR"], w=["F0"])
                    pq, kq_ = nb()
                    for i in range(8):
                        TE(lambda e, pq=pq, i=i: e.transpose(out=pq[0:64, i * 64:(i + 1) * 64], in_=X2[:, i, :], identity=ident64), r=["X2", "ident"], w=[kq_])
                    A(lambda e, pq=pq: e.activation(out=X1, in_=pq[0:64, :].rearrange("p (i c) -> p i c", c=64), func=AF.Copy), r=[kq_], w=["X1"])
                    V(lambda e, n0=n0: e.tensor_tensor(out=Rr[:, :, 0:64], in0=vtm[:, n0:n0 + 8, :], in1=small[:, 1, n0:n0 + 8].unsqueeze(2).to_broadcast([64, 8, 64]), op=ALU.mult), r=["TM1", "small"], w=["RR"])
                    V(lambda e, n0=n0: e.tensor_tensor(out=Rr[:, :, 64:128], in0=ktm[:, n0:n0 + 8, :], in1=small[:, 6, n0:n0 + 8].unsqueeze(2).to_broadcast([64, 8, 64]), op=ALU.mult), r=["TM0", "small"], w=["RR"])
                    for lev in range(6):
                        pa, ka = nb()
                        pa2, ka2 = nb()
                        for i in range(8):
                            pp = pa if i < 4 else pa2
                            TE(lambda e, pp=pp, i=i: e.matmul(pp[0:64, (i % 4) * 128:(i % 4 + 1) * 128], lhsT=X1[:, i, :], rhs=Rr[:, i, :], start=True, stop=True), r=["X1", "RR"], w=[ka if i < 4 else ka2])
                        V(lambda e, pa=pa: e.tensor_tensor(out=Rr[:, 0:4, :], in0=Rr[:, 0:4, :], in1=pa[0:64, :].rearrange("p (i c) -> p i c", c=128), op=ALU.add), r=[ka, "RR"], w=["RR"])
                        V(lambda e, pa2=pa2: e.tensor_tensor(out=Rr[:, 4:8, :], in0=Rr[:, 4:8, :], in1=pa2[0:64, :].rearrange("p (i c) -> p i c", c=128), op=ALU.add), r=[ka2, "RR"], w=["RR"])
                        if lev < 5:
                            pp_, kp_ = nb()
                            pq_, kq2 = nb()
                            for i in range(8):
                                TE(lambda e, pp_=pp_, i=i: e.matmul(pp_[0:64, i * 64:(i + 1) * 64], lhsT=X1[:, i, :], rhs=X2[:, i, :], start=True, stop=True), r=["X1", "X2"], w=[kp_])
                                TE(lambda e, pq_=pq_, i=i: e.matmul(pq_[0:64, i * 64:(i + 1) * 64], lhsT=X2[:, i, :], rhs=X1[:, i, :], start=True, stop=True), r=["X1", "X2"], w=[kq2])
                            A(lambda e, pp_=pp_: e.activation(out=X2, in_=pp_[0:64, :].rearrange("p (i c) -> p i c", c=64), func=AF.Copy), r=[kp_], w=["X2"])
                            V(lambda e, pq_=pq_: e.tensor_copy(out=X1, in_=pq_[0:64, :].rearrange("p (i c) -> p i c", c=64)), r=[kq2], w=["X1"])
                    A(lambda e, n0=n0: e.activation(out=Uv[:, n0:n0 + 8, :], in_=Rr[:, :, 0:64], func=AF.Copy), r=["RR"], w=["F1"])
                    pw, kw = nb()
                    for i in range(8):
                        TE(lambda e, pw=pw, i=i: e.transpose(out=pw[0:64, i * 64:(i + 1) * 64], in_=Rr[:, i, 64:128], identity=ident64), r=["RR", "ident"], w=[kw])
                    A(lambda e, pw=pw, n0=n0: e.activation(out=WT[:, n0:n0 + 8, :], in_=pw[0:64, :].rearrange("p (i c) -> p i c", c=64), func=AF.Copy), r=[kw], w=["B2"])
                if h == 0:
                    tap(1, F[1], ['F1']); tap(3, VB, ['B2']); tap(4, reg_bf('F0')[:, 2048:4096], ['F0']); tap(5, QG, ['B4'])
                    tap(6, TM[0], ['TM0']); tap(7, TM[1], ['TM1'])
                V(lambda e: e.tensor_tensor(out=ktm, in0=ktm, in1=small[:, 7, :].unsqueeze(2).to_broadcast([64, 32, 64]), op=ALU.mult), r=["TM0", "small"], w=["TM0"])
                state_init()
                for n in range(32):
                    cs = slice(n * 64, (n + 1) * 64)
                    p1, k1 = nb()
                    TE(lambda e, p1=p1, n=n: e.matmul(p1[0:64, 0:64], lhsT=WT[:, n, :], rhs=Sbf[:], start=True, stop=True), r=["B2", "Sbf"], w=[k1])
                    ub = PTb[n % 2]
                    V(lambda e, p1=p1, n=n, ub=ub: e.tensor_tensor(out=ub[:], in0=Uv[:, n, :], in1=p1[0:64, 0:64], op=ALU.subtract), r=[k1, "F1"], w=[("PTb", n % 2)])
                    p2, k2 = nb()
                    TE(lambda e, p2=p2, ub=ub, n=n: e.matmul(p2[0:64, 0:64], lhsT=ub[:], rhs=QKT[:, n, :], start=True, stop=False), r=[("PTb", n % 2), "F0"], w=[k2])
                    TE(lambda e, p2=p2, cs=cs: e.matmul(p2[0:64, 0:64], lhsT=Sbf[:], rhs=QG[:, cs], start=False, stop=True), r=["Sbf", "B4"], w=[k2])
                    A(lambda e, p2=p2, cs=cs: e.activation(out=F[2][:, cs], in_=p2[0:64, 0:64], func=AF.Copy), r=[k2], w=["F2"])
                    p3, k3 = nb()
                    TE(lambda e, p3=p3, n=n, ub=ub: e.matmul(p3[0:64, 0:64], lhsT=ktm[:, n, :], rhs=ub[:], start=True, stop=True), r=["TM0", ("PTb", n % 2)], w=[k3])
                    state_update(64, p3, k3, decs_t[:, n:n + 1])
                if h == 0:
                    tap(2, F[2], ['F2'])
                headnorm_gate(F[2], "F2", GB, "B3", hw[:, 0 + l:1 + l], h, tmpk="TM1")
            out_proj(l, 256)

        def xattn(s, l):
            QT = SCR[:, 0:16384].rearrange("p (k t) -> p k t", t=T)
            AO = SCR[:, 16384:32768].rearrange("p (k t) -> p k t", t=T)
            PR = SCR[:, 32768:33792].rearrange("p (m t) -> p m t", t=512)
            KTm = ROPE[:, 0:2048].rearrange("p (k m) -> p k m", m=256)
            Vtm = ROPE[:, 2048:4096].rearrange("p (c d) -> p c d", d=D)
            QTK = rk(0, 16384)
            AOK = rk(16384, 16384)
            PRK = rk(32768, 1024)
            specs = [(nm, l, q4 * 256, 256) for nm in ("xattn_wk", "xattn_wv", "xattn_wq", "xattn_wo") for q4 in range(4)]
            loaded = {}
            nxt = [0]

            def getw(i):
                while nxt[0] < len(specs) and nxt[0] <= i + 3:
                    loaded[nxt[0]] = w_cols(*specs[nxt[0]])
                    nxt[0] += 1
                return loaded[i]

            for q4 in range(4):
                wk_, kk_ = getw(q4)
                for j in range(2):
                    mtile = q4 * 2 + j
                    pb, pk = nb()
                    for k in range(8):
                        TE(lambda e, k=k, pb=pb, j=j, wk_=wk_: e.matmul(pb[:, 0:256], lhsT=wk_[:, k, j * 128:(j + 1) * 128], rhs=memT[:, k, :], start=(k == 0), stop=(k == 7)), r=[kk_, "memT"], w=[pk])
                    A(lambda e, pb=pb, mtile=mtile: e.activation(out=KTm[:, mtile, :], in_=pb[:, 0:256], func=AF.Copy), r=[pk], w=["ROPE"])
            for q4 in range(4):
                wv_, kv_ = getw(4 + q4)
                for mc in range(2):
                    pb, pk = nb()
                    for k in range(8):
                        TE(lambda e, k=k, pb=pb, mc=mc, wv_=wv_: e.matmul(pb[:, 0:256], lhsT=memT[:, k, mc * 128:(mc + 1) * 128], rhs=wv_[:, k, :], start=(k == 0), stop=(k == 7)), r=[kv_, "memT"], w=[pk])
                    A(lambda e, pb=pb, mc=mc, q4=q4: e.activation(out=Vtm[:, mc, q4 * 256:(q4 + 1) * 256], in_=pb[:, 0:256], func=AF.Copy), r=[pk], w=["ROPE"])
            for q4 in range(4):
                wq_, kq_ = getw(8 + q4)
                for j in range(2):
                    mt = q4 * 2 + j
                    for tt in range(4):
                        pb, pk = proj64(wq_, kq_, j * 128, 128, tt)
                        A(lambda e, pb=pb, mt=mt, tt=tt: e.activation(out=QT[:, mt, tt * 512:(tt + 1) * 512], in_=pb[:], func=AF.Copy), r=[pk], w=QTK)
            for tt in range(4):
                ts = slice(tt * 512, (tt + 1) * 512)
                for hh in range(4):
                    for mc in range(2):
                        pb, pk = nb()
                        for kk2 in range(2):
                            TE(lambda e, pb=pb, mc=mc, kk2=kk2, hh=hh, ts=ts: e.matmul(pb[:], lhsT=KTm[:, hh * 2 + kk2, mc * 128:(mc + 1) * 128], rhs=QT[:, hh * 2 + kk2, ts], start=(kk2 == 0), stop=(kk2 == 1)),
                               r=["ROPE"] + QTK, w=[pk])
                        A(lambda e, pb=pb, mc=mc: e.activation(out=PR[:, mc, :], in_=pb[:], func=AF.Exp, scale=1.0 / 16), r=[pk], w=PRK)
                    pden, kden = nb()
                    for mc in range(2):
                        TE(lambda e, pden=pden, mc=mc: e.matmul(pden[:], lhsT=onesb[:], rhs=PR[:, mc, :], start=(mc == 0), stop=(mc == 1)), r=PRK + ["onesb"], w=[kden])
                    V(lambda e, pden=pden: e.reciprocal(out=rsb[:], in_=pden[:]), r=[kden], w=["rsb"])
                    for dc in range(2):
                        pb, pk = nb()
                        for mc in range(2):
                            TE(lambda e, pb=pb, mc=mc, dc=dc, hh=hh: e.matmul(pb[:], lhsT=Vtm[:, mc, hh * 256 + dc * 128:hh * 256 + (dc + 1) * 128], rhs=PR[:, mc, :], start=(mc == 0), stop=(mc == 1)),
                               r=["ROPE"] + PRK, w=[pk])
                        V(lambda e, pb=pb, dc=dc, hh=hh, ts=ts: e.tensor_tensor(out=AO[:, hh * 2 + dc, ts], in0=pb[:], in1=rsb[:], op=ALU.mult), r=[pk, "rsb"], w=AOK)
            for q4 in range(4):
                wo_, ko_ = getw(12 + q4)
                for j in range(2):
                    mt = q4 * 2 + j
                    for tt in range(4):
                        ts = slice(tt * 512, (tt + 1) * 512)
                        pb, pk = nb()
                        for k in range(8):
                            TE(lambda e, pb=pb, k=k, j=j, ts=ts, wo_=wo_: e.matmul(pb[:], lhsT=wo_[:, k, j * 128:(j + 1) * 128], rhs=AO[:, k, ts], start=(k == 0), stop=(k == 7)), r=[ko_] + AOK, w=[pk])
                        V(lambda e, pb=pb, mt=mt, ts=ts: e.tensor_tensor(out=hT[:, mt, ts], in0=hT[:, mt, ts], in1=pb[:], op=ALU.add), r=[pk, "hT"], w=["hT"])

        def ffn(s, l):
            UG = SCR[:, 0:4100].bitcast(F32)
            UV = SCR[:, 4352:8452].bitcast(F32)
            CG_ = SCR[:, 8704:12800].bitcast(F32)
            CV_ = SCR[:, 12800:16896].bitcast(F32)
            cw = SCR[:, 21056:21056 + 264].bitcast(F32).rearrange("p (k j) -> p k j", j=44)
            CGb = SCR[:, REG["TM0"][0]:REG["TM0"][0] + 2048]
            CVb = SCR[:, REG["TM1"][0]:REG["TM1"][0] + 2048]
            o2 = REG[("OM", 2)][0]
            ACTs = [SCR[:, 16896:20992].rearrange("p (j t) -> p j t", t=T), SCR[:, o2:o2 + 4096].rearrange("p (j t) -> p j t", t=T)]
            ACKs = [rk(16896, 4096), rk(o2, 4096)]
            UGK, UVK, CGK, CVK, CWK = rk(0, 4100), rk(4352, 4100), rk(8704, 4096), rk(12800, 4096), rk(21056, 264)
            for k3 in range(3):
                DS(lambda e, k3=k3: e.dma_start(out=cw[:, k3, :], in_=dr["ffn_conv_w"][l, k3].rearrange("(j p) -> p j", p=128)), w=CWK)
            loaded = {}
            dn_bufs = []
            for key in (("OM", 0), ("OM", 1)):
                o_, m_ = REG[key]
                dn_bufs.append((SCR[:, o_:o_ + m_], key))

            def issue_up(g):
                res = []
                for i_, c0 in enumerate((g * 256, DFF + g * 256)):
                    si = 2 * (g % 2) + i_
                    buf, key = W[:, si, :], ("W", si)
                    dst = buf.rearrange("p (k m) -> p k m", m=256)
                    src = dr["ffn_up"][l][:, c0:c0 + 256].rearrange("(k p) m -> p k m", p=128)
                    DG_(lambda e, dst=dst, src=src: e.dma_start(out=dst, in_=src), w=[key])
                    res.append((dst, key))
                loaded[g] = res

            def issue_dn(g):
                buf, key = dn_bufs[g % 2]
                dst = buf.rearrange("p (j m) -> p j m", m=1024)
                src = dr["ffn_down"][l][g * 256:(g + 1) * 256, :].rearrange("(j p) m -> p j m", p=128)
                DG_(lambda e, dst=dst, src=src: e.dma_start(out=dst, in_=src), w=[key])
                loaded[g].append((dst, key))

            TK = lambda nm: [(nm, tt) for tt in range(4)]
            ALLT = TK("UG") + TK("UV") + TK("CG") + TK("CV") + TK("CGb") + TK("CVb") + [("ACTt", bi, j, tt) for bi in range(2) for j in range(2) for tt in range(4)]
            ALLR = sorted(set(map(repr, UGK + UVK + CGK + CVK + CWK + ["TM0", "TM1"] + ACKs[0] + ACKs[1])))
            ALLRK = []
            for k_ in UGK + UVK + CGK + CVK + CWK + ["TM0", "TM1"] + ACKs[0] + ACKs[1]:
                if k_ not in ALLRK:
                    ALLRK.append(k_)
            V(lambda e: e.memset(UG[:, 0:2], 0.0), r=ALLRK, w=ALLRK + ALLT)
            V(lambda e: e.memset(UV[:, 0:2], 0.0), w=[("UV", 0)])

            def up(g):
                (wg_, kg_), (wv_, kv_) = loaded[g][0], loaded[g][1]
                ACT = ACTs[g % 2]
                bi = g % 2
                for j in range(2):
                    ch = g * 2 + j
                    cv = 22 + ch
                    for tt in range(4):
                        c0 = tt * 512
                        hk = [("UG", tt)] + ([("UG", tt - 1)] if tt > 0 else [])
                        hv = [("UV", tt)] + ([("UV", tt - 1)] if tt > 0 else [])
                        pb, pk = proj64(wg_, kg_, j * 128, 128, tt)
                        A(lambda e, pb=pb, c0=c0: e.activation(out=UG[:, 2 + c0:2 + c0 + 512], in_=pb[:], func=AF.Copy), r=[pk], w=[("UG", tt)])
                        pb, pk = proj64(wv_, kv_, j * 128, 128, tt)
                        A(lambda e, pb=pb, c0=c0: e.activation(out=UV[:, 2 + c0:2 + c0 + 512], in_=pb[:], func=AF.Copy), r=[pk], w=[("UV", tt)])
                        A(lambda e, ch=ch, c0=c0: e.activation(out=CG_[:, c0:c0 + 512], in_=UG[:, c0:c0 + 512], func=AF.Copy, scale=cw[:, 0, ch:ch + 1]), r=hk + CWK, w=[("CG", tt)])
                        A(lambda e, cv=cv, c0=c0: e.activation(out=CV_[:, c0:c0 + 512], in_=UV[:, c0:c0 + 512], func=AF.Copy, scale=cw[:, 0, cv:cv + 1]), r=hv + CWK, w=[("CV", tt)])
                        V(lambda e, ch=ch, c0=c0: e.scalar_tensor_tensor(out=CG_[:, c0:c0 + 512], in0=UG[:, 1 + c0:1 + c0 + 512], scalar=cw[:, 1, ch:ch + 1], in1=CG_[:, c0:c0 + 512], op0=ALU.mult, op1=ALU.add), r=hk + CWK + [("CG", tt)], w=[("CG", tt)])
                        V(lambda e, ch=ch, c0=c0: e.scalar_tensor_tensor(out=CG_[:, c0:c0 + 512], in0=UG[:, 2 + c0:2 + c0 + 512], scalar=cw[:, 2, ch:ch + 1], in1=CG_[:, c0:c0 + 512], op0=ALU.mult, op1=ALU.add), r=hk + CWK + [("CG", tt)], w=[("CG", tt)])
                        A(lambda e, c0=c0: e.activation(out=CGb[:, c0:c0 + 512], in_=CG_[:, c0:c0 + 512], func=AF.Silu), r=[("CG", tt)], w=[("CGb", tt)])
                        V(lambda e, cv=cv, c0=c0: e.scalar_tensor_tensor(out=CV_[:, c0:c0 + 512], in0=UV[:, 1 + c0:1 + c0 + 512], scalar=cw[:, 1, cv:cv + 1], in1=CV_[:, c0:c0 + 512], op0=ALU.mult, op1=ALU.add), r=hv + CWK + [("CV", tt)], w=[("CV", tt)])
                        V(lambda e, cv=cv, c0=c0: e.scalar_tensor_tensor(out=CVb[:, c0:c0 + 512], in0=UV[:, 2 + c0:2 + c0 + 512], scalar=cw[:, 2, cv:cv + 1], in1=CV_[:, c0:c0 + 512], op0=ALU.mult, op1=ALU.add), r=hv + CWK + [("CV", tt)], w=[("CVb", tt)])
                        V(lambda e, j=j, ACT=ACT, c0=c0: e.tensor_tensor(out=ACT[:, j, c0:c0 + 512], in0=CGb[:, c0:c0 + 512], in1=CVb[:, c0:c0 + 512], op=ALU.mult), r=[("CGb", tt), ("CVb", tt)], w=[("ACTt", bi, j, tt)])

            def down(g):
                (wd_, kd_) = loaded[g][2]
                ACT, ACK = ACTs[g % 2], ACKs[g % 2]
                for mt in range(8):
                    for tt in range(4):
                        ts = slice(tt * 512, (tt + 1) * 512)
                        pb, pk = nb()
                        for j in range(2):
                            TE(lambda e, pb=pb, j=j, mt=mt, ts=ts, wd_=wd_, ACT=ACT: e.matmul(pb[:], lhsT=wd_[:, j, mt * 128:(mt + 1) * 128], rhs=ACT[:, j, ts], start=(j == 0), stop=(j == 1)), r=[kd_, ("ACTt", g % 2, j, tt)], w=[pk])
                        V(lambda e, pb=pb, mt=mt, ts=ts: e.tensor_tensor(out=hT[:, mt, ts], in0=hT[:, mt, ts], in1=pb[:], op=ALU.add), r=[pk, "hT"], w=["hT"])

            issue_up(0)
            issue_dn(0)
            issue_up(1)
            issue_dn(1)
            for g in range(11):
                up(g)
                if g > 0:
                    down(g - 1)
                    if g + 1 < 11:
                        issue_dn(g + 1)
                if g + 2 < 11:
                    issue_up(g + 2)
            down(10)
            V(lambda e: e.memset(UG[:, 0:2], 0.0), r=ALLT, w=ALLRK + ALLT)

        XNK = [("xnT", i) for i in range(4)]
        xs = xnT[:].rearrange("p k t -> p (k t)").bitcast(F32)
        out_ops = []
        for s in (seqs if seqs is not None else range(NSEQ)):
            for t16 in range(16):
                xt_ = xs[:, (t16 % 4) * 1024:(t16 % 4 + 1) * 1024]
                DS(lambda e, xt_=xt_, t16=t16: e.dma_start(out=xt_, in_=dr["x"][s, t16 * 128:(t16 + 1) * 128, :]), w=XNK)
                for g in range(2):
                    pb, pk = nb()
                    for j in range(4):
                        k = g * 4 + j
                        TE(lambda e, pb=pb, j=j, k=k, xt_=xt_: e.transpose(out=pb[:, j * 128:(j + 1) * 128], in_=xt_[:, k * 128:(k + 1) * 128], identity=ident[:]), r=XNK + ["ident"], w=[pk])
                    A(lambda e, pb=pb, g=g, t16=t16: e.activation(out=hT[:, g * 4:(g + 1) * 4, t16 * 128:(t16 + 1) * 128], in_=pb[:].rearrange("p (k t) -> p k t", t=128), func=AF.Copy), r=[pk], w=["hT"])
            for mt in range(2):
                mt_ = xs[:, 4096 + mt * 1024:4096 + (mt + 1) * 1024]
                DS(lambda e, mt_=mt_, mt=mt: e.dma_start(out=mt_, in_=dr["mem"][s, mt * 128:(mt + 1) * 128, :]), w=XNK)
                A(lambda e, mt_=mt_: e.activation(out=xs[:, 6144:7168], in_=mt_, func=AF.Square, accum_out=rsb[:, 0:1]), r=XNK, w=XNK + ["rsb"])
                A(lambda e: e.activation(out=rsb[:, 1:2], in_=rsb[:, 0:1], func=AF.Sqrt, scale=1.0 / D, bias=EPS), r=["rsb"], w=["rsb"])
                V(lambda e: e.reciprocal(out=rsb[:, 2:3], in_=rsb[:, 1:2]), r=["rsb"], w=["rsb"])
                V(lambda e, mt_=mt_: e.tensor_scalar(out=mt_, in0=mt_, scalar1=rsb[:, 2:3], scalar2=None, op0=ALU.mult), r=XNK + ["rsb"], w=XNK)
                for g in range(2):
                    pb, pk = nb()
                    for j in range(4):
                        k = g * 4 + j
                        TE(lambda e, pb=pb, j=j, k=k, mt_=mt_: e.transpose(out=pb[:, j * 128:(j + 1) * 128], in_=mt_[:, k * 128:(k + 1) * 128], identity=ident[:]), r=XNK + ["ident"], w=[pk])
                    for j in range(4):
                        k = g * 4 + j
                        V(lambda e, pb=pb, j=j, k=k, mt=mt: e.tensor_scalar(out=memT[:, k, mt * 128:(mt + 1) * 128], in0=pb[:, j * 128:(j + 1) * 128], scalar1=memnw[:, k:k + 1], scalar2=None, op0=ALU.mult),
                          r=[pk, "memnw"], w=["memT"])
            for l in (layers if layers is not None else range(NLAYER)):
                if "mix" in stages:
                    norm(0 + l)
                    if 0 in mixers:
                        retention(s, l)
                    if 1 in mixers:
                        gdn(s, l)
                    if 2 in mixers:
                        diag_gated(s, l, 2)
                    if 3 in mixers:
                        diag_gated(s, l, 3)
                if "xat" in stages:
                    norm(2 + l)
                    xattn(s, l)
                if "ffn" in stages:
                    norm(4 + l)
                    ffn(s, l)
            yn = xs[:, 0:4096].rearrange("p (k t) -> p k t", t=512)
            for tt in range(4):
                ts = slice(tt * 512, (tt + 1) * 512)
                A(lambda e, ts=ts: e.activation(out=sq, in_=hT[:, :, ts], func=AF.Square), r=["hT"], w=SQK)
                pb, pk = nb()
                for k in range(8):
                    TE(lambda e, k=k, pb=pb: e.matmul(pb[:], lhsT=onesb[:], rhs=sq[:, k, :], start=(k == 0), stop=(k == 7)), r=SQK + ["onesb"], w=[pk])
                A(lambda e, pb=pb: e.activation(out=rsb[:], in_=pb[:], func=AF.Ln, scale=1.0 / D, bias=EPS), r=[pk], w=["rsb"])
                A(lambda e: e.activation(out=rsb[:], in_=rsb[:], func=AF.Exp, scale=-0.5), r=["rsb"], w=["rsb"])
                for k in range(8):
                    V(lambda e, k=k, ts=ts: e.scalar_tensor_tensor(out=yn[:, k, :], in0=hT[:, k, ts], scalar=normw[:, 6, k:k + 1], in1=rsb[:], op0=ALU.mult, op1=ALU.mult), r=["hT", "rsb", "normw"], w=XNK)
                for t4 in range(4):
                    stg = xs[:, 4096 + t4 * 1024:4096 + (t4 + 1) * 1024]
                    for g in range(2):
                        pb, pk = nb()
                        for j in range(4):
                            k = g * 4 + j
                            TE(lambda e, pb=pb, j=j, k=k, t4=t4: e.transpose(out=pb[:, j * 128:(j + 1) * 128], in_=yn[:, k, t4 * 128:(t4 + 1) * 128], identity=ident[:]), r=XNK + ["ident"], w=[pk])
                        A(lambda e, pb=pb, g=g, stg=stg: e.activation(out=stg[:, g * 512:(g + 1) * 512], in_=pb[:], func=AF.Copy), r=[pk], w=XNK)
                    t0 = tt * 512 + t4 * 128
                    out_ops.append(DS(lambda e, stg=stg, t0=t0: e.dma_start(out=y_d[s, t0:t0 + 128, :], in_=stg), r=XNK))
        P.emit(final_wait_ops=out_ops[-8:] + tap_ops)
    return nc


_CACHE = {}


def kernel(**inputs):
    if "nc" not in _CACHE:
        _CACHE["nc"] = build()
    nc = _CACHE["nc"]
    consts = make_consts()
    in_maps = []
    for c in range(8):
        m = {}
        m["x"] = np.ascontiguousarray(inputs["x"][2 * c:2 * c + 2]).astype(np.float32, copy=False)
        m["mem"] = np.ascontiguousarray(inputs["mem"][2 * c:2 * c + 2]).astype(np.float32, copy=False)
        m["positions"] = np.ascontiguousarray(inputs["positions"][2 * c:2 * c + 2]).astype(np.int32, copy=False)
        for k in WEIGHTS:
            m[k] = np.ascontiguousarray(np.asarray(inputs[k], dtype=np.float32))
        for k in CONST_SHAPES:
            m["c_" + k] = np.ascontiguousarray(consts[k])
        in_maps.append(m)
    res = run_bass_kernel_spmd(nc, in_maps, core_ids=list(range(8)))
    return np.concatenate([np.asarray(r["y"]) for r in res.results], axis=0)
```

```python
import contextlib
import math
import types
import numpy as np
import concourse.bass as bass
import concourse.mybir as mybir
from concourse.bass_utils import run_bass_kernel_spmd

F32 = mybir.dt.float32
BF16 = mybir.dt.bfloat16
I32 = mybir.dt.int32
AF = mybir.ActivationFunctionType
ALU = mybir.AluOpType

SEM_LIMIT = 30000
T = 2048
D = 1024
DFF = 2816
EPS = 1e-6
NEG = -30000.0


class Prog:
    ENGS = ("tensor", "vector", "scalar", "gpsimd", "sync")

    def __init__(self, nc):
        self.nc = nc
        self.ops = []
        self.last_w = {}
        self.readers = {}

    @staticmethod
    def _freeze(fn):
        if fn.__closure__ is None:
            return fn
        cells = []
        for c in fn.__closure__:
            try:
                cells.append(types.CellType(c.cell_contents))
            except ValueError:
                cells.append(c)
        return types.FunctionType(fn.__code__, fn.__globals__, fn.__name__, fn.__defaults__, tuple(cells))

    def op(self, eng, fn, reads=(), writes=(), dma=False):
        fn = self._freeze(fn)
        i = len(self.ops)
        deps = set()
        for k in reads:
            w = self.last_w.get(k)
            if w is not None:
                deps.add(w)
        for k in writes:
            w = self.last_w.get(k)
            if w is not None:
                deps.add(w)
            for r in self.readers.get(k, ()):
                deps.add(r)
        deps.discard(i)
        for k in reads:
            self.readers.setdefault(k, []).append(i)
        for k in writes:
            self.last_w[k] = i
            self.readers[k] = []
        self.ops.append(dict(eng=eng, fn=fn, deps=deps, dma=dma))
        return i

    def emit(self, final_wait_ops=()):
        nc = self.nc
        ops = self.ops
        needed = set()
        for o in ops:
            for d in o["deps"]:
                if ops[d]["eng"] == "tensor" and o["eng"] == "tensor" and not ops[d]["dma"] and not o["dma"]:
                    continue
                needed.add(d)
        for d in final_wait_ops:
            needed.add(d)
        DR = 8
        counters = {}
        sig = {}
        for i, o in enumerate(ops):
            if o["dma"]:
                n = counters.get(("dma", o["eng"]), 0)
                counters[("dma", o["eng"])] = n + 1
                sig[i] = (("d", o["eng"], n % DR), n // DR + 1, 16)
                continue
            if i not in needed:
                continue
            c = counters.get(o["eng"], 0)
            counters[o["eng"]] = c + 1
            sig[i] = (("c", o["eng"], c // SEM_LIMIT), c % SEM_LIMIT + 1, 1)
        with contextlib.ExitStack() as es:
            sems = {}
            for i in sorted(sig):
                key = sig[i][0]
                if key not in sems:
                    sems[key] = es.enter_context(nc.semaphore("s_%s_%s_%d" % key))
            block = es.enter_context(nc.Block())

            def make_body(engname):
                def body(e):
                    waited = {}

                    def wait(key, v, mult):
                        if waited.get(key, 0) >= v:
                            return
                        e.wait_ge(sems[key], v * mult)
                        waited[key] = v
                        if key[0] == "c":
                            for jj in range(key[2]):
                                waited[(key[0], key[1], jj)] = SEM_LIMIT

                    for i, o in enumerate(ops):
                        if o["eng"] != engname:
                            continue
                        for d in sorted(o["deps"]):
                            if d not in sig:
                                continue
                            if ops[d]["eng"] == "tensor" and engname == "tensor" and not ops[d]["dma"] and not o["dma"]:
                                continue
                            key, v, mult = sig[d]
                            wait(key, v, mult)
                        if o["dma"]:
                            key, v, mult = sig[i]
                            if v > 1:
                                wait(key, v - 1, mult)
                        ins = o["fn"](e)
                        if i in sig:
                            key, v, mult = sig[i]
                            ins.then_inc(sems[key], mult)
                    if engname == "sync":
                        for d in final_wait_ops:
                            key, v, mult = sig[d]
                            wait(key, v, mult)
                return body

            for engname in self.ENGS:
                if any(o["eng"] == engname for o in ops) or engname == "sync":
                    getattr(block, engname)(make_body(engname))


def make_consts():
    c = {}
    c["ident"] = np.eye(128, dtype=np.float32)
    j = np.arange(64)
    c["tri"] = (j[:, None] <= j[None, :]).astype(np.float32)
    c["masku"] = (j[:, None] > j[None, :]).astype(np.float32)
    c["negs"] = np.where(j[None, :] >= j[:, None], NEG, 0.0).astype(np.float32)
    c["negi"] = np.where(j[:, None] > j[None, :], NEG, 0.0).astype(np.float32)
    c["caus32"] = ((j[:, None] % 32) <= j[None, :32]).astype(np.float32)
    gam = 1.0 - 2.0 ** (-5.0 - np.arange(4))
    lg = np.log(gam)
    maskr = np.zeros((64, 4, 64), np.float64)
    gq = np.zeros((64, 4, 64), np.float64)
    gk = np.zeros((64, 4), np.float64)
    for h in range(4):
        rel = j[None, :] - j[:, None]
        maskr[:, h, :] = np.where(rel >= 0, np.exp(lg[h] * np.maximum(rel, 0)), 0.0) / 8.0
        gq[:, h, :] = np.exp(lg[h] * (j[None, :] + 1.0))
        gk[:, h] = np.exp(lg[h] * (63.0 - j)) / 8.0
    c["maskr"] = maskr.astype(np.float32)
    c["gq"] = gq.astype(np.float32)
    c["gk"] = gk.astype(np.float32)
    c["retdec"] = [float(np.exp(lg[h] * 64.0)) for h in range(4)]
    p = np.arange(64)
    freq = 10000.0 ** (-(np.arange(0, 64, 2, dtype=np.float32)) / 64.0)
    c["freq"] = freq.astype(np.float32)[p % 32].reshape(64, 1).astype(np.float32)
    c["sgn"] = np.where(p < 32, -1.0, 1.0).reshape(64, 1).astype(np.float32)
    perm = np.zeros((64, 64), np.float32)
    perm[(p + 32) % 64, p] = 1.0
    c["perm"] = perm
    t = np.arange(T)
    c["rst"] = np.stack([(t % 64 != 0), (t % 32 != 0)]).astype(np.float32)
    return c


CONST_SHAPES = dict(ident=[128, 128], tri=[64, 64], masku=[64, 64], negs=[64, 64], negi=[64, 64],
                    caus32=[64, 32], maskr=[64, 4, 64], gq=[64, 4, 64], gk=[64, 4], freq=[64, 1],
                    sgn=[64, 1], perm=[64, 64], rst=[2, T])

WEIGHTS = dict(
    mix_norm_w=[2, D], w_in=[2, D, 3864], gdn_conv_w=[2, 4, 768], gdn_a_log=[2, 4], gdn_dt_bias=[2, 4],
    gdn_norm_w=[2, 64], gla_gk_up=[2, 16, 128], gla_gk_bias=[2, 128], gla_norm_w=[2, 64],
    hgrn_lb_logits=[2, 256], hgrn_norm_w=[2, 64], w_out=[2, D, D], xattn_norm_w=[2, D], mem_norm_w=[D],
    xattn_wq=[2, D, D], xattn_wk=[2, D, D], xattn_wv=[2, D, D], xattn_wo=[2, D, D], ffn_norm_w=[2, D],
    ffn_up=[2, D, 2 * DFF], ffn_conv_w=[2, 3, 2 * DFF], ffn_down=[2, DFF, D], final_norm_w=[D])

RQ, RK, RV, RG = 0, 256, 512, 768
BQ, BK, BV, BA, BB, BG = 1024, 1280, 1536, 1792, 1796, 1800
CQ, CK, CV, CLR, CG = 2056, 2184, 2312, 2568, 2584
DQ, DF_, DI, DG = 2840, 3096, 3352, 3608


def build(NSEQ=2, NLAYER=2, stages=("mix", "xat", "ffn"), mixers=(0, 1, 2, 3), dbg=False, seqs=None, layers=None):
    nc = bass.Bass("TRN2", target_bir_lowering=False)
    dr = {}
    dr["x"] = nc.dram_tensor("x", [2, T, D], F32, kind="ExternalInput").ap()
    dr["mem"] = nc.dram_tensor("mem", [2, 256, D], F32, kind="ExternalInput").ap()
    dr["positions"] = nc.dram_tensor("positions", [2, T], I32, kind="ExternalInput").ap()
    for k, shp in WEIGHTS.items():
        dr[k] = nc.dram_tensor(k, shp, F32, kind="ExternalInput").ap()
    for k, shp in CONST_SHAPES.items():
        dr["c_" + k] = nc.dram_tensor("c_" + k, shp, F32, kind="ExternalInput").ap()
    y_d = nc.dram_tensor("y", [2, T, D], F32, kind="ExternalOutput").ap()
    if dbg:
        dbg_f = nc.dram_tensor("dbg_f", [64, 8, T], F32, kind="ExternalOutput").ap()
        dbg_b = nc.dram_tensor("dbg_b", [64, 8, T], BF16, kind="ExternalOutput").ap()
    C = make_consts()

    with contextlib.ExitStack() as es:
        def sb(name, shape, dt):
            return es.enter_context(nc.sbuf_tensor(name, shape, dt))

        P = Prog(nc)
        es.enter_context(nc.allow_non_contiguous_dma(reason='small strided parameter loads'))

        def V(fn, r=(), w=()):
            return P.op("vector", fn, r, w)

        def A(fn, r=(), w=()):
            return P.op("scalar", fn, r, w)

        def TE(fn, r=(), w=()):
            return P.op("tensor", fn, r, w)

        def GP(fn, r=(), w=()):
            return P.op("gpsimd", fn, r, w)

        def DS(fn, r=(), w=()):
            return P.op("sync", fn, r, w, dma=True)

        def DG_(fn, r=(), w=()):
            return P.op("gpsimd", fn, r, w, dma=True)

        tap_ops = []

        def tap(idx, ap, keys, n=T):
            if not dbg:
                return
            dst = (dbg_f if ap.dtype == F32 else dbg_b)[0:ap.shape[0], idx, 0:n]
            tap_ops.append(DS(lambda e: e.dma_start(out=dst, in_=ap), r=keys))

        banks = [es.enter_context(nc.psum_tensor(f"pb{i}", [128, 512], F32)) for i in range(8)]
        bank_ctr = [0]

        def nb():
            i = bank_ctr[0] % 8
            bank_ctr[0] += 1
            return banks[i], ("pb", i)

        hT = sb("hT", [128, 8, T], F32)
        xnT = sb("xnT", [128, 8, T], BF16)
        NSLOT = 4
        W = sb("W", [128, NSLOT, 2048], BF16)
        slot_ctr = [0]

        def wslot():
            i = slot_ctr[0] % NSLOT
            slot_ctr[0] += 1
            return i, ("W", i)

        REG = {"F0": (0, 4096), "F1": (4096, 4096), "F2": (8192, 4096)}
        for i in range(5):
            REG["B%d" % i] = (12288 + i * 2048, 2048)
        REG["TM0"] = (22528, 2048)
        REG["TM1"] = (24576, 2048)
        for h in range(4):
            REG[("OM", h)] = (26624 + h * 2048, 2048)
        SCRN = 34816
        SCR = sb("SCR", [128, SCRN], BF16)
        ROPE = sb("ROPE", [128, 4096], BF16)

        def rk(off, n):
            return [k for k, (o, m) in REG.items() if o < off + n and off < o + m]

        def reg_bf(key, parts=64):
            o, m = REG[key]
            return SCR[0:parts, o:o + m]

        def reg_f32(key, parts=64):
            o, m = REG[key]
            return SCR[0:parts, o:o + m].bitcast(F32)

        F = [reg_f32("F%d" % i) for i in range(3)]
        Bf = [reg_bf("B%d" % i) for i in range(5)]
        TM = [reg_bf("TM0"), reg_bf("TM1")]
        OMv = [reg_bf(("OM", h)) for h in range(4)]
        cosT = ROPE[0:64, 0:2048]
        sinS = ROPE[0:64, 2048:4096]
        sq = SCR[:, 26624:26624 + 4096].rearrange("p (k t) -> p k t", t=512)
        SQK = [("OM", 0), ("OM", 1)]
        rsb = sb("rsb", [128, 512], F32)
        ident = sb("ident", [128, 128], F32)
        identb = sb("identb", [128, 128], BF16)
        onesb = sb("onesb", [128, 128], BF16)
        ones64f = sb("ones64f", [64, 64], F32)
        onecol = sb("onecol", [64, 1], F32)
        cst = {}
        for k in ("tri", "masku", "negs", "negi", "caus32", "maskr", "gq", "gk", "freq", "sgn", "perm"):
            cst[k] = sb("k_" + k, CONST_SHAPES[k], F32)
        permb = sb("permb", [64, 64], BF16)
        ident64 = ident[0:64, 0:64]
        normw = sb("normw", [128, 7, 8], F32)
        memnw = sb("memnw", [128, 8], F32)
        hw = sb("hw", [64, 16], F32)
        gsc = sb("gsc", [64, 24], F32)
        glab = sb("glab", [32, 8], F32)
        glup = sb("glup", [16, 2, 128], F32)
        gcw = sb("gcw", [64, 2, 4, 12], F32)
        S32 = sb("S32", [64, 64], F32)
        ab_all = sb("ab_all", [64, 256], F32)
        Sbz = sb("Sbz", [64, 64], BF16)
        Sbf = sb("Sbf", [64, 64], BF16)
        PTb = [sb(f"PTb{i}", [64, 64], BF16) for i in range(2)]
        small = sb("small", [64, 8, 32], F32)
        decs_t = sb("decs_t", [64, 64], F32)
        memT = sb("memT", [128, 8, 256], BF16)
        wsmall = sb("wsmall", [128, 8, 16], BF16)

        for k in cst:
            DS(lambda e, k=k: e.dma_start(out=cst[k][:], in_=dr["c_" + k]), w=[("c", k)])
        DS(lambda e: e.dma_start(out=ident[:], in_=dr["c_ident"]), w=["ident"])
        V(lambda e: e.tensor_copy(out=identb[:], in_=ident[:]), r=["ident"], w=["identb"])
        V(lambda e: e.memset(onesb[:], 1.0), w=["onesb"])
        V(lambda e: e.memset(ones64f[:], 1.0), w=["ones64f"])
        V(lambda e: e.memset(onecol[:], 1.0), w=["onecol"])
        V(lambda e: e.memset(Sbz[:], 0.0), w=["Sbf"])
        V(lambda e: e.tensor_copy(out=permb[:], in_=cst["perm"][:]), r=[("c", "perm")], w=["permb"])
        for a, nm in enumerate(["mix_norm_w", "xattn_norm_w", "ffn_norm_w"]):
            for l in range(2):
                DS(lambda e, a=a, l=l, nm=nm: e.dma_start(out=normw[:, 2 * a + l, :], in_=dr[nm][l].rearrange("(k p) -> p k", p=128)), w=["normw"])
        DS(lambda e: e.dma_start(out=normw[:, 6, :], in_=dr["final_norm_w"].rearrange("(k p) -> p k", p=128)), w=["normw"])
        DS(lambda e: e.dma_start(out=memnw[:], in_=dr["mem_norm_w"].rearrange("(k p) -> p k", p=128)), w=["memnw"])
        for a, nm in enumerate(["gdn_norm_w", "gla_norm_w", "hgrn_norm_w"]):
            DS(lambda e, a=a, nm=nm: e.dma_start(out=hw[:, 2 * a:2 * a + 2], in_=dr[nm].rearrange("l p -> p l")), w=["hw"])
        for l in range(2):
            DS(lambda e, l=l: e.dma_start(out=gsc[:, 16 + 4 * l:20 + 4 * l], in_=dr["hgrn_lb_logits"][l].rearrange("(h p) -> p h", p=64)), w=["gsc"])
        A(lambda e: e.activation(out=gsc[:, 16:24], in_=gsc[:, 16:24], func=AF.Exp), r=["gsc"], w=["gsc"])
        V(lambda e: e.tensor_tensor(out=hw[:, 10:14], in0=gsc[:, 16:20], in1=gsc[:, 20:24], op=ALU.add), r=["gsc"], w=["hw"])
        V(lambda e: e.reciprocal(out=hw[:, 10:14], in_=hw[:, 10:14]), r=["hw"], w=["hw"])
        V(lambda e: e.tensor_tensor(out=hw[:, 6:10], in0=gsc[:, 20:24], in1=hw[:, 10:14], op=ALU.mult), r=["hw", "gsc"], w=["hw"])
        V(lambda e: e.tensor_tensor(out=hw[:, 10:14], in0=gsc[:, 16:20], in1=hw[:, 10:14], op=ALU.mult), r=["hw", "gsc"], w=["hw"])
        V(lambda e: e.memset(hw[:, 14:15], 0.0), w=["hw"])
        V(lambda e: e.memset(hw[:, 15:16], 1.0), w=["hw"])
        DS(lambda e: e.dma_start(out=gsc[:, 0:8], in_=dr["gdn_a_log"].rearrange("l h -> (l h)").unsqueeze(0).broadcast_to([64, 8])), w=["gsc"])
        DS(lambda e: e.dma_start(out=gsc[:, 8:16], in_=dr["gdn_dt_bias"].rearrange("l h -> (l h)").unsqueeze(0).broadcast_to([64, 8])), w=["gsc"])
        A(lambda e: e.activation(out=gsc[:, 0:8], in_=gsc[:, 0:8], func=AF.Exp), r=["gsc"], w=["gsc"])
        V(lambda e: e.tensor_scalar(out=gsc[:, 0:8], in0=gsc[:, 0:8], scalar1=-1.0, scalar2=None, op0=ALU.mult), r=["gsc"], w=["gsc"])
        for l in range(2):
            DS(lambda e, l=l: e.dma_start(out=glab[:, 4 * l:4 * l + 4], in_=dr["gla_gk_bias"][l].rearrange("(h p) -> p h", p=32)), w=["glab"])
            DS(lambda e, l=l: e.dma_start(out=glup[:, l, :], in_=dr["gla_gk_up"][l]), w=["glup"])
            for k4 in range(4):
                DS(lambda e, l=l, k4=k4: e.dma_start(out=gcw[:, l, k4, :], in_=dr["gdn_conv_w"][l, k4].rearrange("(j p) -> p j", p=64)), w=["gcw"])
        V(lambda e: e.tensor_scalar(out=glab[:], in0=glab[:], scalar1=-1.0, scalar2=None, op0=ALU.mult), r=["glab"], w=["glab"])

        def w_cols(name, l, c0, n):
            src = dr[name][l][:, c0:c0 + n].rearrange("(k p) m -> p k m", p=128)
            i, key = wslot()
            dst = W[:, i, 0:8 * n].rearrange("p (k m) -> p k m", m=n)
            DG_(lambda e, dst=dst, src=src: e.dma_start(out=dst, in_=src), w=[key])
            return dst, key

        def w_small(l, c0, n):
            src = dr["w_in"][l][:, c0:c0 + n].rearrange("(k p) m -> p k m", p=128)
            dst = wsmall[:, :, 0:n]
            DG_(lambda e: e.dma_start(out=dst, in_=src), w=["wsmall"])
            return dst, "wsmall"

        def norm(widx):
            for tt in range(4):
                ts = slice(tt * 512, (tt + 1) * 512)
                A(lambda e, ts=ts: e.activation(out=sq, in_=hT[:, :, ts], func=AF.Square), r=["hT"], w=SQK)
                pb, pk = nb()
                for k in range(8):
                    TE(lambda e, k=k, pb=pb: e.matmul(pb[:], lhsT=onesb[:], rhs=sq[:, k, :], start=(k == 0), stop=(k == 7)), r=SQK + ["onesb"], w=[pk])
                A(lambda e, pb=pb: e.activation(out=rsb[:], in_=pb[:], func=AF.Ln, scale=1.0 / D, bias=EPS), r=[pk], w=["rsb"])
                A(lambda e: e.activation(out=rsb[:], in_=rsb[:], func=AF.Exp, scale=-0.5), r=["rsb"], w=["rsb"])
                for k in range(8):
                    V(lambda e, k=k, ts=ts: e.scalar_tensor_tensor(out=xnT[:, k, ts], in0=hT[:, k, ts], scalar=normw[:, widx, k:k + 1], in1=rsb[:], op0=ALU.mult, op1=ALU.mult),
                      r=["hT", "rsb", "normw"], w=[("xnT", tt)])

        def proj64(wv, wkey, c0, M, tt):
            pb, pk = nb()
            for k in range(8):
                TE(lambda e, k=k, pb=pb: e.matmul(pb[0:M, :], lhsT=wv[:, k, c0:c0 + M], rhs=xnT[:, k, tt * 512:(tt + 1) * 512], start=(k == 0), stop=(k == 7)),
                   r=[wkey, ("xnT", tt)], w=[pk])
            return pb, pk

        def to_tm(src, skey, dst, dkey, Cc, Dd, scale_ap=None):
            nch = T // Cc
            per = 1024 // Dd
            dv = dst[0:Cc, 0:nch * Dd].rearrange("p (n d) -> p n d", d=Dd)
            for g in range(nch // per):
                pb, pk = nb()
                pbb = pb[:].bitcast(BF16)
                for j in range(per):
                    n = g * per + j
                    TE(lambda e, n=n, j=j, pbb=pbb: e.transpose(out=pbb[0:Cc, j * Dd:(j + 1) * Dd], in_=src[0:Dd, n * Cc:(n + 1) * Cc], identity=identb[0:Dd, 0:Dd]),
                       r=[skey, "identb"], w=[pk])
                o = dv[:, g * per:(g + 1) * per, :]
                i_ = pbb[0:Cc, :].rearrange("p (n d) -> p n d", d=Dd)
                A(lambda e, o=o, i_=i_: e.activation(out=o, in_=i_, func=AF.Copy), r=[pk], w=[dkey])
                if scale_ap is not None:
                    V(lambda e, o=o: e.tensor_scalar(out=o, in0=o, scalar1=scale_ap, scalar2=None, op0=ALU.mult), r=[dkey], w=[dkey])
            return dv

        def headnorm_gate(osrc, okey, gate, gkey, nw_ap, h, tmpk="TM0"):
            tmp = reg_bf(tmpk)
            A(lambda e: e.activation(out=tmp, in_=osrc, func=AF.Square), r=[okey], w=[tmpk])
            for tt in range(4):
                ts = slice(tt * 512, (tt + 1) * 512)
                pb, pk = nb()
                TE(lambda e, pb=pb, ts=ts: e.matmul(pb[0:64, :], lhsT=onesb[0:64, 0:64], rhs=tmp[:, ts], start=True, stop=True), r=[tmpk, "onesb"], w=[pk])
                A(lambda e, pb=pb, ts=ts: e.activation(out=rsb[0:64, :], in_=pb[0:64, :], func=AF.Ln, scale=1.0 / 64, bias=EPS), r=[pk], w=["rsb"])
                A(lambda e: e.activation(out=rsb[0:64, :], in_=rsb[0:64, :], func=AF.Exp, scale=-0.5), r=["rsb"], w=["rsb"])
                V(lambda e, ts=ts: e.scalar_tensor_tensor(out=osrc[:, ts], in0=osrc[:, ts], scalar=nw_ap, in1=rsb[0:64, :], op0=ALU.mult, op1=ALU.mult), r=[okey, "rsb", "hw"], w=[okey])
            V(lambda e: e.tensor_tensor(out=OMv[h], in0=osrc, in1=gate, op=ALU.mult), r=[okey, gkey], w=[("OM", h)])

        def out_proj(l, row0):
            src = dr["w_out"][l][row0:row0 + 256, :].rearrange("(h p) m -> p h m", p=64)
            i, key = wslot()
            i2, key2 = wslot()
            dsts = [W[0:64, i, :].rearrange("p (h m) -> p h m", m=1024), W[0:64, i2, :].rearrange("p (h m) -> p h m", m=1024)]
            DG_(lambda e: e.dma_start(out=dsts[0], in_=src[:, 0:2, :]), w=[key])
            DG_(lambda e: e.dma_start(out=dsts[1], in_=src[:, 2:4, :]), w=[key2])
            for mt in range(8):
                for tt in range(4):
                    ts = slice(tt * 512, (tt + 1) * 512)
                    pb, pk = nb()
                    for h in range(4):
                        TE(lambda e, h=h, pb=pb, mt=mt, ts=ts: e.matmul(pb[:], lhsT=dsts[h // 2][:, h % 2, mt * 128:(mt + 1) * 128], rhs=OMv[h][:, ts], start=(h == 0), stop=(h == 3)),
                           r=[key, key2, ("OM", h)], w=[pk])
                    V(lambda e, pb=pb, mt=mt, ts=ts: e.tensor_tensor(out=hT[:, mt, ts], in0=hT[:, mt, ts], in1=pb[:], op=ALU.add), r=[pk, "hT"], w=["hT"])

        def state_init():
            V(lambda e: e.memset(S32[:], 0.0), w=["S32"])
            V(lambda e: e.memset(Sbf[:], 0.0), w=["Sbf"])

        def state_update(Dk, p3, k3, dec):
            V(lambda e: e.scalar_tensor_tensor(out=S32[0:Dk, :], in0=S32[0:Dk, :], scalar=dec, in1=p3[0:Dk, 0:64], op0=ALU.mult, op1=ALU.add), r=["S32", k3, "dec"], w=["S32"])
            A(lambda e: e.activation(out=Sbf[0:Dk, :], in_=S32[0:Dk, :], func=AF.Copy), r=["S32"], w=["Sbf"])

        SBK = [("Sb", v) for v in range(64)]

        def lin_loop(Cc, Dk, ks, kskey, qs, qskey, qi, qikey, vtm, ktm, mask_ap, mkey, dec_ap, odst, okey):
            nblk = T // Cc
            npass = nblk // 32
            PTall = reg_bf("B2")
            SKV = F[0].rearrange("p (n v) -> p n v", v=64)
            Sball = reg_bf("F1")

            def blk(n):
                if Cc == 64:
                    return slice(0, 64), n, slice(n * 64, (n + 1) * 64)
                return slice((n % 2) * 32, (n % 2) * 32 + 32), n // 2, slice(n * 32, (n + 1) * 32)

            def phase_kv(ps_i):
                if Cc == 64:
                    for g in range(4):
                        p3, k3 = nb()
                        for j in range(8):
                            n = ps_i * 32 + g * 8 + j
                            pp, c64, cs = blk(n)
                            TE(lambda e, p3=p3, j=j, pp=pp, c64=c64: e.matmul(p3[0:Dk, j * 64:(j + 1) * 64], lhsT=ktm[pp, c64, :], rhs=vtm[pp, c64, :], start=True, stop=True), r=["TM1", "TM0"], w=[k3])
                        A(lambda e, p3=p3, g=g: e.activation(out=SKV[0:Dk, g * 8:(g + 1) * 8, :], in_=p3[0:Dk, :].rearrange("p (n v) -> p n v", v=64), func=AF.Copy), r=[k3], w=["F0"])
                    return
                SKVp = F[0].rearrange("p (m two v) -> p two m v", two=2, v=64)
                for g in range(2):
                    pbs = [nb(), nb()]
                    for j in range(16):
                        n = ps_i * 32 + g * 16 + j
                        pp, c64, cs = blk(n)
                        p3, k3 = pbs[n % 2]
                        jj = j // 2
                        TE(lambda e, p3=p3, jj=jj, pp=pp, c64=c64: e.matmul(p3[0:Dk, jj * 64:(jj + 1) * 64], lhsT=ktm[pp, c64, :], rhs=vtm[pp, c64, :], start=True, stop=True), r=["TM1", "TM0"], w=[k3])
                    for par in range(2):
                        p3, k3 = pbs[par]
                        A(lambda e, p3=p3, g=g, par=par: e.activation(out=SKVp[0:Dk, par, g * 8:(g + 1) * 8, :], in_=p3[0:Dk, :].rearrange("p (n v) -> p n v", v=64), func=AF.Copy), r=[k3], w=["F0"])

            def phase_scores():
                if Cc == 64:
                    PTv = PTall.rearrange("p (n c) -> p n c", c=64)
                    for g in range(4):
                        p1, k1 = nb()
                        for j in range(8):
                            n = g * 8 + j
                            pp, c64, cs = blk(n)
                            TE(lambda e, p1=p1, j=j, cs=cs: e.matmul(p1[0:64, j * 64:(j + 1) * 64], lhsT=ks[0:Dk, cs], rhs=qs[0:Dk, cs], start=True, stop=True), r=[kskey, qskey], w=[k1])
                        V(lambda e, p1=p1, g=g: e.tensor_tensor(out=PTv[:, g * 8:(g + 1) * 8, :], in0=p1[0:64, :].rearrange("p (n c) -> p n c", c=64), in1=mask_ap.unsqueeze(1).to_broadcast([64, 8, 64]), op=ALU.mult), r=[k1, mkey], w=["B2"])
                    return PTv
                PTv = PTall[:, 0:1024].rearrange("p (n c) -> p n c", c=32)
                for g in range(2):
                    pbs = [nb(), nb()]
                    for j in range(32):
                        n = g * 32 + j
                        pp, c64, cs = blk(n)
                        cl = c64 - g * 16
                        p1, k1 = pbs[n % 2]
                        TE(lambda e, p1=p1, pp=pp, cl=cl, cs=cs: e.matmul(p1[pp, cl * 32:(cl + 1) * 32], lhsT=ks[0:Dk, cs], rhs=qs[0:Dk, cs], start=True, stop=True), r=[kskey, qskey], w=[k1])
                    for par in range(2):
                        p1, k1 = pbs[par]
                        pp = slice(par * 32, par * 32 + 32)
                        V(lambda e, p1=p1, g=g, pp=pp: e.tensor_tensor(out=PTv[pp, g * 16:(g + 1) * 16, :], in0=p1[pp, :].rearrange("p (n c) -> p n c", c=32), in1=mask_ap[pp, :].unsqueeze(1).to_broadcast([32, 16, 32]), op=ALU.mult), r=[k1, mkey], w=["B2"])
                return PTv

            def phase_scan(ps_i):
                Sb = Sball[:, ps_i * 2048:(ps_i + 1) * 2048].rearrange("p (n v) -> p n v", v=64)
                for v in range(64):
                    init = 0.0 if ps_i == 0 else S32[0:Dk, v:v + 1]
                    V(lambda e, v=v, init=init, Sb=Sb: e.tensor_tensor_scan(out=Sb[0:Dk, :, v], data0=dec_ap[:, ps_i * 32:(ps_i + 1) * 32], data1=SKV[0:Dk, :, v], initial=init, op0=ALU.mult, op1=ALU.add),
                      r=["F0", "dec", "S32"], w=([("Sb", ps_i, v), "F1"] if v == 0 else [("Sb", ps_i, v)]))
                return Sb

            def phase_out(ps_i, PTv, Sb, carry):
                SBKp = [("Sb", ps_i, v) for v in range(64)]
                if Cc == 64:
                    for g in range(4):
                        p2, k2 = nb()
                        for j in range(8):
                            nl = g * 8 + j
                            n = ps_i * 32 + nl
                            pp, c64, cs = blk(n)
                            first = (nl == 0 and carry is None)
                            TE(lambda e, p2=p2, j=j, pp=pp, c64=c64, first=first: e.matmul(p2[0:64, j * 64:(j + 1) * 64], lhsT=vtm[pp, c64, :], rhs=PTv[pp, c64, :], start=True, stop=first), r=["TM0", "B2"], w=[k2])
                            if not first:
                                sprev = carry if nl == 0 else Sb[0:Dk, nl - 1, :]
                                rk_ = ["Sbf"] if nl == 0 else SBKp + ["F1"]
                                TE(lambda e, p2=p2, j=j, cs=cs, sprev=sprev: e.matmul(p2[0:64, j * 64:(j + 1) * 64], lhsT=sprev, rhs=qi[0:Dk, cs], start=False, stop=True), r=rk_ + [qikey], w=[k2])
                        t0 = (ps_i * 32 + g * 8) * 64
                        A(lambda e, p2=p2, t0=t0: e.activation(out=odst[:, t0:t0 + 512], in_=p2[0:64, :], func=AF.Copy), r=[k2], w=[okey])
                    return
                for g in range(2):
                    pbs = [nb(), nb()]
                    pI, kI = nb()
                    for j in range(16):
                        nl = g * 16 + j
                        n = ps_i * 32 + nl
                        pp, c64, cs = blk(n)
                        p2, k2 = pbs[j % 2]
                        jj = j // 2
                        TE(lambda e, p2=p2, jj=jj, pp=pp, c64=c64: e.matmul(p2[0:64, jj * 32:(jj + 1) * 32], lhsT=vtm[pp, c64, :], rhs=PTv[pp, c64, :], start=True, stop=True), r=["TM0", "B2"], w=[k2])
                        sprev = carry if nl == 0 else Sb[0:Dk, nl - 1, :]
                        rk_ = ["Sbf"] if nl == 0 else SBKp + ["F1"]
                        TE(lambda e, pI=pI, j=j, cs=cs, sprev=sprev: e.matmul(pI[0:64, j * 32:(j + 1) * 32], lhsT=sprev, rhs=qi[0:Dk, cs], start=True, stop=True), r=rk_ + [qikey], w=[kI])
                    t0 = (ps_i * 32 + g * 16) * 32
                    A(lambda e, pI=pI, t0=t0: e.activation(out=odst[:, t0:t0 + 512], in_=pI[0:64, :], func=AF.Copy), r=[kI], w=[okey])
                    ov = odst[:, t0:t0 + 512].rearrange("p (m two c) -> p two m c", two=2, c=32)
                    for par in range(2):
                        p2, k2 = pbs[par]
                        V(lambda e, p2=p2, ov=ov, par=par: e.tensor_tensor(out=ov[:, par, :, :], in0=ov[:, par, :, :], in1=p2[0:64, 0:256].rearrange("p (m c) -> p m c", c=32), op=ALU.add), r=[k2, okey], w=[okey])

            import os
            cut = int(os.environ.get("LL_CUT", "99"))
            phase_kv(0)
            if cut == 1:
                return
            PTv = phase_scores()
            if cut == 2:
                return
            Sb0 = phase_scan(0)
            if cut == 3:
                return
            if npass == 1:
                phase_out(0, PTv, Sb0, None)
                return
            SBK0 = [("Sb", 0, v) for v in range(64)]
            V(lambda e: e.tensor_copy(out=S32[0:Dk, :], in_=Sb0[0:Dk, 31, :]), r=SBK0 + ["F1"], w=["S32"])
            V(lambda e: e.tensor_copy(out=Sbf[0:Dk, :], in_=Sb0[0:Dk, 31, :]), r=SBK0 + ["F1"], w=["Sbf"])
            phase_out(0, PTv, Sb0, Sbz[0:Dk, :])
            if cut == 4:
                return
            phase_kv(1)
            Sb1 = phase_scan(1)
            phase_out(1, PTv, Sb1, Sbf[0:Dk, :])

        def rope_tables(s):
            pos_i = F[2].bitcast(I32)
            DS(lambda e: e.dma_start(out=pos_i, in_=dr["positions"][s:s + 1, :].broadcast_to([64, T])), w=["F2"])
            V(lambda e: e.tensor_copy(out=F[0], in_=pos_i), r=["F2"], w=["F0"])
            V(lambda e: e.tensor_scalar(out=F[0], in0=F[0], scalar1=cst["freq"][:, 0:1], scalar2=None, op0=ALU.mult), r=["F0", ("c", "freq")], w=["F0"])
            MAGIC = 12582912.0
            C1 = 6.28125
            C2 = 2 * math.pi - 6.28125
            for which, dstt in ((0, sinS), (1, cosT)):
                shift = 0.0 if which == 0 else math.pi / 2
                V(lambda e, shift=shift: e.tensor_scalar(out=F[1], in0=F[0], scalar1=shift, scalar2=None, op0=ALU.add), r=["F0"], w=["F1"])
                V(lambda e: e.tensor_scalar(out=F[2], in0=F[1], scalar1=1.0 / (2 * math.pi), scalar2=MAGIC, op0=ALU.mult, op1=ALU.add), r=["F1"], w=["F2"])
                V(lambda e: e.tensor_scalar(out=F[2], in0=F[2], scalar1=MAGIC, scalar2=None, op0=ALU.subtract), r=["F2"], w=["F2"])
                V(lambda e: e.scalar_tensor_tensor(out=F[1], in0=F[2], scalar=-C1, in1=F[1], op0=ALU.mult, op1=ALU.add), r=["F1", "F2"], w=["F1"])
                V(lambda e: e.scalar_tensor_tensor(out=F[1], in0=F[2], scalar=-C2, in1=F[1], op0=ALU.mult, op1=ALU.add), r=["F1", "F2"], w=["F1"])
                V(lambda e: e.tensor_scalar(out=F[1], in0=F[1], scalar1=-math.pi, scalar2=math.pi, op0=ALU.max, op1=ALU.min), r=["F1"], w=["F1"])
                if which == 0:
                    A(lambda e: e.activation(out=F[2], in_=F[1], func=AF.Sin), r=["F1"], w=["F2"])
                    V(lambda e: e.tensor_scalar(out=sinS, in0=F[2], scalar1=cst["sgn"][:, 0:1], scalar2=None, op0=ALU.mult), r=["F2", ("c", "sgn")], w=["ROPE"])
                else:
                    A(lambda e: e.activation(out=cosT, in_=F[1], func=AF.Sin), r=["F1"], w=["ROPE"])

        def retention(s, l):
            cut = 9
            rope_tables(s)
            if cut == 0:
                return
            wq, kq = w_cols("w_in", l, RQ, 256)
            wk, kk = w_cols("w_in", l, RK, 256)
            wv, kv = w_cols("w_in", l, RV, 256)
            wg, kg = w_cols("w_in", l, RG, 256)
            tmpb = reg_bf("TM1")
            for h in range(4):
                QB, KB, VB, GB, QI = Bf
                for tt in range(4):
                    ts = slice(tt * 512, (tt + 1) * 512)
                    for (w_, wk_, dstb, dk_) in ((wq, kq, QB, "B0"), (wk, kk, KB, "B1")):
                        pb, pk = proj64(w_, wk_, h * 64, 64, tt)
                        A(lambda e, pb=pb: e.activation(out=tmpb[:, 0:512], in_=pb[0:64, :], func=AF.Copy), r=[pk], w=["TM1"])
                        p2, k2 = nb()
                        TE(lambda e, p2=p2: e.matmul(p2[0:64, :], lhsT=permb[:], rhs=tmpb[:, 0:512], start=True, stop=True), r=["TM1", "permb"], w=[k2])
                        A(lambda e, pb=pb, ts=ts: e.activation(out=F[0][:, ts], in_=pb[0:64, :], func=AF.Copy), r=[pk], w=["F0"])
                        A(lambda e, p2=p2, ts=ts: e.activation(out=F[1][:, ts], in_=p2[0:64, :], func=AF.Copy), r=[k2], w=["F1"])
                        V(lambda e, ts=ts: e.tensor_tensor(out=F[0][:, ts], in0=F[0][:, ts], in1=cosT[:, ts], op=ALU.mult), r=["F0", "ROPE"], w=["F0"])
                        V(lambda e, ts=ts: e.tensor_tensor(out=F[1][:, ts], in0=F[1][:, ts], in1=sinS[:, ts], op=ALU.mult), r=["F1", "ROPE"], w=["F1"])
                        V(lambda e, ts=ts, dstb=dstb: e.tensor_tensor(out=dstb[:, ts], in0=F[0][:, ts], in1=F[1][:, ts], op=ALU.add), r=["F0", "F1"], w=[dk_])
                    pb, pk = proj64(wv, kv, h * 64, 64, tt)
                    A(lambda e, pb=pb, ts=ts: e.activation(out=VB[:, ts], in_=pb[0:64, :], func=AF.Copy), r=[pk], w=["B2"])
                    pb, pk = proj64(wg, kg, h * 64, 64, tt)
                    A(lambda e, pb=pb, ts=ts: e.activation(out=GB[:, ts], in_=pb[0:64, :], func=AF.Silu), r=[pk], w=["B3"])
                if cut == 1:
                    continue
                V(lambda e, h=h: e.tensor_tensor(out=QI.rearrange("p (n c) -> p n c", c=64), in0=QB.rearrange("p (n c) -> p n c", c=64),
                                                 in1=cst["gq"][:, h, :].unsqueeze(1).to_broadcast([64, 32, 64]), op=ALU.mult), r=["B0", ("c", "gq")], w=["B4"])
                vtm = to_tm(VB, "B2", TM[0], "TM0", 64, 64)
                ktm = to_tm(KB, "B1", TM[1], "TM1", 64, 64, scale_ap=cst["gk"][:, h:h + 1])
                if cut == 2:
                    continue
                dec = C["retdec"][h]
                V(lambda e: e.memset(decs_t[:, 0:32], dec), w=["dec"])
                lin_loop(64, 64, KB, "B1", QB, "B0", QI, "B4", vtm, ktm, cst["maskr"][:, h, :], ("c", "maskr"), decs_t[0:64, 0:32], F[2], "F2")
                headnorm_gate(F[2], "F2", GB, "B3", hw[:, 15:16], h)
            out_proj(l, 0)

        def diag_gated(s, l, mixer):
            Dk = 32 if mixer == 2 else 64
            if mixer == 2:
                wq, kq = w_cols("w_in", l, CQ, 128)
                wk, kk = w_cols("w_in", l, CK, 128)
                wv, kv = w_cols("w_in", l, CV, 256)
                wg, kg = w_cols("w_in", l, CG, 256)
                wl, kl = w_small(l, CLR, 16)
            else:
                wq, kq = w_cols("w_in", l, DQ, 256)
                wf, kf = w_cols("w_in", l, DF_, 256)
                wv, kv = w_cols("w_in", l, DI, 256)
                wg, kg = w_cols("w_in", l, DG, 256)
            gsl = -1.0 / 16 if mixer == 2 else -1.0
            qsc = Dk ** -0.5 if mixer == 2 else 1.0
            for h in range(4):
                QS, KS, KT, VB, GB = Bf
                L_, E_, K_ = F
                for tt in range(4):
                    ts = slice(tt * 512, (tt + 1) * 512)
                    if mixer == 2:
                        pb, pk = proj64(wl, kl, 0, 16, tt)
                        A(lambda e, pb=pb: e.activation(out=rsb[0:16, :], in_=pb[0:16, :], func=AF.Copy), r=[pk], w=["rsb"])
                        pb, pk = nb()
                        TE(lambda e, pb=pb, h=h: e.matmul(pb[0:32, :], lhsT=glup[:, l, h * 32:(h + 1) * 32], rhs=rsb[0:16, :], start=True, stop=True), r=["rsb", "glup"], w=[pk])
                        A(lambda e, pb=pb, ts=ts, h=h: e.activation(out=L_[0:32, ts], in_=pb[0:32, :], func=AF.Exp, scale=-1.0, bias=glab[:, l * 4 + h:l * 4 + h + 1]), r=[pk, "glab"], w=["F0"])
                        A(lambda e, ts=ts: e.activation(out=L_[0:32, ts], in_=L_[0:32, ts], func=AF.Ln, bias=1.0), r=["F0"], w=["F0"])
                        pb, pk = proj64(wk, kk, h * 32, 32, tt)
                        A(lambda e, pb=pb, ts=ts: e.activation(out=K_[0:32, ts], in_=pb[0:32, :], func=AF.Copy), r=[pk], w=["F2"])
                    else:
                        lbc = hw[:, 6 + h:7 + h] if l == 1 else hw[:, 14:15]
                        omc = hw[:, 10 + h:11 + h] if l == 1 else hw[:, 15:16]
                        pb, pk = proj64(wf, kf, h * 64, 64, tt)
                        A(lambda e, pb=pb, ts=ts: e.activation(out=E_[:, ts], in_=pb[0:64, :], func=AF.Sigmoid), r=[pk], w=["F1"])
                        V(lambda e, ts=ts, lbc=lbc, omc=omc: e.tensor_scalar(out=L_[:, ts], in0=E_[:, ts], scalar1=omc, scalar2=lbc, op0=ALU.mult, op1=ALU.add), r=["F1", "hw"], w=["F0"])
                        A(lambda e, ts=ts: e.activation(out=L_[:, ts], in_=L_[:, ts], func=AF.Ln), r=["F0"], w=["F0"])
                        V(lambda e, ts=ts: e.tensor_scalar(out=L_[:, ts], in0=L_[:, ts], scalar1=-1.0, scalar2=None, op0=ALU.mult), r=["F0"], w=["F0"])
                        V(lambda e, ts=ts: e.tensor_scalar(out=K_[:, ts], in0=E_[:, ts], scalar1=-1.0, scalar2=1.0, op0=ALU.mult, op1=ALU.add), r=["F1"], w=["F2"])
                        V(lambda e, ts=ts, omc=omc: e.tensor_scalar(out=K_[:, ts], in0=K_[:, ts], scalar1=omc, scalar2=None, op0=ALU.mult), r=["F2", "hw"], w=["F2"])
                    pb, pk = proj64(wv, kv, h * 64, 64, tt)
                    A(lambda e, pb=pb, ts=ts: e.activation(out=VB[:, ts], in_=pb[0:64, :], func=AF.Copy), r=[pk], w=["B3"])
                    pb, pk = proj64(wg, kg, h * 64, 64, tt)
                    A(lambda e, pb=pb, ts=ts: e.activation(out=GB[:, ts], in_=pb[0:64, :], func=AF.Silu), r=[pk], w=["B4"])
                V(lambda e: e.tensor_tensor_scan(out=E_[0:Dk, :], data0=onecol[0:Dk, 0:1].to_broadcast([Dk, T]), data1=L_[0:Dk, :], initial=0.0, op0=ALU.mult, op1=ALU.add), r=["F0", "onecol"], w=["F1"])
                Ev = E_[0:Dk, :].rearrange("p (m c) -> p m c", c=32)
                Lv = L_[0:Dk, :].rearrange("p (m c) -> p m c", c=32)
                V(lambda e: e.memset(decs_t[0:Dk, 0:1], 0.0), w=["dec"])
                V(lambda e: e.tensor_copy(out=decs_t[0:Dk, 1:64], in_=Ev[:, 0:63, 31]), r=["F1"], w=["dec"])
                V(lambda e: e.tensor_tensor(out=Ev, in0=Ev, in1=decs_t[0:Dk, 0:64].unsqueeze(2).to_broadcast([Dk, 64, 32]), op=ALU.subtract), r=["F1", "dec"], w=["F1"])
                A(lambda e: e.activation(out=L_[0:Dk, :], in_=E_[0:Dk, :], func=AF.Exp, scale=-gsl), r=["F1"], w=["F0"])
                V(lambda e: e.tensor_tensor(out=KS[0:Dk, :], in0=K_[0:Dk, :], in1=L_[0:Dk, :], op=ALU.mult), r=["F0", "F2"], w=["B1"])
                V(lambda e: e.tensor_tensor(out=Lv, in0=Ev, in1=Ev[:, :, 31:32].to_broadcast([Dk, 64, 32]), op=ALU.subtract), r=["F1", "B1"], w=["F0"])
                A(lambda e: e.activation(out=L_[0:Dk, :], in_=L_[0:Dk, :], func=AF.Exp, scale=-gsl), r=["F0"], w=["F0"])
                V(lambda e: e.tensor_tensor(out=KT[0:Dk, :], in0=K_[0:Dk, :], in1=L_[0:Dk, :], op=ALU.mult), r=["F0", "F2"], w=["B2"])
                A(lambda e: e.activation(out=decs_t[0:Dk, 0:64], in_=Ev[:, :, 31], func=AF.Exp, scale=gsl), r=["F1"], w=["dec"])
                A(lambda e: e.activation(out=L_[0:Dk, :], in_=E_[0:Dk, :], func=AF.Exp, scale=gsl), r=["F1", "B2"], w=["F0"])
                for tt in range(4):
                    ts = slice(tt * 512, (tt + 1) * 512)
                    pb, pk = proj64(wq, kq, h * Dk, Dk, tt)
                    V(lambda e, pb=pb, ts=ts: e.scalar_tensor_tensor(out=QS[0:Dk, ts], in0=pb[0:Dk, :], scalar=qsc, in1=L_[0:Dk, ts], op0=ALU.mult, op1=ALU.mult), r=[pk, "F0"], w=["B0"])
                vtm = to_tm(VB, "B3", TM[0], "TM0", 64, 64)
                ktm = to_tm(KT, "B2", TM[1], "TM1", 64, Dk)
                lin_loop(32, Dk, KS, "B1", QS, "B0", QS, "B0", vtm, ktm, cst["caus32"][:], ("c", "caus32"), decs_t[0:Dk, 0:64], F[2], "F2")
                nwc = hw[:, 2 + l:3 + l] if mixer == 2 else hw[:, 4 + l:5 + l]
                headnorm_gate(F[2], "F2", GB, "B4", nwc, h)
            out_proj(l, 512 if mixer == 2 else 768)

        def gdn(s, l):
            wq, kq = w_cols("w_in", l, BQ, 256)
            wk, kk = w_cols("w_in", l, BK, 256)
            wv, kv = w_cols("w_in", l, BV, 256)
            wg, kg = w_cols("w_in", l, BG, 256)
            wab, kab = w_small(l, BA, 8)
            pb, pk = nb()
            for n in range(32):
                for k in range(8):
                    TE(lambda e, pb=pb, n=n, k=k: e.matmul(pb[0:64, 8 * n:8 * n + 8], lhsT=xnT[:, k, n * 64:(n + 1) * 64], rhs=wab[:, k, :], start=(k == 0), stop=(k == 7)),
                       r=[kab, ("xnT", n // 8)], w=[pk])
            A(lambda e, pb=pb: e.activation(out=ab_all[:], in_=pb[0:64, 0:256], func=AF.Copy), r=[pk], w=["ab_all"])
            X1 = ROPE[0:64, 0:1024].bitcast(F32).rearrange("p (i c) -> p i c", c=64)
            X2 = ROPE[0:64, 1024:2048].bitcast(F32).rearrange("p (i c) -> p i c", c=64)
            Rr = ROPE[0:64, 2048:4096].bitcast(F32).rearrange("p (i c) -> p i c", c=128)
            X3 = ROPE[0:64, 2048:3072].bitcast(F32).rearrange("p (i c) -> p i c", c=64)
            for h in range(4):
                QB, KB, VB, GB, QG = Bf
                for ti, (w_, wk_, dstb, dk_) in enumerate(((wq, kq, QB, "B0"), (wk, kk, KB, "B1"), (wv, kv, VB, "B2"))):
                    for tt in range(4):
                        ts = slice(tt * 512, (tt + 1) * 512)
                        pb, pk = proj64(w_, wk_, h * 64, 64, tt)
                        A(lambda e, pb=pb, ts=ts: e.activation(out=F[0][:, ts], in_=pb[0:64, :], func=AF.Copy), r=[pk], w=["F0"])
                    cj = ti * 4 + h
                    V(lambda e, cj=cj: e.tensor_scalar(out=F[1], in0=F[0], scalar1=gcw[:, l, 3, cj:cj + 1], scalar2=None, op0=ALU.mult), r=["F0", "gcw"], w=["F1"])
                    for sh in (1, 2, 3):
                        V(lambda e, cj=cj, sh=sh: e.scalar_tensor_tensor(out=F[1][:, sh:T], in0=F[0][:, 0:T - sh], scalar=gcw[:, l, 3 - sh, cj:cj + 1], in1=F[1][:, sh:T], op0=ALU.mult, op1=ALU.add), r=["F0", "F1", "gcw"], w=["F1"])
                    if ti == 2:
                        A(lambda e: e.activation(out=VB, in_=F[1], func=AF.Silu), r=["F1"], w=["B2"])
                    else:
                        A(lambda e: e.activation(out=F[1], in_=F[1], func=AF.Silu), r=["F1"], w=["F1"])
                        tmp = reg_bf("TM0")
                        A(lambda e: e.activation(out=tmp, in_=F[1], func=AF.Square), r=["F1"], w=["TM0"])
                        for tt in range(4):
                            ts = slice(tt * 512, (tt + 1) * 512)
                            pb, pk = nb()
                            TE(lambda e, pb=pb, ts=ts: e.matmul(pb[0:64, :], lhsT=onesb[0:64, 0:64], rhs=tmp[:, ts], start=True, stop=True), r=["TM0", "onesb"], w=[pk])
                            A(lambda e, pb=pb: e.activation(out=rsb[0:64, :], in_=pb[0:64, :], func=AF.Ln, scale=1.0, bias=EPS), r=[pk], w=["rsb"])
                            A(lambda e: e.activation(out=rsb[0:64, :], in_=rsb[0:64, :], func=AF.Exp, scale=-0.5), r=["rsb"], w=["rsb"])
                            sc = 0.125 if ti == 0 else 1.0
                            V(lambda e, ts=ts, sc=sc, dstb=dstb: e.scalar_tensor_tensor(out=dstb[:, ts], in0=F[1][:, ts], scalar=sc, in1=rsb[0:64, :], op0=ALU.mult, op1=ALU.mult), r=["F1", "rsb"], w=[dk_])
                for tt in range(4):
                    ts = slice(tt * 512, (tt + 1) * 512)
                    pb, pk = proj64(wg, kg, h * 64, 64, tt)
                    A(lambda e, pb=pb, ts=ts: e.activation(out=GB[:, ts], in_=pb[0:64, :], func=AF.Silu), r=[pk], w=["B3"])
                abv = ab_all[:].rearrange("p (n j h) -> p j h n", j=2, h=4)
                A(lambda e: e.activation(out=small[:, 0:2, :], in_=abv[:, :, h, :], func=AF.Copy), r=["ab_all"], w=["small"])
                gi = l * 4 + h
                A(lambda e: e.activation(out=small[:, 1, :], in_=small[:, 1, :], func=AF.Sigmoid), r=["small"], w=["small"])
                A(lambda e: e.activation(out=small[:, 2, :], in_=small[:, 0, :], func=AF.Exp, bias=gsc[:, 8 + gi:9 + gi]), r=["small", "gsc"], w=["small"])
                A(lambda e: e.activation(out=small[:, 2, :], in_=small[:, 2, :], func=AF.Ln, bias=1.0), r=["small"], w=["small"])
                V(lambda e: e.tensor_scalar(out=small[:, 2, :], in0=small[:, 2, :], scalar1=gsc[:, gi:gi + 1], scalar2=None, op0=ALU.mult), r=["small", "gsc"], w=["small"])
                V(lambda e: e.tensor_scalar(out=small[:, 0, :], in0=small[:, 1, :], scalar1=-1.0, scalar2=None, op0=ALU.mult), r=["small"], w=["small"])
                pb, pk = nb()
                TE(lambda e, pb=pb: e.matmul(pb[0:64, 0:32], lhsT=cst["tri"][:], rhs=small[:, 2, :], start=True, stop=True), r=["small", ("c", "tri")], w=[pk])
                TE(lambda e, pb=pb: e.matmul(pb[0:64, 32:64], lhsT=ones64f[:], rhs=small[:, 2, :], start=True, stop=True), r=["small", "ones64f"], w=[pk])
                A(lambda e, pb=pb: e.activation(out=small[:, 3:5, :], in_=pb[0:64, 0:64].rearrange("p (j n) -> p j n", j=2), func=AF.Copy), r=[pk], w=["small"])
                A(lambda e: e.activation(out=small[:, 5, :], in_=small[:, 3, :], func=AF.Exp), r=["small"], w=["small"])
                V(lambda e: e.tensor_tensor(out=small[:, 6, :], in0=small[:, 1, :], in1=small[:, 5, :], op=ALU.mult), r=["small"], w=["small"])
                V(lambda e: e.tensor_tensor(out=small[:, 7, :], in0=small[:, 4, :], in1=small[:, 3, :], op=ALU.subtract), r=["small"], w=["small"])
                A(lambda e: e.activation(out=small[:, 7, :], in_=small[:, 7, :], func=AF.Exp), r=["small"], w=["small"])
                A(lambda e: e.activation(out=decs_t[:, 0:32], in_=small[:, 4, :], func=AF.Exp), r=["small"], w=["dec"])
                if h == 0:
                    tap(0, QB, ['B0']); tap(1, KB, ['B1']); tap(2, VB, ['B2'])
                    tap(0, small[:].rearrange('p a b -> p (a b)'), ['small'], n=256)
                ktm = to_tm(KB, "B1", TM[0], "TM0", 64, 64)
                vtm = to_tm(VB, "B2", TM[1], "TM1", 64, 64)
                qtm_t = reg_bf("F0")[:, 0:2048]
                qtm = to_tm(QB, "B0", qtm_t, "F0", 64, 64)
                V(lambda e: e.tensor_tensor(out=qtm, in0=qtm, in1=small[:, 5, :].unsqueeze(2).to_broadcast([64, 32, 64]), op=ALU.mult), r=["F0", "small"], w=["F0"])
                for g in range(8):
                    pb, pk = nb()
                    pbb = pb[:].bitcast(BF16)
                    for j in range(4):
                        n = g * 4 + j
                        TE(lambda e, pbb=pbb, j=j, n=n: e.transpose(out=pbb[0:64, j * 64:(j + 1) * 64], in_=qtm[:, n, :], identity=identb[0:64, 0:64]), r=["F0", "identb"], w=[pk])
                    A(lambda e, pbb=pbb, g=g: e.activation(out=QG[:, g * 256:(g + 1) * 256], in_=pbb[0:64, 0:256], func=AF.Copy), r=[pk], w=["B4"])
                Uv = F[1].rearrange("p (n v) -> p n v", v=64)
                WT = VB.rearrange("p (n c) -> p n c", c=64)
                QKT = reg_bf("F0")[:, 2048:4096].rearrange("p (n c) -> p n c", c=64)
                for G in range(4):
                    n0 = G * 8
                    V(lambda e, n0=n0: e.tensor_tensor(out=X1, in0=small[:, 2, n0:n0 + 8].unsqueeze(2).to_broadcast([64, 8, 64]), in1=cst["masku"][:].unsqueeze(1).to_broadcast([64, 8, 64]), op=ALU.mult),
                      r=["small", ("c", "masku")], w=["X1"])
                    X1f = ROPE[0:64, 0:1024].bitcast(F32)
                    pdt, kdt = nb()
                    TE(lambda e, pdt=pdt: e.matmul(pdt[0:64, :], lhsT=cst["tri"][:], rhs=X1f, start=True, stop=False), r=["X1", ("c", "tri")], w=[kdt])
                    for i in range(8):
                        TE(lambda e, pdt=pdt, i=i: e.matmul(pdt[0:64, i * 64:(i + 1) * 64], lhsT=ident64, rhs=cst["negs"][:], start=False, stop=(i == 7)), r=[("c", "negs"), "ident"], w=[kdt])
                    A(lambda e, pdt=pdt: e.activation(out=X2, in_=pdt[0:64, :].rearrange("p (i c) -> p i c", c=64), func=AF.Exp), r=[kdt], w=["X2"])
                    pd, kd = nb()
                    for i in range(8):
                        TE(lambda e, pd=pd, i=i: e.matmul(pd[0:64, i * 64:(i + 1) * 64], lhsT=X1[:, i, :], rhs=cst["tri"][:], start=True, stop=False), r=["X1", ("c", "tri")], w=[kd])
                        TE(lambda e, pd=pd, i=i: e.matmul(pd[0:64, i * 64:(i + 1) * 64], lhsT=ident64, rhs=cst["negi"][:], start=False, stop=True), r=[("c", "negi"), "ident"], w=[kd])
                    A(lambda e, pd=pd: e.activation(out=X3, in_=pd[0:64, :].rearrange("p (i c) -> p i c", c=64), func=AF.Exp), r=[kd], w=["RR"])
                    pkk, kkk = nb()
                    pqk, kqk = nb()
                    for i in range(8):
                        cs = slice((n0 + i) * 64, (n0 + i + 1) * 64)
                        TE(lambda e, pkk=pkk, i=i, cs=cs: e.matmul(pkk[0:64, i * 64:(i + 1) * 64], lhsT=KB[:, cs], rhs=KB[:, cs], start=True, stop=True), r=["B1"], w=[kkk])
                        TE(lambda e, pqk=pqk, i=i, cs=cs: e.matmul(pqk[0:64, i * 64:(i + 1) * 64], lhsT=KB[:, cs], rhs=QB[:, cs], start=True, stop=True), r=["B1", "B0"], w=[kqk])
                    V(lambda e, pkk=pkk: e.tensor_tensor(out=X2, in0=pkk[0:64, :].rearrange("p (i c) -> p i c", c=64), in1=X2, op=ALU.mult), r=[kkk, "X2"], w=["X2"])
                    V(lambda e, n0=n0: e.tensor_tensor(out=X2, in0=X2, in1=small[:, 0, n0:n0 + 8].unsqueeze(2).to_broadcast([64, 8, 64]), op=ALU.mult), r=["X2", "small"], w=["X2"])
                    V(lambda e, pqk=pqk, n0=n0: e.tensor_tensor(out=QKT[:, n0:n0 + 8, :], in0=pqk[0:64, :].rearrange("p (i c) -> p i c", c=64), in1=X3, op=ALU.mult), r=[kqk, "RR"], w=["F0"])
                    pq, kq_ = nb()
                    for i in range(8):
                        TE(lambda e, pq=pq, i=i: e.transpose(out=pq[0:64, i * 64:(i + 1) * 64], in_=X2[:, i, :], identity=ident64), r=["X2", "ident"], w=[kq_])
                    A(lambda e, pq=pq: e.activation(out=X1, in_=pq[0:64, :].rearrange("p (i c) -> p i c", c=64), func=AF.Copy), r=[kq_], w=["X1"])
                    V(lambda e, n0=n0: e.tensor_tensor(out=Rr[:, :, 0:64], in0=vtm[:, n0:n0 + 8, :], in1=small[:, 1, n0:n0 + 8].unsqueeze(2).to_broadcast([64, 8, 64]), op=ALU.mult), r=["TM1", "small"], w=["RR"])
                    V(lambda e, n0=n0: e.tensor_tensor(out=Rr[:, :, 64:128], in0=ktm[:, n0:n0 + 8, :], in1=small[:, 6, n0:n0 + 8].unsqueeze(2).to_broadcast([64, 8, 64]), op=ALU.mult), r=["TM0", "small"], w=["RR"])
                    for lev in range(6):
                        pa, ka = nb()
                        pa2, ka2 = nb()
                        for i in range(8):
                            pp = pa if i < 4 else pa2
                            TE(lambda e, pp=pp, i=i: e.matmul(pp[0:64, (i % 4) * 128:(i % 4 + 1) * 128], lhsT=X1[:, i, :], rhs=Rr[:, i, :], start=True, stop=True), r=["X1", "RR"], w=[ka if i < 4 else ka2])
                        V(lambda e, pa=pa: e.tensor_tensor(out=Rr[:, 0:4, :], in0=Rr[:, 0:4, :], in1=pa[0:64, :].rearrange("p (i c) -> p i c", c=128), op=ALU.add), r=[ka, "RR"], w=["RR"])
                        V(lambda e, pa2=pa2: e.tensor_tensor(out=Rr[:, 4:8, :], in0=Rr[:, 4:8, :], in1=pa2[0:64, :].rearrange("p (i c) -> p i c", c=128), op=ALU.add), r=[ka2, "RR"], w=["RR"])
                        if lev < 5:
                            pp_, kp_ = nb()
                            pq_, kq2 = nb()
                            for i in range(8):
                                TE(lambda e, pp_=pp_, i=i: e.matmul(pp_[0:64, i * 64:(i + 1) * 64], lhsT=X1[:, i, :], rhs=X2[:, i, :], start=True, stop=True), r=["X1", "X2"], w=[kp_])
                                TE(lambda e, pq_=pq_, i=i: e.matmul(pq_[0:64, i * 64:(i + 1) * 64], lhsT=X2[:, i, :], rhs=X1[:, i, :], start=True, stop=True), r=["X1", "X2"], w=[kq2])
                            A(lambda e, pp_=pp_: e.activation(out=X2, in_=pp_[0:64, :].rearrange("p (i c) -> p i c", c=64), func=AF.Copy), r=[kp_], w=["X2"])
                            V(lambda e, pq_=pq_: e.tensor_copy(out=X1, in_=pq_[0:64, :].rearrange("p (i c) -> p i c", c=64)), r=[kq2], w=["X1"])
                    A(lambda e, n0=n0: e.activation(out=Uv[:, n0:n0 + 8, :], in_=Rr[:, :, 0:64], func=AF.Copy), r=["RR"], w=["F1"])
                    pw, kw = nb()
                    for i in range(8):
                        TE(lambda e, pw=pw, i=i: e.transpose(out=pw[0:64, i * 64:(i + 1) * 64], in_=Rr[:, i, 64:128], identity=ident64), r=["RR", "ident"], w=[kw])
                    A(lambda e, pw=pw, n0=n0: e.activation(out=WT[:, n0:n0 + 8, :], in_=pw[0:64, :].rearrange("p (i c) -> p i c", c=64), func=AF.Copy), r=[kw], w=["B2"])
                if h == 0:
                    tap(1, F[1], ['F1']); tap(3, VB, ['B2']); tap(4, reg_bf('F0')[:, 2048:4096], ['F0']); tap(5, QG, ['B4'])
                    tap(6, TM[0], ['TM0']); tap(7, TM[1], ['TM1'])
                V(lambda e: e.tensor_tensor(out=ktm, in0=ktm, in1=small[:, 7, :].unsqueeze(2).to_broadcast([64, 32, 64]), op=ALU.mult), r=["TM0", "small"], w=["TM0"])
                state_init()
                for n in range(32):
                    cs = slice(n * 64, (n + 1) * 64)
                    p1, k1 = nb()
                    TE(lambda e, p1=p1, n=n: e.matmul(p1[0:64, 0:64], lhsT=WT[:, n, :], rhs=Sbf[:], start=True, stop=True), r=["B2", "Sbf"], w=[k1])
                    ub = PTb[n % 2]
                    V(lambda e, p1=p1, n=n, ub=ub: e.tensor_tensor(out=ub[:], in0=Uv[:, n, :], in1=p1[0:64, 0:64], op=ALU.subtract), r=[k1, "F1"], w=[("PTb", n % 2)])
                    p2, k2 = nb()
                    TE(lambda e, p2=p2, ub=ub, n=n: e.matmul(p2[0:64, 0:64], lhsT=ub[:], rhs=QKT[:, n, :], start=True, stop=False), r=[("PTb", n % 2), "F0"], w=[k2])
                    TE(lambda e, p2=p2, cs=cs: e.matmul(p2[0:64, 0:64], lhsT=Sbf[:], rhs=QG[:, cs], start=False, stop=True), r=["Sbf", "B4"], w=[k2])
                    A(lambda e, p2=p2, cs=cs: e.activation(out=F[2][:, cs], in_=p2[0:64, 0:64], func=AF.Copy), r=[k2], w=["F2"])
                    p3, k3 = nb()
                    TE(lambda e, p3=p3, n=n, ub=ub: e.matmul(p3[0:64, 0:64], lhsT=ktm[:, n, :], rhs=ub[:], start=True, stop=True), r=["TM0", ("PTb", n % 2)], w=[k3])
                    state_update(64, p3, k3, decs_t[:, n:n + 1])
                if h == 0:
                    tap(2, F[2], ['F2'])
                headnorm_gate(F[2], "F2", GB, "B3", hw[:, 0 + l:1 + l], h, tmpk="TM1")
            out_proj(l, 256)

        def xattn(s, l):
            QT = SCR[:, 0:16384].rearrange("p (k t) -> p k t", t=T)
            AO = SCR[:, 16384:32768].rearrange("p (k t) -> p k t", t=T)
            PR = SCR[:, 32768:33792].rearrange("p (m t) -> p m t", t=512)
            KTm = ROPE[:, 0:2048].rearrange("p (k m) -> p k m", m=256)
            Vtm = ROPE[:, 2048:4096].rearrange("p (c d) -> p c d", d=D)
            QTK = rk(0, 16384)
            AOK = rk(16384, 16384)
            PRK = rk(32768, 1024)
            specs = [(nm, l, q4 * 256, 256) for nm in ("xattn_wk", "xattn_wv", "xattn_wq", "xattn_wo") for q4 in range(4)]
            loaded = {}
            nxt = [0]

            def getw(i):
                while nxt[0] < len(specs) and nxt[0] <= i + 3:
                    loaded[nxt[0]] = w_cols(*specs[nxt[0]])
                    nxt[0] += 1
                return loaded[i]

            for q4 in range(4):
                wk_, kk_ = getw(q4)
                for j in range(2):
                    mtile = q4 * 2 + j
                    pb, pk = nb()
                    for k in range(8):
                        TE(lambda e, k=k, pb=pb, j=j, wk_=wk_: e.matmul(pb[:, 0:256], lhsT=wk_[:, k, j * 128:(j + 1) * 128], rhs=memT[:, k, :], start=(k == 0), stop=(k == 7)), r=[kk_, "memT"], w=[pk])
                    A(lambda e, pb=pb, mtile=mtile: e.activation(out=KTm[:, mtile, :], in_=pb[:, 0:256], func=AF.Copy), r=[pk], w=["ROPE"])
            for q4 in range(4):
                wv_, kv_ = getw(4 + q4)
                for mc in range(2):
                    pb, pk = nb()
                    for k in range(8):
                        TE(lambda e, k=k, pb=pb, mc=mc, wv_=wv_: e.matmul(pb[:, 0:256], lhsT=memT[:, k, mc * 128:(mc + 1) * 128], rhs=wv_[:, k, :], start=(k == 0), stop=(k == 7)), r=[kv_, "memT"], w=[pk])
                    A(lambda e, pb=pb, mc=mc, q4=q4: e.activation(out=Vtm[:, mc, q4 * 256:(q4 + 1) * 256], in_=pb[:, 0:256], func=AF.Copy), r=[pk], w=["ROPE"])
            for q4 in range(4):
                wq_, kq_ = getw(8 + q4)
                for j in range(2):
                    mt = q4 * 2 + j
                    for tt in range(4):
                        pb, pk = proj64(wq_, kq_, j * 128, 128, tt)
                        A(lambda e, pb=pb, mt=mt, tt=tt: e.activation(out=QT[:, mt, tt * 512:(tt + 1) * 512], in_=pb[:], func=AF.Copy), r=[pk], w=QTK)
            for tt in range(4):
                ts = slice(tt * 512, (tt + 1) * 512)
                for hh in range(4):
                    for mc in range(2):
                        pb, pk = nb()
                        for kk2 in range(2):
                            TE(lambda e, pb=pb, mc=mc, kk2=kk2, hh=hh, ts=ts: e.matmul(pb[:], lhsT=KTm[:, hh * 2 + kk2, mc * 128:(mc + 1) * 128], rhs=QT[:, hh * 2 + kk2, ts], start=(kk2 == 0), stop=(kk2 == 1)),
                               r=["ROPE"] + QTK, w=[pk])
                        A(lambda e, pb=pb, mc=mc: e.activation(out=PR[:, mc, :], in_=pb[:], func=AF.Exp, scale=1.0 / 16), r=[pk], w=PRK)
                    pden, kden = nb()
                    for mc in range(2):
                        TE(lambda e, pden=pden, mc=mc: e.matmul(pden[:], lhsT=onesb[:], rhs=PR[:, mc, :], start=(mc == 0), stop=(mc == 1)), r=PRK + ["onesb"], w=[kden])
                    V(lambda e, pden=pden: e.reciprocal(out=rsb[:], in_=pden[:]), r=[kden], w=["rsb"])
                    for dc in range(2):
                        pb, pk = nb()
                        for mc in range(2):
                            TE(lambda e, pb=pb, mc=mc, dc=dc, hh=hh: e.matmul(pb[:], lhsT=Vtm[:, mc, hh * 256 + dc * 128:hh * 256 + (dc + 1) * 128], rhs=PR[:, mc, :], start=(mc == 0), stop=(mc == 1)),
                               r=["ROPE"] + PRK, w=[pk])
                        V(lambda e, pb=pb, dc=dc, hh=hh, ts=ts: e.tensor_tensor(out=AO[:, hh * 2 + dc, ts], in0=pb[:], in1=rsb[:], op=ALU.mult), r=[pk, "rsb"], w=AOK)
            for q4 in range(4):
                wo_, ko_ = getw(12 + q4)
                for j in range(2):
                    mt = q4 * 2 + j
                    for tt in range(4):
                        ts = slice(tt * 512, (tt + 1) * 512)
                        pb, pk = nb()
                        for k in range(8):
                            TE(lambda e, pb=pb, k=k, j=j, ts=ts, wo_=wo_: e.matmul(pb[:], lhsT=wo_[:, k, j * 128:(j + 1) * 128], rhs=AO[:, k, ts], start=(k == 0), stop=(k == 7)), r=[ko_] + AOK, w=[pk])
                        V(lambda e, pb=pb, mt=mt, ts=ts: e.tensor_tensor(out=hT[:, mt, ts], in0=hT[:, mt, ts], in1=pb[:], op=ALU.add), r=[pk, "hT"], w=["hT"])

        def ffn(s, l):
            UG = SCR[:, 0:4100].bitcast(F32)
            UV = SCR[:, 4352:8452].bitcast(F32)
            CG_ = SCR[:, 8704:12800].bitcast(F32)
            CV_ = SCR[:, 12800:16896].bitcast(F32)
            cw = SCR[:, 21056:21056 + 264].bitcast(F32).rearrange("p (k j) -> p k j", j=44)
            CGb = SCR[:, REG["TM0"][0]:REG["TM0"][0] + 2048]
            CVb = SCR[:, REG["TM1"][0]:REG["TM1"][0] + 2048]
            o2 = REG[("OM", 2)][0]
            ACTs = [SCR[:, 16896:20992].rearrange("p (j t) -> p j t", t=T), SCR[:, o2:o2 + 4096].rearrange("p (j t) -> p j t", t=T)]
            ACKs = [rk(16896, 4096), rk(o2, 4096)]
            UGK, UVK, CGK, CVK, CWK = rk(0, 4100), rk(4352, 4100), rk(8704, 4096), rk(12800, 4096), rk(21056, 264)
            for k3 in range(3):
                DS(lambda e, k3=k3: e.dma_start(out=cw[:, k3, :], in_=dr["ffn_conv_w"][l, k3].rearrange("(j p) -> p j", p=128)), w=CWK)
            loaded = {}
            dn_bufs = []
            for key in (("OM", 0), ("OM", 1)):
                o_, m_ = REG[key]
                dn_bufs.append((SCR[:, o_:o_ + m_], key))

            def issue_up(g):
                res = []
                for i_, c0 in enumerate((g * 256, DFF + g * 256)):
                    si = 2 * (g % 2) + i_
                    buf, key = W[:, si, :], ("W", si)
                    dst = buf.rearrange("p (k m) -> p k m", m=256)
                    src = dr["ffn_up"][l][:, c0:c0 + 256].rearrange("(k p) m -> p k m", p=128)
                    DG_(lambda e, dst=dst, src=src: e.dma_start(out=dst, in_=src), w=[key])
                    res.append((dst, key))
                loaded[g] = res

            def issue_dn(g):
                buf, key = dn_bufs[g % 2]
                dst = buf.rearrange("p (j m) -> p j m", m=1024)
                src = dr["ffn_down"][l][g * 256:(g + 1) * 256, :].rearrange("(j p) m -> p j m", p=128)
                DG_(lambda e, dst=dst, src=src: e.dma_start(out=dst, in_=src), w=[key])
                loaded[g].append((dst, key))

            TK = lambda nm: [(nm, tt) for tt in range(4)]
            ALLT = TK("UG") + TK("UV") + TK("CG") + TK("CV") + TK("CGb") + TK("CVb") + [("ACTt", bi, j, tt) for bi in range(2) for j in range(2) for tt in range(4)]
            ALLR = sorted(set(map(repr, UGK + UVK + CGK + CVK + CWK + ["TM0", "TM1"] + ACKs[0] + ACKs[1])))
            ALLRK = []
            for k_ in UGK + UVK + CGK + CVK + CWK + ["TM0", "TM1"] + ACKs[0] + ACKs[1]:
                if k_ not in ALLRK:
                    ALLRK.append(k_)
            V(lambda e: e.memset(UG[:, 0:2], 0.0), r=ALLRK, w=ALLRK + ALLT)
            V(lambda e: e.memset(UV[:, 0:2], 0.0), w=[("UV", 0)])

            def up(g):
                (wg_, kg_), (wv_, kv_) = loaded[g][0], loaded[g][1]
                ACT = ACTs[g % 2]
                bi = g % 2
                for j in range(2):
                    ch = g * 2 + j
                    cv = 22 + ch
                    for tt in range(4):
                        c0 = tt * 512
                        hk = [("UG", tt)] + ([("UG", tt - 1)] if tt > 0 else [])
                        hv = [("UV", tt)] + ([("UV", tt - 1)] if tt > 0 else [])
                        pb, pk = proj64(wg_, kg_, j * 128, 128, tt)
                        A(lambda e, pb=pb, c0=c0: e.activation(out=UG[:, 2 + c0:2 + c0 + 512], in_=pb[:], func=AF.Copy), r=[pk], w=[("UG", tt)])
                        pb, pk = proj64(wv_, kv_, j * 128, 128, tt)
                        A(lambda e, pb=pb, c0=c0: e.activation(out=UV[:, 2 + c0:2 + c0 + 512], in_=pb[:], func=AF.Copy), r=[pk], w=[("UV", tt)])
                        A(lambda e, ch=ch, c0=c0: e.activation(out=CG_[:, c0:c0 + 512], in_=UG[:, c0:c0 + 512], func=AF.Copy, scale=cw[:, 0, ch:ch + 1]), r=hk + CWK, w=[("CG", tt)])
                        A(lambda e, cv=cv, c0=c0: e.activation(out=CV_[:, c0:c0 + 512], in_=UV[:, c0:c0 + 512], func=AF.Copy, scale=cw[:, 0, cv:cv + 1]), r=hv + CWK, w=[("CV", tt)])
                        V(lambda e, ch=ch, c0=c0: e.scalar_tensor_tensor(out=CG_[:, c0:c0 + 512], in0=UG[:, 1 + c0:1 + c0 + 512], scalar=cw[:, 1, ch:ch + 1], in1=CG_[:, c0:c0 + 512], op0=ALU.mult, op1=ALU.add), r=hk + CWK + [("CG", tt)], w=[("CG", tt)])
                        V(lambda e, ch=ch, c0=c0: e.scalar_tensor_tensor(out=CG_[:, c0:c0 + 512], in0=UG[:, 2 + c0:2 + c0 + 512], scalar=cw[:, 2, ch:ch + 1], in1=CG_[:, c0:c0 + 512], op0=ALU.mult, op1=ALU.add), r=hk + CWK + [("CG", tt)], w=[("CG", tt)])
                        A(lambda e, c0=c0: e.activation(out=CGb[:, c0:c0 + 512], in_=CG_[:, c0:c0 + 512], func=AF.Silu), r=[("CG", tt)], w=[("CGb", tt)])
                        V(lambda e, cv=cv, c0=c0: e.scalar_tensor_tensor(out=CV_[:, c0:c0 + 512], in0=UV[:, 1 + c0:1 + c0 + 512], scalar=cw[:, 1, cv:cv + 1], in1=CV_[:, c0:c0 + 512], op0=ALU.mult, op1=ALU.add), r=hv + CWK + [("CV", tt)], w=[("CV", tt)])
                        V(lambda e, cv=cv, c0=c0: e.scalar_tensor_tensor(out=CVb[:, c0:c0 + 512], in0=UV[:, 2 + c0:2 + c0 + 512], scalar=cw[:, 2, cv:cv + 1], in1=CV_[:, c0:c0 + 512], op0=ALU.mult, op1=ALU.add), r=hv + CWK + [("CV", tt)], w=[("CVb", tt)])
                        V(lambda e, j=j, ACT=ACT, c0=c0: e.tensor_tensor(out=ACT[:, j, c0:c0 + 512], in0=CGb[:, c0:c0 + 512], in1=CVb[:, c0:c0 + 512], op=ALU.mult), r=[("CGb", tt), ("CVb", tt)], w=[("ACTt", bi, j, tt)])

            def down(g):
                (wd_, kd_) = loaded[g][2]
                ACT, ACK = ACTs[g % 2], ACKs[g % 2]
                for mt in range(8):
                    for tt in range(4):
                        ts = slice(tt * 512, (tt + 1) * 512)
                        pb, pk = nb()
                        for j in range(2):
                            TE(lambda e, pb=pb, j=j, mt=mt, ts=ts, wd_=wd_, ACT=ACT: e.matmul(pb[:], lhsT=wd_[:, j, mt * 128:(mt + 1) * 128], rhs=ACT[:, j, ts], start=(j == 0), stop=(j == 1)), r=[kd_, ("ACTt", g % 2, j, tt)], w=[pk])
                        V(lambda e, pb=pb, mt=mt, ts=ts: e.tensor_tensor(out=hT[:, mt, ts], in0=hT[:, mt, ts], in1=pb[:], op=ALU.add), r=[pk, "hT"], w=["hT"])

            issue_up(0)
            issue_dn(0)
            issue_up(1)
            issue_dn(1)
            for g in range(11):
                up(g)
                if g > 0:
                    down(g - 1)
                    if g + 1 < 11:
                        issue_dn(g + 1)
                if g + 2 < 11:
                    issue_up(g + 2)
            down(10)
            V(lambda e: e.memset(UG[:, 0:2], 0.0), r=ALLT, w=ALLRK + ALLT)

        XNK = [("xnT", i) for i in range(4)]
        xs = xnT[:].rearrange("p k t -> p (k t)").bitcast(F32)
        out_ops = []
        for s in (seqs if seqs is not None else range(NSEQ)):
            for t16 in range(16):
                xt_ = xs[:, (t16 % 4) * 1024:(t16 % 4 + 1) * 1024]
                xsk = ("xs", t16 % 4)
                DS(lambda e, xt_=xt_, t16=t16: e.dma_start(out=xt_, in_=dr["x"][s, t16 * 128:(t16 + 1) * 128, :]), w=([xsk] + XNK if t16 < 4 else [xsk]))
                for g in range(2):
                    pb, pk = nb()
                    for j in range(4):
                        k = g * 4 + j
                        TE(lambda e, pb=pb, j=j, k=k, xt_=xt_: e.transpose(out=pb[:, j * 128:(j + 1) * 128], in_=xt_[:, k * 128:(k + 1) * 128], identity=ident[:]), r=[xsk, "ident"], w=[pk])
                    A(lambda e, pb=pb, g=g, t16=t16: e.activation(out=hT[:, g * 4:(g + 1) * 4, t16 * 128:(t16 + 1) * 128], in_=pb[:].rearrange("p (k t) -> p k t", t=128), func=AF.Copy), r=[pk], w=["hT"])
            for mt in range(2):
                mt_ = xs[:, 4096 + mt * 1024:4096 + (mt + 1) * 1024]
                DS(lambda e, mt_=mt_, mt=mt: e.dma_start(out=mt_, in_=dr["mem"][s, mt * 128:(mt + 1) * 128, :]), w=XNK)
                A(lambda e, mt_=mt_: e.activation(out=xs[:, 6144:7168], in_=mt_, func=AF.Square, accum_out=rsb[:, 0:1]), r=XNK, w=XNK + ["rsb"])
                A(lambda e: e.activation(out=rsb[:, 1:2], in_=rsb[:, 0:1], func=AF.Sqrt, scale=1.0 / D, bias=EPS), r=["rsb"], w=["rsb"])
                V(lambda e: e.reciprocal(out=rsb[:, 2:3], in_=rsb[:, 1:2]), r=["rsb"], w=["rsb"])
                V(lambda e, mt_=mt_: e.tensor_scalar(out=mt_, in0=mt_, scalar1=rsb[:, 2:3], scalar2=None, op0=ALU.mult), r=XNK + ["rsb"], w=XNK)
                for g in range(2):
                    pb, pk = nb()
                    for j in range(4):
                        k = g * 4 + j
                        TE(lambda e, pb=pb, j=j, k=k, mt_=mt_: e.transpose(out=pb[:, j * 128:(j + 1) * 128], in_=mt_[:, k * 128:(k + 1) * 128], identity=ident[:]), r=XNK + ["ident"], w=[pk])
                    for j in range(4):
                        k = g * 4 + j
                        V(lambda e, pb=pb, j=j, k=k, mt=mt: e.tensor_scalar(out=memT[:, k, mt * 128:(mt + 1) * 128], in0=pb[:, j * 128:(j + 1) * 128], scalar1=memnw[:, k:k + 1], scalar2=None, op0=ALU.mult),
                          r=[pk, "memnw"], w=["memT"])
            for l in (layers if layers is not None else range(NLAYER)):
                if "mix" in stages:
                    norm(0 + l)
                    if 0 in mixers:
                        retention(s, l)
                    if 1 in mixers:
                        gdn(s, l)
                    if 2 in mixers:
                        diag_gated(s, l, 2)
                    if 3 in mixers:
                        diag_gated(s, l, 3)
                if "xat" in stages:
                    norm(2 + l)
                    xattn(s, l)
                if "ffn" in stages:
                    norm(4 + l)
                    ffn(s, l)
            yn = xs[:, 0:4096].rearrange("p (k t) -> p k t", t=512)
            for tt in range(4):
                ts = slice(tt * 512, (tt + 1) * 512)
                A(lambda e, ts=ts: e.activation(out=sq, in_=hT[:, :, ts], func=AF.Square), r=["hT"], w=SQK)
                pb, pk = nb()
                for k in range(8):
                    TE(lambda e, k=k, pb=pb: e.matmul(pb[:], lhsT=onesb[:], rhs=sq[:, k, :], start=(k == 0), stop=(k == 7)), r=SQK + ["onesb"], w=[pk])
                A(lambda e, pb=pb: e.activation(out=rsb[:], in_=pb[:], func=AF.Ln, scale=1.0 / D, bias=EPS), r=[pk], w=["rsb"])
                A(lambda e: e.activation(out=rsb[:], in_=rsb[:], func=AF.Exp, scale=-0.5), r=["rsb"], w=["rsb"])
                for k in range(8):
                    V(lambda e, k=k, ts=ts: e.scalar_tensor_tensor(out=yn[:, k, :], in0=hT[:, k, ts], scalar=normw[:, 6, k:k + 1], in1=rsb[:], op0=ALU.mult, op1=ALU.mult), r=["hT", "rsb", "normw"], w=([("yn", k)] + XNK if tt == 0 else [("yn", k)]))
                for t4 in range(4):
                    stg = xs[:, 4096 + t4 * 1024:4096 + (t4 + 1) * 1024]
                    for g in range(2):
                        pb, pk = nb()
                        for j in range(4):
                            k = g * 4 + j
                            TE(lambda e, pb=pb, j=j, k=k, t4=t4: e.transpose(out=pb[:, j * 128:(j + 1) * 128], in_=yn[:, k, t4 * 128:(t4 + 1) * 128], identity=ident[:]), r=[("yn", k), "ident"], w=[pk])
                        A(lambda e, pb=pb, g=g, stg=stg: e.activation(out=stg[:, g * 512:(g + 1) * 512], in_=pb[:], func=AF.Copy), r=[pk], w=[("ystg", t4)])
                    t0 = tt * 512 + t4 * 128
                    out_ops.append(DS(lambda e, stg=stg, t0=t0: e.dma_start(out=y_d[s, t0:t0 + 128, :], in_=stg), r=[("ystg", t4)] + XNK))
        P.emit(final_wait_ops=out_ops[-8:] + tap_ops)
    return nc


_CACHE = {}


def kernel(**inputs):
    if "nc" not in _CACHE:
        _CACHE["nc"] = build()
    nc = _CACHE["nc"]
    consts = make_consts()
    in_maps = []
    for c in range(8):
        m = {}
        m["x"] = np.ascontiguousarray(inputs["x"][2 * c:2 * c + 2]).astype(np.float32, copy=False)
        m["mem"] = np.ascontiguousarray(inputs["mem"][2 * c:2 * c + 2]).astype(np.float32, copy=False)
        m["positions"] = np.ascontiguousarray(inputs["positions"][2 * c:2 * c + 2]).astype(np.int32, copy=False)
        for k in WEIGHTS:
            m[k] = np.ascontiguousarray(np.asarray(inputs[k], dtype=np.float32))
        for k in CONST_SHAPES:
            m["c_" + k] = np.ascontiguousarray(consts[k])
        in_maps.append(m)
    res = run_bass_kernel_spmd(nc, in_maps, core_ids=list(range(8)))
    return np.concatenate([np.asarray(r["y"]) for r in res.results], axis=0)
```
